# Optimizing a Trainium2 kernel written in Bass

```python
import math
import jax, jax.numpy as jnp
from jax import lax
import numpy as np

D_MODEL = 1024
BATCH = 8
SEQ = 4096
DEPTH = 2

D_MIX = D_MODEL
HEAD_DIM = 64
D_SSD = D_MIX // 2
D_ATT = D_MIX - D_SSD
N_SSD_HEADS = D_SSD // HEAD_DIM
N_ATT_HEADS = D_ATT // HEAD_DIM
SSD_GROUPS = 2
D_STATE = 128
CONV_K = 4
CHUNK = 128
Q_BLOCK = 128
D_CONV = D_SSD + 2 * SSD_GROUPS * D_STATE
D_IN_SSD = D_SSD + D_CONV + N_SSD_HEADS
D_IN_ATT = 3 * D_ATT + N_ATT_HEADS
D_IN = D_IN_SSD + D_IN_ATT
D_FF_DENSE = 11 * D_MODEL // 4
N_EXPERTS = 8
TOP_K = 2
D_FF_EXPERT = D_FF_DENSE // 2
PLE_DIM = 256
N_DENSE = (DEPTH + 1) // 2
N_MOE = DEPTH // 2
EPS = 1e-6

kernel_name = 'hybrid_ssd_fox_moe_ple_block'


def rmsnorm(x, g):
    xf = x.astype(jnp.float32)
    y = xf * lax.rsqrt(jnp.mean(xf * xf, axis=-1, keepdims=True) + EPS)
    return (y * g.astype(jnp.float32)).astype(x.dtype)


def causal_depthwise_conv(u, w, b):
    c = u.shape[-1]
    y = lax.conv_general_dilated(
        u, w[:, None, :].astype(u.dtype), window_strides=(1,),
        padding=[(w.shape[0] - 1, 0)], dimension_numbers=('NWC', 'WIO', 'NWC'),
        feature_group_count=c)
    return y + b.astype(u.dtype)


def segsum(a):
    t = a.shape[-1]
    c = jnp.cumsum(a, axis=-1)
    d = c[..., :, None] - c[..., None, :]
    return jnp.where(jnp.tril(jnp.ones((t, t), dtype=bool)), d, -jnp.inf)


def ssd_chunked(xh, dt, a, bm, cm):
    b, s, h, pdim = xh.shape
    nc = s // CHUNK
    rep = h // bm.shape[2]
    bm = jnp.repeat(bm, rep, axis=2).reshape(b, nc, CHUNK, h, D_STATE)
    cm = jnp.repeat(cm, rep, axis=2).reshape(b, nc, CHUNK, h, D_STATE)
    xdt = (xh * dt[..., None]).reshape(b, nc, CHUNK, h, pdim)
    adt = (dt * a).reshape(b, nc, CHUNK, h).transpose(0, 3, 1, 2)
    a_cum = jnp.cumsum(adt, axis=-1)
    decay_in = jnp.exp(segsum(adt))
    y_diag = jnp.einsum('bclhn,bcshn,bhcls,bcshp->bclhp', cm, bm, decay_in, xdt)
    decay_to_end = jnp.exp(a_cum[..., -1:] - a_cum)
    states = jnp.einsum('bclhn,bhcl,bclhp->bchpn', bm, decay_to_end, xdt)
    chunk_decay = jnp.exp(a_cum[..., -1])

    def step(hstate, inp):
        s_c, d_c = inp
        return hstate * d_c[..., None, None] + s_c, hstate

    h0 = jnp.zeros((b, h, pdim, D_STATE), jnp.float32)
    _, prev = lax.scan(step, h0, (states.transpose(1, 0, 2, 3, 4), chunk_decay.transpose(2, 0, 1)))
    prev = prev.transpose(1, 0, 2, 3, 4)
    y_off = jnp.einsum('bclhn,bchpn,bhcl->bclhp', cm, prev, jnp.exp(a_cum))
    return (y_diag + y_off).reshape(b, s, h, pdim)


def forgetting_attention(q, k, v, log_f):
    b, s, h, d = q.shape
    nb = s // Q_BLOCK
    f_cum = jnp.cumsum(log_f.astype(jnp.float32), axis=1).transpose(0, 2, 1)
    q_blocks = q.reshape(b, nb, Q_BLOCK, h, d).transpose(1, 0, 2, 3, 4)
    fq_blocks = f_cum.reshape(b, h, nb, Q_BLOCK).transpose(2, 0, 1, 3)
    key_pos = jnp.arange(s)
    scale = d ** -0.5

    def one_block(args):
        i, q_i, fq_i = args
        logits = jnp.einsum('bqhd,bkhd->bhqk', q_i, k).astype(jnp.float32) * scale
        logits = logits + fq_i[..., :, None] - f_cum[..., None, :]
        q_pos = i * Q_BLOCK + jnp.arange(Q_BLOCK)
        logits = jnp.where(q_pos[:, None] >= key_pos[None, :], logits, -jnp.inf)
        probs = jax.nn.softmax(logits, axis=-1)
        return jnp.einsum('bhqk,bkhd->bqhd', probs.astype(v.dtype), v)

    out = lax.map(one_block, (jnp.arange(nb), q_blocks, fq_blocks))
    return out.transpose(1, 0, 2, 3, 4).reshape(b, s, h, d)


def swiglu(t, wg, wu, wd):
    return (jax.nn.silu(t @ wg) * (t @ wu)) @ wd


def moe_swiglu(h, w_router, wg, wu, wd):
    b, s, d = h.shape
    t = h.reshape(b * s, d)
    logits = (t @ w_router).astype(jnp.float32)
    top_v, top_i = lax.top_k(logits, TOP_K)
    gates = jax.nn.softmax(top_v, axis=-1)
    combine = jnp.sum(jax.nn.one_hot(top_i, N_EXPERTS, dtype=jnp.float32) * gates[..., None], axis=1)
    out = jnp.zeros_like(t)
    for e in range(N_EXPERTS):
        out = out + combine[:, e:e + 1].astype(t.dtype) * swiglu(t, wg[e], wu[e], wd[e])
    return out.reshape(b, s, d)


def setup_inputs(seed: int = 0) -> dict:
    key = jax.random.key(seed)
    ks = jax.random.split(key, 32)
    f32 = jnp.float32

    def nrm(k, shape, fan_in):
        return jax.random.normal(k, shape, f32) * (fan_in ** -0.5)

    def gain(k, shape):
        return 1.0 + 0.05 * jax.random.normal(k, shape, f32)

    dt0 = jnp.exp(jax.random.uniform(ks[6], (DEPTH, N_SSD_HEADS), f32,
                                     minval=math.log(1e-3), maxval=math.log(1e-1)))
    return {
        'x': jax.random.normal(ks[0], (BATCH, SEQ, D_MODEL), f32),
        'p': jax.random.normal(ks[1], (DEPTH, BATCH, SEQ, PLE_DIM), f32),
        'norm1_g': gain(ks[2], (DEPTH, D_MODEL)),
        'w_in': nrm(ks[3], (DEPTH, D_MODEL, D_IN), D_MODEL),
        'conv_w': nrm(ks[4], (DEPTH, CONV_K, D_CONV), CONV_K),
        'conv_b': 0.01 * jax.random.normal(ks[5], (DEPTH, D_CONV), f32),
        'dt_bias': dt0 + jnp.log(-jnp.expm1(-dt0)),
        'a_log': jnp.log(jax.random.uniform(ks[7], (DEPTH, N_SSD_HEADS), f32, minval=1.0, maxval=16.0)),
        'd_skip': gain(ks[8], (DEPTH, N_SSD_HEADS)),
        'ssd_norm_g': gain(ks[9], (DEPTH, D_SSD)),
        'fg_bias': 2.0 + 0.5 * jax.random.normal(ks[10], (DEPTH, N_ATT_HEADS), f32),
        'q_norm_g': gain(ks[11], (DEPTH, HEAD_DIM)),
        'k_norm_g': gain(ks[12], (DEPTH, HEAD_DIM)),
        'attn_norm_g': gain(ks[13], (DEPTH, D_ATT)),
        'w_out': nrm(ks[14], (DEPTH, D_MIX, D_MODEL), D_MIX),
        'norm2_g': gain(ks[15], (DEPTH, D_MODEL)),
        'w_gate_dense': nrm(ks[16], (N_DENSE, D_MODEL, D_FF_DENSE), D_MODEL),
        'w_up_dense': nrm(ks[17], (N_DENSE, D_MODEL, D_FF_DENSE), D_MODEL),
        'w_down_dense': nrm(ks[18], (N_DENSE, D_FF_DENSE, D_MODEL), D_FF_DENSE),
        'w_router': nrm(ks[19], (N_MOE, D_MODEL, N_EXPERTS), D_MODEL),
        'w_gate_exp': nrm(ks[20], (N_MOE, N_EXPERTS, D_MODEL, D_FF_EXPERT), D_MODEL),
        'w_up_exp': nrm(ks[21], (N_MOE, N_EXPERTS, D_MODEL, D_FF_EXPERT), D_MODEL),
        'w_down_exp': nrm(ks[22], (N_MOE, N_EXPERTS, D_FF_EXPERT, D_MODEL), D_FF_EXPERT),
        'ple_norm_g': gain(ks[23], (DEPTH, D_MODEL)),
        'w_ple_gate': nrm(ks[24], (DEPTH, D_MODEL, D_MODEL), D_MODEL),
        'w_ple_proj': nrm(ks[25], (DEPTH, PLE_DIM, D_MODEL), PLE_DIM),
    }


def reference(x, p, norm1_g, w_in, conv_w, conv_b, dt_bias, a_log, d_skip, ssd_norm_g,
              fg_bias, q_norm_g, k_norm_g, attn_norm_g, w_out, norm2_g,
              w_gate_dense, w_up_dense, w_down_dense, w_router, w_gate_exp, w_up_exp,
              w_down_exp, ple_norm_g, w_ple_gate, w_ple_proj):
    f32 = jnp.float32
    b, s, _ = x.shape
    split_in = [D_SSD, D_SSD + D_CONV, D_IN_SSD, D_IN_SSD + D_ATT,
                D_IN_SSD + 2 * D_ATT, D_IN_SSD + 3 * D_ATT]
    for i in range(DEPTH):
        u = rmsnorm(x, norm1_g[i])
        proj = u @ w_in[i]
        z, xbc, dt_raw, q, k, v, f_raw = jnp.split(proj, split_in, axis=-1)

        xbc = jax.nn.silu(causal_depthwise_conv(xbc, conv_w[i], conv_b[i]))
        xs, bm, cm = jnp.split(xbc, [D_SSD, D_SSD + SSD_GROUPS * D_STATE], axis=-1)
        dt = jax.nn.softplus(dt_raw.astype(f32) + dt_bias[i].astype(f32))
        a = -jnp.exp(a_log[i].astype(f32))
        xh = xs.reshape(b, s, N_SSD_HEADS, HEAD_DIM).astype(f32)
        y = ssd_chunked(xh, dt, a,
                        bm.reshape(b, s, SSD_GROUPS, D_STATE).astype(f32),
                        cm.reshape(b, s, SSD_GROUPS, D_STATE).astype(f32))
        y = y + d_skip[i].astype(f32)[:, None] * xh
        y_ssd = rmsnorm(y.reshape(b, s, D_SSD) * jax.nn.silu(z.astype(f32)), ssd_norm_g[i]).astype(x.dtype)

        qh = rmsnorm(q.reshape(b, s, N_ATT_HEADS, HEAD_DIM), q_norm_g[i])
        kh = rmsnorm(k.reshape(b, s, N_ATT_HEADS, HEAD_DIM), k_norm_g[i])
        vh = v.reshape(b, s, N_ATT_HEADS, HEAD_DIM)
        log_f = jax.nn.log_sigmoid(f_raw.astype(f32) + fg_bias[i].astype(f32))
        y_att = forgetting_attention(qh, kh, vh, log_f)
        y_att = rmsnorm(y_att.reshape(b, s, D_ATT), attn_norm_g[i])

        x = x + jnp.concatenate([y_ssd, y_att], axis=-1) @ w_out[i]

        u2 = rmsnorm(x, norm2_g[i])
        j = i // 2
        if i % 2 == 0:
            x = x + swiglu(u2, w_gate_dense[j], w_up_dense[j], w_down_dense[j])
        else:
            x = x + moe_swiglu(u2, w_router[j], w_gate_exp[j], w_up_exp[j], w_down_exp[j])

        gate = jax.nn.sigmoid(rmsnorm(x, ple_norm_g[i]) @ w_ple_gate[i])
        x = x + gate * (p[i] @ w_ple_proj[i])
    return x
```

```python
import contextlib
import numpy as np
import concourse.bass as bass
import concourse.mybir as mybir
from concourse.bass_utils import run_bass_kernel_spmd
from concourse.alu_op_type import AluOpType as ALU

AF = mybir.ActivationFunctionType
F32 = mybir.dt.float32
BF16 = mybir.dt.bfloat16

S = 4096
D = 1024
T = 512
NCH = S // T
NCAT = 3128
NPL = 88
EPS = 1e-6
DFF_D = 2816
DFF_E = 1408
NE = 8

SEM_WIN = 8192
NDS = 12

DEBUG = False
NO_PRECAST = False
STRICT_POOL = False
PRECAST_LIMIT = None
PE_WARM_DUMMY = True
PRECAST_STORE_Q = "pool"
STOP_AFTER = None


class Buf:
    __slots__ = ("name", "w", "r")

    def __init__(self, name=""):
        self.name = name
        self.w = None
        self.r = {}


class Prog:
    ENG = ("pe", "act", "dve", "pool", "sp")

    def __init__(self, nc, stack):
        self.nc = nc
        self.stack = stack
        self.q = {e: [] for e in self.ENG}
        self.cnt = {e: 0 for e in self.ENG}
        self.known = {e: {} for e in self.ENG}
        self.csem = {e: [] for e in self.ENG}
        self.dsem = {}
        self.dval = {}
        self.drr = {}
        for qn in ("sp", "pool", "act"):
            self.dsem[qn] = [stack.enter_context(nc.semaphore(f"d_{qn}_{i}")) for i in range(NDS)]
            self.dval[qn] = [0] * NDS
            self.drr[qn] = 0

    def _csem(self, eng, win):
        lst = self.csem[eng]
        while len(lst) <= win:
            lst.append(self.stack.enter_context(self.nc.semaphore(f"c_{eng}_{len(lst)}")))
        return lst[win]

    def _need(self, eng, waits, ev):
        key, val = ev
        if self.known[eng].get(key, 0) >= val:
            return
        self.known[eng][key] = val
        waits[key] = max(waits.get(key, 0), val)

    def _deps(self, eng, reads, writes, is_dma):
        waits = {}
        for b in reads:
            if b.w is not None:
                self._need(eng, waits, b.w[:2])
        for b in writes:
            if b.w is not None:
                k, v, we = b.w
                if is_dma or we != eng or k[0] == "d" or (STRICT_POOL and eng == "pool"):
                    self._need(eng, waits, (k, v))
            for k, (v, re) in b.r.items():
                if is_dma or re != eng or k[0] == "d" or (STRICT_POOL and eng == "pool"):
                    self._need(eng, waits, (k, v))
        return waits

    def _lower_waits(self, waits):
        out = []
        for key, val in waits.items():
            if key[0] == "c":
                win = (val - 1) // SEM_WIN
                out.append((self._csem(key[1], win), val - win * SEM_WIN))
            else:
                out.append((self.dsem[key[1]][key[2]], val))
        return out

    def _record(self, ev, eng, reads, writes):
        key, val = ev
        for b in reads:
            old = b.r.get(key)
            if old is None or old[0] < val:
                b.r[key] = (val, eng)
        for b in writes:
            b.w = (key, val, eng)
            b.r = {}

    def op(self, eng, fn, reads=(), writes=()):
        waits = self._deps(eng, reads, writes, False)
        self.cnt[eng] += 1
        idx = self.cnt[eng]
        win = (idx - 1) // SEM_WIN
        sem = self._csem(eng, win)
        self.q[eng].append((self._lower_waits(waits), fn, (sem, 1)))
        ev = (("c", eng), idx)
        self._record(ev, eng, reads, writes)
        return ev

    def dma(self, qn, fn, reads=(), writes=()):
        waits = self._deps(qn, reads, writes, True)
        slot = self.drr[qn]
        self.drr[qn] = (slot + 1) % NDS
        cur = self.dval[qn][slot]
        key = ("d", qn, slot)
        if cur > 0:
            self._need(qn, waits, (key, cur))
        self.dval[qn][slot] = cur + 16
        self.q[qn].append((self._lower_waits(waits), fn, (self.dsem[qn][slot], 16)))
        ev = (key, cur + 16)
        self._record(ev, qn, reads, writes)
        return ev

    def drain_dmas(self, eng="sp"):
        waits = {}
        for qn in ("sp", "pool", "act"):
            for s in range(NDS):
                if self.dval[qn][s] > 0:
                    self._need(eng, waits, (("d", qn, s), self.dval[qn][s]))
        self.q[eng].append((self._lower_waits(waits), None, None))

    def emit(self):
        nc = self.nc
        qs = self.q
        self.q = {e: [] for e in self.ENG}

        def run(engobj, lst):
            for waits, fn, inc in lst:
                for s, v in waits:
                    engobj.wait_ge(s, v)
                if fn is not None:
                    ins = fn(engobj)
                    ins.then_inc(inc[0], inc[1])

        with nc.Block() as block:
            @block.tensor
            def _(e):
                run(e, qs["pe"])

            @block.scalar
            def _(e):
                run(e, qs["act"])

            @block.vector
            def _(e):
                run(e, qs["dve"])

            @block.gpsimd
            def _(e):
                run(e, qs["pool"])

            @block.sync
            def _(e):
                run(e, qs["sp"])


def build_program():
    nc = bass.Bass("TRN2", target_bir_lowering=False)
    dr = lambda name, shape, dt, kind: nc.dram_tensor(name, shape, dt, kind=kind).ap()
    skind = "ExternalOutput" if DEBUG else "Internal"
    xT_in = dr("xT", [D, S], F32, "ExternalInput")
    pT_in = dr("pT", [2, 256, S], F32, "ExternalInput")
    pvec_in = dr("pvec", [128, 2 * NPL], F32, "ExternalInput")
    wincat_in = dr("wincat", [2, D, NCAT], F32, "ExternalInput")
    wout_in = dr("wout", [2, D, D], F32, "ExternalInput")
    NFD, NFE = DFF_D // 128, DFF_E // 128
    wgd_in = dr("wgd", [1, NFD, 128, 1024], F32, "ExternalInput")
    wud_in = dr("wud", [1, NFD, 128, 1024], F32, "ExternalInput")
    wdd_in = dr("wdd", [1, 8, 128, NFD * 128], F32, "ExternalInput")
    wr_in = dr("wr", [1, D, NE], F32, "ExternalInput")
    wge_in = dr("wge", [NE, NFE, 128, 1024], F32, "ExternalInput")
    wue_in = dr("wue", [NE, NFE, 128, 1024], F32, "ExternalInput")
    wde_in = dr("wde", [NE, 8, 128, NFE * 128], F32, "ExternalInput")
    wpg_in = dr("wpg", [2, D, D], F32, "ExternalInput")
    wpp_in = dr("wpp", [2, 256, D], F32, "ExternalInput")
    yT_out = dr("yT", [D, S], F32, "ExternalOutput")

    wgd_b = dr("wgd_b", [1, NFD, 128, 1024], BF16, "Internal")
    wud_b = dr("wud_b", [1, NFD, 128, 1024], BF16, "Internal")
    wdd_b = dr("wdd_b", [1, 8, 128, NFD * 128], BF16, "Internal")
    wge_b = dr("wge_b", [NE, NFE, 128, 1024], BF16, "Internal")
    wue_b = dr("wue_b", [NE, NFE, 128, 1024], BF16, "Internal")
    wde_b = dr("wde_b", [NE, 8, 128, NFE * 128], BF16, "Internal")
    qaug_d = dr("qaug_d", [8, 66, S], BF16, skind)
    kaug_d = dr("kaug_d", [8, 65, S], BF16, skind)
    v_d = dr("v_d", [8, 128, 32, 64], BF16, skind)
    yssd_d = dr("yssd_d", [512, S], BF16, skind)
    o_d = dr("o_d", [8, 64, S], F32, skind)
    xmid_d = dr("xmid_d", [D, S], F32, skind)

    with contextlib.ExitStack() as gst:
        P = Prog(nc, gst)

        uid = [0]

        def sbuf(st, name, shape, dt):
            uid[0] += 1
            return st.enter_context(nc.sbuf_tensor(f"s{uid[0]}_{name}", shape, dt))

        def psum(st, name, shape, dt):
            uid[0] += 1
            return st.enter_context(nc.psum_tensor(f"p{uid[0]}_{name}", shape, dt))

        def mm(out, lhsT, rhs, start, stop, reads, writes):
            P.op("pe", lambda e: e.matmul(out, lhsT=lhsT, rhs=rhs, start=start, stop=stop), reads, writes)

        def tr(out, in_, ident, reads, writes):
            P.op("pe", lambda e: e.transpose(out, in_, ident), reads, writes)

        def act(out, in_, func, reads, writes, bias=None, scale=None, eng="act"):
            kw = {}
            if bias is not None:
                kw["bias"] = bias
            if scale is not None:
                kw["scale"] = scale
            P.op(eng, lambda e: e.activation(out=out, in_=in_, func=func, **kw), reads, writes)

        def tt(eng, out, in0, in1, op, reads, writes):
            P.op(eng, lambda e: e.tensor_tensor(out=out, in0=in0, in1=in1, op=op), reads, writes)

        def ts(eng, out, in0, s1, s2, op0, op1, reads, writes):
            if op1 is None:
                P.op(eng, lambda e: e.tensor_scalar(out=out, in0=in0, scalar1=s1, scalar2=None, op0=op0), reads, writes)
            else:
                P.op(eng, lambda e: e.tensor_scalar(out=out, in0=in0, scalar1=s1, scalar2=s2, op0=op0, op1=op1), reads, writes)

        def stt(out, in0, scalar, in1, op0, op1, reads, writes):
            P.op("dve", lambda e: e.scalar_tensor_tensor(out=out, in0=in0, scalar=scalar, in1=in1, op0=op0, op1=op1), reads, writes)

        def cp(eng, out, in_, reads, writes):
            if eng == "act":
                P.op("act", lambda e: e.activation(out=out, in_=in_, func=AF.Copy), reads, writes)
            else:
                P.op(eng, lambda e: e.tensor_copy(out=out, in_=in_), reads, writes)

        def memset(eng, ap, val, writes):
            P.op(eng, lambda e: e.memset(ap, val), (), writes)

        def dma(qn, out, in_, reads, writes):
            P.dma(qn, lambda e: e.dma_start(out=out, in_=in_), reads, writes)

        def load_cast(dst3, src2, ncols, reads, writes):
            kc = dst3.shape[1]
            srcv = src2.rearrange("(k p) n -> p k n", p=128)
            for k in range(kc):
                c0 = 0
                while c0 < ncols:
                    c1 = min(ncols, c0 + 2048)
                    dma("pool", dst3[:, k, c0:c1], srcv[:, k, c0:c1], reads, writes)
                    c0 = c1

        ident_bf = sbuf(gst, "ident_bf", [128, 128], BF16)
        ident_f = sbuf(gst, "ident_f", [128, 128], F32)
        ones_bf = sbuf(gst, "ones_bf", [128, 128], BF16)
        ones_f = sbuf(gst, "ones_f", [128, 128], F32)
        bdones = sbuf(gst, "bdones", [128, 128], BF16)
        maskb = sbuf(gst, "maskb", [128, 4, T], BF16)
        ssdmask = sbuf(gst, "ssdmask", [128, 4, 128], BF16)
        delta = sbuf(gst, "delta", [48, 2, 4, 128], F32)
        delta_b = sbuf(gst, "delta_b", [48, 2, 4, 128], BF16)
        mhl = sbuf(gst, "mhl", [48, 2], F32)
        resetm = sbuf(gst, "resetm", [48, T], F32)
        pvec = sbuf(gst, "pvec", [128, 2 * NPL], F32)
        dvec = sbuf(gst, "dvec", [128, 8], F32)
        posF = sbuf(gst, "posF", [128, 32, 8], F32)
        st0 = contextlib.ExitStack()
        tmpf = sbuf(st0, "tmpf", [128, 4, T], F32)
        b_const = Buf("const")
        b_pvec = Buf("pvec")
        b_dvec = Buf("dvec")
        b_posF = Buf("posF")
        b_tmpf = Buf("tmpf")

        dma("sp", pvec[:], pvec_in, (), [b_pvec])
        memset("pool", ident_f[:], 1.0, [b_const])
        P.op("pool", lambda e: e.affine_select(out=ident_f[:], in_=ident_f[:], pattern=[[-1, 128]],
                                                compare_op=ALU.is_equal, fill=0.0, base=0, channel_multiplier=1),
             [b_const], [b_const])
        cp("pool", ident_bf[:], ident_f[:], [b_const], [b_const])
        memset("pool", ones_f[:], 1.0, [b_const])
        memset("pool", ones_bf[:], 1.0, [b_const])
        memset("pool", bdones[:], 0.0, [b_const])
        memset("pool", bdones[0:64, 0:64], 1.0, [b_const])
        memset("pool", bdones[64:128, 64:128], 1.0, [b_const])
        memset("pool", tmpf[:], 0.0, [b_tmpf])
        for k in range(4):
            P.op("pool", lambda e, k=k: e.affine_select(out=tmpf[:, k, :], in_=tmpf[:, k, :], pattern=[[1, T]],
                                                        compare_op=ALU.is_ge, fill=-30000.0, base=-128 * k,
                                                        channel_multiplier=-1),
                 [b_tmpf], [b_tmpf])
        cp("pool", maskb[:], tmpf[:], [b_tmpf], [b_const])
        P.op("pool", lambda e: e.affine_select(out=tmpf[:, 0, :].rearrange("p (j l) -> p j l", l=128),
                                               in_=tmpf[:, 0, :].rearrange("p (j l) -> p j l", l=128),
                                               pattern=[[0, 4], [1, 128]], compare_op=ALU.is_ge, fill=-30000.0,
                                               base=0, channel_multiplier=-1),
             [b_tmpf, b_const], [b_tmpf])
        memset("pool", tmpf[:, 1, :], 0.0, [b_tmpf])
        P.op("pool", lambda e: e.affine_select(out=tmpf[:, 1, :].rearrange("p (j l) -> p j l", l=128),
                                               in_=tmpf[:, 1, :].rearrange("p (j l) -> p j l", l=128),
                                               pattern=[[0, 4], [1, 128]], compare_op=ALU.is_ge, fill=-30000.0,
                                               base=0, channel_multiplier=-1),
             [b_tmpf], [b_tmpf])
        cp("pool", ssdmask[:].rearrange("p j l -> p (j l)"), tmpf[:, 1, :], [b_tmpf], [b_const])
        memset("pool", delta[:], 0.0, [b_const])
        for g in range(2):
            for base_p in (0, 32):
                for off in (0, 8):
                    P.op("pool", lambda e, g=g, bp=base_p, off=off: e.affine_select(
                        out=delta[bp:bp + 16, g, :, :], in_=delta[bp:bp + 16, g, :, :], pattern=[[-1, 4], [0, 128]],
                        compare_op=ALU.not_equal, fill=1.0, base=-4 * g - off, channel_multiplier=1),
                        [b_const], [b_const])
        cp("pool", delta_b[:], delta[:], [b_const], [b_const])
        memset("pool", mhl[:], 0.0, [b_const])
        for base_p in (0, 32):
            P.op("pool", lambda e, bp=base_p: e.affine_select(
                out=mhl[bp:bp + 16, 0:1], in_=mhl[bp:bp + 16, 0:1], pattern=[[0, 1]],
                compare_op=ALU.is_ge, fill=1.0, base=-8, channel_multiplier=1), [b_const], [b_const])
        ts("pool", mhl[:, 1:2], mhl[:, 0:1], -1.0, 1.0, ALU.mult, ALU.add, [b_const], [b_const])
        memset("pool", resetm[:], 1.0, [b_const])
        memset("pool", resetm[:].rearrange("p (c l) -> p c l", l=128)[:, :, 0:1], 0.0, [b_const])
        memset("pool", posF[:], 0.0, [b_posF])
        P.drain_dmas("sp")
        P.emit()
        st0.close()

        PV = lambda l, c: pvec[:, l * NPL + c: l * NPL + c + 1]

        WB = {}
        precast = []

        def add_precast(name, dst, src, ncols):
            WB[name] = Buf(name)
            c0_ = 0
            while c0_ < ncols:
                c1_ = min(ncols, c0_ + 1024)
                precast.append((WB[name], dst[:, c0_:c1_], src[:, c0_:c1_], c1_ - c0_))
                c0_ = c1_

        for f_ in range(NFD):
            add_precast(("gd", 0, f_), wgd_b[0, f_], wgd_in[0, f_], 1024)
            add_precast(("ud", 0, f_), wud_b[0, f_], wud_in[0, f_], 1024)
        for o_ in range(8):
            add_precast(("dd", 0, o_), wdd_b[0, o_], wdd_in[0, o_], NFD * 128)
        n_pre_dense = len(precast)
        for e_ in range(NE):
            for f_ in range(NFE):
                add_precast(("ge", e_, f_), wge_b[e_, f_], wge_in[e_, f_], 1024)
                add_precast(("ue", e_, f_), wue_b[e_, f_], wue_in[e_, f_], 1024)
            for o_ in range(8):
                add_precast(("de", e_, o_), wde_b[e_, o_], wde_in[e_, o_], NFE * 128)
        pre_i = [0]

        stg = sbuf(gst, "stg", [128, 3, 1024], BF16)
        b_stg = [Buf(f"stg{i}") for i in range(3)]

        def issue_precast(n, limit):
            if PRECAST_LIMIT is not None:
                limit = min(limit, PRECAST_LIMIT)
            n = min(n, limit - pre_i[0])
            while n > 0:
                g_ = min(3, n)
                items = precast[pre_i[0]:pre_i[0] + g_]
                pre_i[0] += g_
                n -= g_
                for i_, (b_, d_, s_, w_) in enumerate(items):
                    dma("pool", stg[:, i_, 0:w_], s_, (), [b_stg[i_]])
                for i_, (b_, d_, s_, w_) in enumerate(items):
                    dma(PRECAST_STORE_Q, d_, stg[:, i_, 0:w_], [b_stg[i_]], [b_])

        for layer in range(2):
            x_src = xT_in if layer == 0 else xmid_d
            x_dst = xmid_d if layer == 0 else yT_out
            xsv = x_src.rearrange("(k p) t -> p k t", p=128)
            xdv = x_dst.rearrange("(k p) t -> p k t", p=128)

            act(dvec[0:48, 0:1], PV(layer, 65)[0:48, :], AF.Exp, [b_pvec], [b_dvec])
            ts("dve", dvec[0:48, 0:1], dvec[0:48, 0:1], -1.0, None, ALU.mult, None, [b_dvec], [b_dvec])
            ts("dve", dvec[:, 1:2], PV(layer, 82), 0.125, None, ALU.mult, None, [b_pvec], [b_dvec])
            ts("dve", dvec[0:8, 2:3], PV(layer, 84)[0:8, :], -1.0, None, ALU.mult, None, [b_pvec], [b_dvec])

            with contextlib.ExitStack() as st:
                win = sbuf(st, "win", [128, 8, NCAT], BF16)
                xc = sbuf(st, "xc", [128, 8, T], F32)
                sq = sbuf(st, "sq", [128, 8, T], BF16)
                uT = sbuf(st, "uT", [128, 8, T], BF16)
                lnt = sbuf(st, "lnt", [128, T], F32)
                rstd = sbuf(st, "rstd", [128, T], F32)
                zs = sbuf(st, "zs", [128, 4, T], F32)
                xpre = sbuf(st, "xpre", [128, 8, T + 4], BF16)
                xact = sbuf(st, "xact", [128, 8, T], BF16)
                diag = sbuf(st, "diag", [128, 32, 128], BF16)
                xsB = [sbuf(st, f"xsB{i}", [128, 768], BF16) for i in range(2)]
                dt40 = sbuf(st, "dt40", [48, T], F32)
                adt = sbuf(st, "adt", [48, T], F32)
                acum = sbuf(st, "acum", [48, T], F32)
                lndt = sbuf(st, "lndt", [48, T], F32)
                lhsD = sbuf(st, "lhsD", [48, T], BF16)
                splh = sbuf(st, "splh", [48, T], BF16)
                spll = sbuf(st, "spll", [48, T], BF16)
                acomb = sbuf(st, "acomb", [48, T], BF16)
                rhsD = sbuf(st, "rhsD", [48, 2, 2, 4, 128], BF16)
                expD = [sbuf(st, f"expD{i}", [128, 4, 128], F32) for i in range(2)]
                Eb = [sbuf(st, f"Eb{i}", [128, 4, 128], F32) for i in range(2)]
                Wt = [sbuf(st, f"Wt{i}", [128, 4, 128], BF16) for i in range(2)]
                Cs = [sbuf(st, f"Cs{i}", [128, 4, 128], BF16) for i in range(2)]
                xdd = [sbuf(st, f"xdd{i}", [128, 4, 64], BF16) for i in range(2)]
                prev_f = sbuf(st, "prev_f", [128, 2, 4, 64], F32)
                prev_b = sbuf(st, "prev_b", [128, 2, 4, 64], BF16)
                ych = sbuf(st, "ych", [128, 4, T], F32)
                yout = sbuf(st, "yout", [128, 4, T], BF16)
                qkst = [sbuf(st, f"qkst{i}", [128, T], BF16) for i in range(4)]
                hsq = [sbuf(st, f"hsq{i}", [128, T], BF16) for i in range(2)]
                vsb = sbuf(st, "vsb", [128, 8, 4, 64], BF16)
                fE = sbuf(st, "fE", [8, T], F32)
                fsp = sbuf(st, "fsp", [8, T], F32)
                fcum = sbuf(st, "fcum", [8, T], F32)
                fneg = sbuf(st, "fneg", [8, T], BF16)
                fneg2 = sbuf(st, "fneg2", [8, T], BF16)
                fcar = sbuf(st, "fcar", [8, 1], F32)
                ones8 = sbuf(st, "ones8", [8, T], F32)
                pm = [psum(st, f"pm{i}", [128, T], F32) for i in range(2)]
                pn = psum(st, "pn", [128, T], F32)
                pD = psum(st, "pD", [128, 4, 128], F32)
                pAb = psum(st, "pAb", [128, 4, 128], F32)
                pGs = psum(st, "pGs", [128, T], F32)
                py = psum(st, "py", [128, 4, 128], F32)
                ptr = psum(st, "ptr", [128, 1024], BF16)
                B = {n: Buf(n) for n in ["win", "xc", "sq", "uT", "lnt", "rstd", "zs", "xpre", "xact", "diag",
                                         "dtE", "dt40", "adt", "acum", "lndt", "lhsD", "rhsD", "splh", "spll", "acomb", "prev_f", "prev_b",
                                         "ych", "ysq", "yout", "qsb", "ksb", "vsb", "fE", "fsp", "fcum", "fneg",
                                         "fcar", "fneg2", "pm0", "pm1", "pn", "pD", "pAb", "pG", "pG1", "pst", "py", "ptr",
                                         "xsB0", "xsB1", "expD0", "expD1", "Eb0", "Eb1", "Wt0", "Wt1", "Cs0", "Cs1",
                                         "xdd0", "xdd1", "hsq0", "hsq1", "qkst0", "qkst1", "qkst2", "qkst3", "rhsD0", "rhsD1",
                                         "qaug_d", "kaug_d", "v_d", "yssd_d"]}
                pmi = [0]
                deferred = [None]
                buT = [Buf(f"uT{k}") for k in range(8)]
                bsq1 = [Buf(f"sq1_{k}") for k in range(8)]

                def next_pm():
                    i = pmi[0] % 2
                    pmi[0] += 1
                    return pm[i], B[f"pm{i}"]

                load_cast(win, wincat_in[layer], NCAT, (), [B["win"]])
                for tap in range(4):
                    for o in range(8):
                        ts("dve", diag[:, tap * 8 + o, :], ident_f[:], PV(layer, 24 + tap * 8 + o), None, ALU.mult, None,
                           [b_const, b_pvec], [B["diag"]])
                memset("pool", xpre[:], 0.0, [B["xpre"]])
                memset("pool", prev_f[:], 0.0, [B["prev_f"]])
                memset("pool", prev_b[:], 0.0, [B["prev_b"]])
                memset("pool", fcar[:], 0.0, [B["fcar"]])
                memset("pool", ones8[:], 1.0, [b_const])
                memset("pool", lhsD[:], 0.0, [B["lhsD"]])
                memset("pool", lhsD[0:16, :], 1.0, [B["lhsD"]])
                memset("pool", rhsD[:], 0.0, [B["rhsD"]])
                for par in range(2):
                    for g in range(2):
                        cp("pool", rhsD[32:48, par, g, :, :], delta_b[32:48, g, :, :], [b_const], [B["rhsD"]])

                for c in range(NCH):
                    c0 = c * T
                    if layer == 0:
                        issue_precast((n_pre_dense + NCH - 1) // NCH, n_pre_dense)

                    dma("sp", xc[:], xsv[:, :, c0:c0 + T], (), [B["xc"]])
                    for k in range(8):
                        act(sq[:, k, :], xc[:, k, :], AF.Square, [B["xc"]], [bsq1[k]])
                    for k in range(8):
                        mm(pn[:], ones_bf[:], sq[:, k, :], k == 0, k == 7, [b_const, bsq1[k]], [B["pn"]])
                    act(lnt[:], pn[:], AF.Ln, [B["pn"]], [B["lnt"]], bias=EPS, scale=1.0 / D)
                    act(rstd[:], lnt[:], AF.Exp, [B["lnt"]], [B["rstd"]], scale=-0.5)
                    for k in range(8):
                        stt(uT[:, k, :], xc[:, k, :], PV(layer, k), rstd[:], ALU.mult, ALU.mult,
                            [B["xc"], b_pvec, B["rstd"]], [buT[k]])
                    for o in range(12):
                        pt, bp = next_pm()
                        for k in range(8):
                            mm(pt[:], win[:, k, o * 128:(o + 1) * 128], uT[:, k, :], k == 0, k == 7,
                               [B["win"], buT[k]], [bp])
                        if o < 4:
                            act(zs[:, o, :], pt[:], AF.Silu, [bp], [B["zs"]])
                        else:
                            cp("dve", xpre[:, o - 4, 3:3 + T], pt[:], [bp], [B["xpre"]])
                        if o == 5 and deferred[0] is not None:
                            deferred[0]()
                            deferred[0] = None
                    for o in range(8):
                        pt, bp = next_pm()
                        for tap in range(4):
                            mm(pt[:], diag[:, tap * 8 + o, :], xpre[:, o, tap:tap + T], tap == 0, tap == 3,
                               [B["diag"], B["xpre"]], [bp])
                        act(xact[:, o, :], pt[:], AF.Silu, [bp, b_pvec], [B["xact"]], bias=PV(layer, 56 + o))
                    cp("pool", xpre[:, :, 0:3], xpre[:, :, T:T + 3], [B["xpre"]], [B["xpre"]])
                    pt, bp = next_pm()
                    for k in range(8):
                        mm(pt[0:48, :], win[:, k, 1536:1584], uT[:, k, :], k == 0, k == 7, [B["win"], buT[k]], [bp])
                    act(adt[:], pt[0:48, :], AF.Exp, [bp, b_pvec], [B["adt"]], bias=PV(layer, 64)[0:48, :])
                    act(dt40[:], adt[:], AF.Ln, [B["adt"]], [B["dt40"]], bias=1.0)
                    act(lndt[:], dt40[:], AF.Ln, [B["dt40"]], [B["lndt"]])
                    ts("dve", adt[:], dt40[:], dvec[0:48, 0:1], None, ALU.mult, None, [B["dt40"], b_dvec], [B["adt"]])
                    P.op("dve", lambda e: e.tensor_tensor_scan(out=acum[:], data0=resetm[:], data1=adt[:], initial=0.0,
                                                               op0=ALU.mult, op1=ALU.add),
                         [b_const, B["adt"]], [B["acum"]])
                    tt("dve", lndt[32:48, :], lndt[32:48, :], acum[32:48, :], ALU.subtract,
                       [B["lndt"], B["acum"]], [B["lndt"]])
                    for (r0, src, bsrc, dstt, bdst) in ((0, acum, B["acum"], acomb, B["acomb"]),
                                                        (32, lndt, B["lndt"], lhsD, B["lhsD"])):
                        rs_ = slice(r0, r0 + 16)
                        cp("pool", splh[rs_, :], src[rs_, :], [bsrc], [B["splh"]])
                        tt("dve", dt40[rs_, :], src[rs_, :], splh[rs_, :], ALU.subtract, [bsrc, B["splh"], B["dt40"]], [B["dt40"]])
                        cp("pool", spll[rs_, :], dt40[rs_, :], [B["dt40"]], [B["spll"]])
                        ts("dve", dstt[rs_, :], splh[rs_, :], mhl[rs_, 0:1], None, ALU.mult, None, [B["splh"], b_const], [bdst])
                        stt(dstt[rs_, :], spll[rs_, :], mhl[rs_, 1:2], dstt[rs_, :], ALU.mult, ALU.add,
                            [B["spll"], b_const, bdst], [bdst])
                    qk_items = [(which, hp) for which in range(2) for hp in range(4)]
                    qk_pt = {}

                    qk_banks = [(pm[0][:], B["pm0"]), (pm[1][:], B["pm1"]),
                                (pD[:].rearrange("p j l -> p (j l)"), B["pD"]), (pAb[:].rearrange("p j l -> p (j l)"), B["pAb"])]

                    def qk_proj(idx):
                        which, hp = qk_items[idx]
                        col0 = (1584 if which == 0 else 2096) + hp * 128
                        pt, bp = qk_banks[idx % 4]
                        for k in range(8):
                            mm(pt[:], win[:, k, col0:col0 + 128], uT[:, k, :], k == 0, k == 7, [B["win"], buT[k]], [bp])
                        qk_pt[idx] = (pt, bp)

                    def qk_norm(idx):
                        which, hp = qk_items[idx]
                        pt, bp = qk_pt[idx]
                        i = idx % 2
                        qi = idx % 4
                        dst = qkst[qi]
                        bdst = B[f"qkst{qi}"]
                        gcol = dvec[:, 1:2] if which == 0 else PV(layer, 83)
                        act(hsq[i][:], pt[:], AF.Square, [bp], [B[f"hsq{i}"]])
                        mm(pn[:], bdones[:], hsq[i][:], True, True, [b_const, B[f"hsq{i}"]], [B["pn"]])
                        act(lnt[:], pn[:], AF.Ln, [B["pn"]], [B["lnt"]], bias=EPS, scale=1.0 / 64)
                        act(rstd[:], lnt[:], AF.Exp, [B["lnt"]], [B["rstd"]], scale=-0.5)
                        stt(dst[:], pt[:], gcol, rstd[:], ALU.mult, ALU.mult, [bp, b_dvec, b_pvec, B["rstd"]], [bdst])
                        ddst = qaug_d if which == 0 else kaug_d
                        for half in range(2):
                            dma("pool", ddst[2 * hp + half, 0:64, c0:c0 + T], dst[half * 64:(half + 1) * 64, :], [bdst], ())

                    qk_proj(0)
                    qk_proj(1)
                    for idx in range(8):
                        if idx + 2 < 8:
                            qk_proj(idx + 2)
                        qk_norm(idx)
                    pt, bp = next_pm()
                    for k in range(8):
                        mm(pt[0:8, :], win[:, k, 3120:3128], uT[:, k, :], k == 0, k == 7, [B["win"], buT[k]], [bp])
                    act(fE[:], pt[0:8, :], AF.Exp, [bp, b_dvec], [B["fE"]], bias=dvec[0:8, 2:3], scale=-1.0)
                    act(fsp[:], fE[:], AF.Ln, [B["fE"]], [B["fsp"]], bias=1.0)
                    P.op("dve", lambda e: e.tensor_tensor_scan(out=fcum[:], data0=ones8[:], data1=fsp[:], initial=fcar[:],
                                                               op0=ALU.mult, op1=ALU.add),
                         [b_const, B["fsp"], B["fcar"]], [B["fcum"]])
                    cp("dve", fcar[:], fcum[:, T - 1:T], [B["fcum"]], [B["fcar"]])
                    ts("dve", fneg[:], fcum[:], -1.0, None, ALU.mult, None, [B["fcum"]], [B["fneg"]])
                    dma("pool", qaug_d[:, 64, c0:c0 + T], fneg[:], [B["fneg"]], ())
                    stt(fE[:], fcum[:], -1.0, fneg[:], ALU.mult, ALU.subtract, [B["fcum"], B["fneg"]], [B["fE"]])
                    cp("dve", fneg2[:], fE[:], [B["fE"]], [B["fneg2"]])
                    dma("pool", qaug_d[:, 65, c0:c0 + T], fneg2[:], [B["fneg2"]], ())
                    pt, bp = next_pm()
                    for s4 in range(4):
                        tr(pt[:, s4 * 8:(s4 + 1) * 8], fcum[:, s4 * 128:(s4 + 1) * 128], ident_f[0:8, 0:8],
                           [B["fcum"], b_const], [bp])
                    cp("dve", posF[:, c * 4:(c + 1) * 4, :], pt[:, 0:32].rearrange("p (s h) -> p s h", h=8), [bp], [b_posF])
                    for t4 in range(4):
                        pt, bp = next_pm()
                        for k in range(8):
                            mm(pt[:], uT[:, k, t4 * 128:(t4 + 1) * 128], win[:, k, 2608:3120], k == 0, k == 7,
                               [B["win"], buT[k]], [bp])
                        cp("act", vsb[:, :, t4, :], pt[:].rearrange("p (h d) -> p h d", d=64), [bp], [B["vsb"]])
                    for h in range(8):
                        dma("pool", v_d[h, :, c * 4:(c + 1) * 4, :], vsb[:, h, :, :], [B["vsb"]], ())
                    pDg = [pD[:], pm[0][:].rearrange("p (j l) -> p j l", l=128)]
                    pAg = [pAb[:], pm[1][:].rearrange("p (j l) -> p j l", l=128)]
                    bpD = [B["pD"], B["pm0"]]
                    bpA = [B["pAb"], B["pm1"]]
                    pGg = [pGs[:, 0:128], pGs[:, 384:512]]
                    bpG = [B["pG"], B["pG"]]
                    B["pst"] = B["pG"]

                    def ssd_prep(sc):
                        cs = slice(sc * 128, (sc + 1) * 128)
                        xb = xsB[sc % 2]
                        bxb = B[f"xsB{sc % 2}"]
                        for o in range(6):
                            tr(ptr[:, o * 128:(o + 1) * 128], xact[:, o, cs], ident_bf[:], [B["xact"], b_const], [B["ptr"]])
                        cp("act", xb[:], ptr[:, 0:768], [B["ptr"]], [bxb])
                        par = sc % 2
                        brh = B[f"rhsD{par}"]
                        for g in range(2):
                            tt("dve", rhsD[0:16, par, g, :, :],
                               acomb[0:16, cs].unsqueeze(1).broadcast_to([16, 4, 128]),
                               delta[0:16, g, :, :], ALU.mult, [B["acomb"], b_const, B["rhsD"]], [brh])

                    def stageA(sc, g):
                        cs = slice(sc * 128, (sc + 1) * 128)
                        par = sc % 2
                        brh = B[f"rhsD{par}"]
                        mm(pGg[g], xact[:, 4 + g, cs], xact[:, 6 + g, cs], True, True, [B["xact"]], [bpG[g]])
                        mm(pDg[g].rearrange("p j l -> p (j l)"), lhsD[0:48, cs],
                           rhsD[0:48, par, g, :, :].rearrange("p j l -> p (j l)"), True, False,
                           [B["lhsD"], B["rhsD"], brh], [bpD[g]])
                        mm(pDg[g].rearrange("p j l -> p (j l)"), ident_bf[:], ssdmask[:].rearrange("p j l -> p (j l)"),
                           False, True, [b_const], [bpD[g]])
                        mm(pAg[g].rearrange("p j l -> p (j l)"), ones_bf[0:16, :],
                           rhsD[0:16, par, g, :, :].rearrange("p j l -> p (j l)"), True, True,
                           [b_const, brh], [bpA[g]])

                    def stageAct(sc, g):
                        cs = slice(sc * 128, (sc + 1) * 128)
                        i = g
                        act(expD[i][:], pDg[g], AF.Exp, [bpD[g]], [B[f"expD{i}"]])
                        act(Eb[i][:], pAg[g], AF.Exp, [bpA[g]], [B[f"Eb{i}"]])
                        tt("dve", Wt[i][:], expD[i][:], pGg[g].unsqueeze(1).broadcast_to([128, 4, 128]), ALU.mult,
                           [B[f"expD{i}"], bpG[g]], [B[f"Wt{i}"]])
                        tt("pool", Cs[i][:], Eb[i][:], xact[:, 6 + g, cs].unsqueeze(1).broadcast_to([128, 4, 128]), ALU.mult,
                           [B[f"Eb{i}"], B["xact"]], [B[f"Cs{i}"]])

                    def stageB(sc, g):
                        i = g
                        xb = xsB[sc % 2]
                        bxb = B[f"xsB{sc % 2}"]
                        for j in range(4):
                            h = 4 * g + j
                            hp, half = h // 2, h % 2
                            mm(py[half * 64:(half + 1) * 64, hp, :], xb[:, h * 64:(h + 1) * 64], Wt[i][:, j, :], True, False,
                               [bxb, B[f"Wt{i}"]], [B["py"]])
                            mm(py[half * 64:(half + 1) * 64, hp, :], prev_b[:, g, j, :], Cs[i][:, j, :], False, True,
                               [B["prev_b"], B[f"Cs{i}"]], [B["py"]])

                    def stageC(sc, g):
                        i = g
                        xb = xsB[sc % 2]
                        bxb = B[f"xsB{sc % 2}"]
                        tt("dve", xdd[i][:], xb[:, g * 256:(g + 1) * 256].rearrange("p (j d) -> p j d", d=64),
                           expD[i][:, :, 127:128].broadcast_to([128, 4, 64]), ALU.mult,
                           [bxb, B[f"expD{i}"]], [B[f"xdd{i}"]])
                        mm(pGs[:, 128:384], xb[:, 512 + g * 128:512 + (g + 1) * 128], xdd[i][:].rearrange("p j d -> p (j d)"),
                           True, True, [bxb, B[f"xdd{i}"]], [B["pst"]])
                        tt("dve", prev_f[:, g, :, :], prev_f[:, g, :, :], Eb[i][:, :, 127:128].broadcast_to([128, 4, 64]), ALU.mult,
                           [B["prev_f"], B[f"Eb{i}"]], [B["prev_f"]])
                        tt("dve", prev_f[:, g, :, :], prev_f[:, g, :, :], pGs[:, 128:384].rearrange("p (j d) -> p j d", d=64), ALU.add,
                           [B["prev_f"], B["pst"]], [B["prev_f"]])
                        cp("pool", prev_b[:, g, :, :], prev_f[:, g, :, :], [B["prev_f"]], [B["prev_b"]])

                    ssd_prep(0)
                    for sc in range(4):
                        cs = slice(sc * 128, (sc + 1) * 128)
                        for g in range(2):
                            stageA(sc, g)
                        for g in range(2):
                            stageAct(sc, g)
                        if sc + 1 < 4:
                            ssd_prep(sc + 1)
                        for g in range(2):
                            stageB(sc, g)
                        for g in range(2):
                            stageC(sc, g)
                        for hp in range(4):
                            stt(ych[:, hp, cs], xact[:, hp, cs], PV(layer, 66 + hp), py[:, hp, :], ALU.mult, ALU.add,
                                [B["xact"], b_pvec, B["py"]], [B["ych"]])
                    tt("pool", ych[:], ych[:], zs[:], ALU.mult, [B["ych"], B["zs"]], [B["ych"]])

                    def finish_ssd(c0=c0):
                        for k in range(4):
                            act(sq[:, k, :], ych[:, k, :], AF.Square, [B["ych"]], [bsq1[k]])
                        for k in range(4):
                            mm(pn[:], ones_bf[:], sq[:, k, :], k == 0, k == 3, [b_const, bsq1[k]], [B["pn"]])
                        act(lnt[:], pn[:], AF.Ln, [B["pn"]], [B["lnt"]], bias=EPS, scale=1.0 / 512)
                        act(rstd[:], lnt[:], AF.Exp, [B["lnt"]], [B["rstd"]], scale=-0.5)
                        for k in range(4):
                            stt(yout[:, k, :], ych[:, k, :], PV(layer, 70 + k), rstd[:], ALU.mult, ALU.mult,
                                [B["ych"], b_pvec, B["rstd"]], [B["yout"]])
                        dma("pool", yssd_d.rearrange("(k p) t -> p k t", p=128)[:, :, c0:c0 + T], yout[:], [B["yout"]], ())

                    deferred[0] = finish_ssd
                deferred[0]()
                P.drain_dmas("sp")
                P.emit()
            if STOP_AFTER == (layer, "p1"):
                break

            with contextlib.ExitStack() as st:
                kaug = [sbuf(st, f"kaug{i}", [66, S], BF16) for i in range(2)]
                vaug = [sbuf(st, f"vaug{i}", [128, 32, 128], BF16) for i in range(2)]
                qaug = [sbuf(st, f"qaug{i}", [66, T], BF16) for i in range(2)]
                pT = [sbuf(st, f"pT{i}", [128, T], BF16) for i in range(3)]
                rec = [sbuf(st, f"rec{i}", [64, T], F32) for i in range(2)]
                osb = [sbuf(st, f"osb{i}", [64, T], F32) for i in range(2)]
                ps_s = [psum(st, f"ps_s{i}", [128, T], F32) for i in range(3)]
                ps_o = [psum(st, f"ps_o{i}", [128, T], F32) for i in range(2)]
                ps_w = psum(st, "ps_w", [128, T], F32)
                b_psw = Buf("ps_w")
                B = {n: Buf(n) for n in ["kaug0", "kaug1", "vaug0", "vaug1", "qaug0", "qaug1", "pT0", "pT1", "pT2",
                                         "rec0", "rec1", "osb0", "osb1", "ps_s0", "ps_s1", "ps_s2", "ps_o0", "ps_o1", "o_d"]}
                for i in range(2):
                    memset("pool", vaug[i][:, :, 64:128], 1.0, [B[f"vaug{i}"]])
                    memset("pool", kaug[i][64:66, :], 1.0, [B[f"kaug{i}"]])
                blocks = []
                for h in range(8):
                    for c in range(NCH):
                        for j in range(4 * c + 4):
                            blocks.append((h, c, j))
                NB = len(blocks)
                dma("sp", kaug[0][0:64, :], kaug_d[0, 0:64, :], (), [B["kaug0"]])
                dma("sp", vaug[0][:, :, 0:64], v_d[0], (), [B["vaug0"]])

                def s_step(bi):
                    h, c, j = blocks[bi]
                    hb = h % 2
                    qb = (h * NCH + c) % 2
                    si = bi % 3
                    if j == 0:
                        dma("sp", qaug[qb][:], qaug_d[h, :, c * T:(c + 1) * T], (), [B[f"qaug{qb}"]])
                        issue_precast(3 if layer == 0 else 2, len(precast))
                    lo = max(0, j - 4 * c) * 128
                    mm(ps_s[si][:, lo:T], kaug[hb][0:66, j * 128:(j + 1) * 128], qaug[qb][0:66, lo:T], True, j < 4 * c,
                       [B[f"kaug{hb}"], B[f"qaug{qb}"]], [B[f"ps_s{si}"]])
                    if j >= 4 * c:
                        mm(ps_s[si][:, lo:T], ident_bf[:], maskb[:, j - 4 * c, lo:T], False, True, [b_const], [B[f"ps_s{si}"]])

                s_step(0)
                s_step(1)
                for bi in range(NB):
                    h, c, j = blocks[bi]
                    hb = h % 2
                    qb = (h * NCH + c) % 2
                    si = bi % 3
                    nj = 4 * c + 4
                    po, bpo = ps_o[qb], B[f"ps_o{qb}"]
                    if bi + 2 < NB:
                        s_step(bi + 2)
                    lo = max(0, j - 4 * c) * 128
                    act(pT[si][:, lo:T], ps_s[si][:, lo:T], AF.Exp, [B[f"ps_s{si}"], b_posF], [B[f"pT{si}"]], bias=posF[:, j, h:h + 1])
                    mm(po[:, lo:T], vaug[hb][:, j, :], pT[si][:, lo:T], j == 0, j == nj - 1, [B[f"vaug{hb}"], B[f"pT{si}"]], [bpo])
                    if PE_WARM_DUMMY:
                        mm(ps_w[:], ident_bf[:], maskb[:, 0, :], True, True, [b_const], [b_psw])
                    if j == nj - 1:
                        P.op("dve", lambda e, qb=qb, po=po: e.reciprocal(out=rec[qb][:], in_=po[64:128, :]), [bpo], [B[f"rec{qb}"]])
                        tt("dve", osb[qb][:], po[0:64, :], rec[qb][:], ALU.mult, [bpo, B[f"rec{qb}"]], [B[f"osb{qb}"]])
                        dma("pool", o_d[h, :, c * T:(c + 1) * T], osb[qb][:], [B[f"osb{qb}"]], ())
                        if c == 1 and h + 1 < 8:
                            nb_ = (h + 1) % 2
                            dma("sp", kaug[nb_][0:64, :], kaug_d[h + 1, 0:64, :], (), [B[f"kaug{nb_}"]])
                            dma("sp", vaug[nb_][:, :, 0:64], v_d[h + 1], (), [B[f"vaug{nb_}"]])
                P.drain_dmas("sp")
                P.emit()
            if STOP_AFTER == (layer, "p2"):
                break

            with contextlib.ExitStack() as st:
                moe = (layer == 1)
                nexp = NE if moe else 1
                nf = (DFF_E if moe else DFF_D) // 128
                wo_s = sbuf(st, "wo_s", [128, 4, D], BF16)
                wo_a = sbuf(st, "wo_a", [64, 8, D], BF16)
                wpg = sbuf(st, "wpg", [128, 8, D], BF16)
                wpp = sbuf(st, "wpp", [128, 2, D], BF16)
                xc = sbuf(st, "xc3", [128, 8, T], F32)
                ys = sbuf(st, "ys3", [128, 4, T], BF16)
                oc = sbuf(st, "oc3", [64, 8, T], F32)
                ya = sbuf(st, "ya3", [64, 8, T], BF16)
                sq = sbuf(st, "sq3", [128, 8, T], BF16)
                uT = sbuf(st, "uT3", [128, 8, T], BF16)
                lnt = sbuf(st, "lnt3", [128, T], F32)
                rstd = sbuf(st, "rstd3", [128, T], F32)
                hT = sbuf(st, "hT3", [128, nf, T], BF16)
                sg = [sbuf(st, f"sg3{i}", [128, T], F32) for i in range(2)]
                n_gu = 3 if moe else 5
                n_dp = 2 if moe else 3
                wgp = [sbuf(st, f"wgp{i}", [128, 8, 128], BF16) for i in range(n_gu)]
                wup = [sbuf(st, f"wup{i}", [128, 8, 128], BF16) for i in range(n_gu)]
                wdp = [sbuf(st, f"wdp{i}", [128, nf, 128], BF16) for i in range(n_dp)]
                pc_b = sbuf(st, "pc_b", [128, 2, T], BF16)
                tmp = [sbuf(st, f"tmp3{i}", [128, T], F32) for i in range(2)]
                pm = [psum(st, f"pm3{i}", [128, T], F32) for i in range(6)]
                pn = psum(st, "pn3", [128, T], F32)
                names = ["wo_s", "wo_a", "wpg", "wpp", "xc", "ys", "oc", "osq", "ya", "sq", "uT", "lnt", "rstd", "hT",
                         "sg0", "sg1", "wgp0", "wgp1", "wgp2", "wup0", "wup1", "wup2", "wdp0", "wdp1", "pc_f", "pc_b",
                         "gate0", "gate1", "tmp0", "tmp1", "pm0", "pm1", "pm2", "pm3", "pm4", "pm5", "pn", "x_dst",
                         "wr", "u2f", "lg", "cmb", "cbc", "dg", "mx"]
                B = {n: Buf(n) for n in names}
                for i_ in range(5):
                    B.setdefault(f"wgp{i_}", Buf(f"wgp{i_}"))
                    B.setdefault(f"wup{i_}", Buf(f"wup{i_}"))
                    B.setdefault(f"wdp{i_}", Buf(f"wdp{i_}"))
                if moe:
                    wr = sbuf(st, "wr3", [128, 8, NE], F32)
                    u2f = sbuf(st, "u2f3", [128, 8, T], F32)
                    lg = sbuf(st, "lg3", [128, 4, NE], F32)
                    mx = sbuf(st, "mx3", [128, 4, 8], F32)
                    cmb = sbuf(st, "cmb3", [128, 4, NE], F32)
                    cm2 = sbuf(st, "cm23", [128, 4, NE], F32)
                    gsm = sbuf(st, "gsm3", [128, 4, 4], F32)
                    dg = sbuf(st, "dg3", [128, NE, 128], F32)
                    cbc = sbuf(st, "cbc3", [128, NE, T], F32)
                pmi = [0]

                def next_pm3():
                    i = pmi[0] % 6
                    pmi[0] += 1
                    return pm[i], B[f"pm{i}"]

                load_cast(wo_s, wout_in[layer, 0:512, :], D, (), [B["wo_s"]])
                wo_av = wout_in[layer, 512:1024, :].rearrange("(h p) n -> p h n", p=64)
                for h in range(8):
                    dma("pool", wo_a[:, h, :], wo_av[:, h, :], (), [B["wo_a"]])
                load_cast(wpg, wpg_in[layer], D, (), [B["wpg"]])
                load_cast(wpp, wpp_in[layer], D, (), [B["wpp"]])
                if moe:
                    dma("sp", wr[:], wr_in[0].rearrange("(k p) n -> p k n", p=128), (), [B["wr"]])

                bsq = [Buf(f"sq{k}") for k in range(8)]
                bxc = [Buf(f"xc{k}") for k in range(8)]
                buT = [Buf(f"uT3_{k}") for k in range(8)]
                bu2f = [Buf(f"u2f{k}") for k in range(8)]

                def rmsnorm_full(gcol0, want_f32):
                    for k in range(8):
                        act(sq[:, k, :], xc[:, k, :], AF.Square, [bxc[k]], [bsq[k], B["sq"]])
                    for k in range(8):
                        mm(pn[:], ones_bf[:], sq[:, k, :], k == 0, k == 7, [b_const, bsq[k]], [B["pn"]])
                    act(lnt[:], pn[:], AF.Ln, [B["pn"]], [B["lnt"]], bias=EPS, scale=1.0 / D)
                    act(rstd[:], lnt[:], AF.Exp, [B["lnt"]], [B["rstd"]], scale=-0.5)
                    for k in range(8):
                        if want_f32:
                            stt(u2f[:, k, :], xc[:, k, :], PV(layer, gcol0 + k), rstd[:], ALU.mult, ALU.mult,
                                [bxc[k], b_pvec, B["rstd"]], [bu2f[k]])
                            cp("act", uT[:, k, :], u2f[:, k, :], [bu2f[k]], [buT[k]])
                        else:
                            stt(uT[:, k, :], xc[:, k, :], PV(layer, gcol0 + k), rstd[:], ALU.mult, ALU.mult,
                                [bxc[k], b_pvec, B["rstd"]], [buT[k]])

                piece = [0]
                dpiece = [0]

                def load_side(cc):
                    cc0 = cc * T
                    dma("sp", ys[:], yssd_d.rearrange("(k p) t -> p k t", p=128)[:, :, cc0:cc0 + T], (), [B["ys"]])
                    dma("sp", oc[:], o_d.rearrange("h p t -> p h t")[:, :, cc0:cc0 + T], (), [B["oc"]])

                def attn_norm():
                    for h in range(8):
                        act(sq[0:64, h, :], oc[:, h, :], AF.Square, [B["oc"]], [bsq[h], B["sq"]])
                    for h in range(8):
                        mm(pn[:], ones_bf[0:64, :], sq[0:64, h, :], h == 0, h == 7, [b_const, bsq[h]], [B["pn"]])
                    act(lnt[:], pn[:], AF.Ln, [B["pn"]], [B["lnt"]], bias=EPS, scale=1.0 / 512)
                    act(rstd[:], lnt[:], AF.Exp, [B["lnt"]], [B["rstd"]], scale=-0.5)
                    for h in range(8):
                        stt(ya[:, h, :], oc[:, h, :], PV(layer, 74 + h)[0:64, :], rstd[0:64, :], ALU.mult, ALU.mult,
                            [B["oc"], b_pvec, B["rstd"]], [B["ya"]])

                load_side(0)
                for c in range(NCH):
                    c0 = c * T
                    for k in range(8):
                        dma("sp", xc[:, k, :], xsv[:, k, c0:c0 + T], (), [bxc[k]])
                    dma("pool", pc_b[:], pT_in[layer].rearrange("(k p) t -> p k t", p=128)[:, :, c0:c0 + T], (), [B["pc_b"]])
                    if c == 0:
                        attn_norm()
                    for o in range(8):
                        pt, bp = next_pm3()
                        for j in range(4):
                            mm(pt[:], wo_s[:, j, o * 128:(o + 1) * 128], ys[:, j, :], j == 0, False, [B["wo_s"], B["ys"]], [bp])
                        for h in range(8):
                            mm(pt[:], wo_a[:, h, o * 128:(o + 1) * 128], ya[:, h, :], False, h == 7, [B["wo_a"], B["ya"]], [bp])
                        tt("dve", xc[:, o, :], xc[:, o, :], pt[:], ALU.add, [bxc[o], bp], [bxc[o]])
                    if c + 1 < NCH:
                        load_side(c + 1)
                    rmsnorm_full(8, moe)
                    def router_part1():
                        pt, bp = next_pm3()
                        for t4 in range(4):
                            for k in range(8):
                                mm(pt[:, t4 * 8:(t4 + 1) * 8], u2f[:, k, t4 * 128:(t4 + 1) * 128], wr[:, k, :], k == 0, k == 7,
                                   [bu2f[k], B["wr"]], [bp])
                        cp("dve", lg[:].rearrange("p a b -> p (a b)"), pt[:, 0:32], [bp], [B["lg"]])
                        for t4 in range(4):
                            P.op("dve", lambda e, t4=t4: e.max(out=mx[:, t4, :], in_=lg[:, t4, :]), [B["lg"]], [B["mx"]])
                        tt("dve", gsm[:, :, 0:1], mx[:, :, 1:2], mx[:, :, 0:1], ALU.subtract, [B["mx"]], [B["cmb"]])
                        act(gsm[:, :, 1:2], gsm[:, :, 0:1], AF.Exp, [B["cmb"]], [B["cmb"]])
                        ts("dve", gsm[:, :, 2:3], gsm[:, :, 1:2], 1.0, None, ALU.add, None, [B["cmb"]], [B["cmb"]])
                        P.op("dve", lambda e: e.reciprocal(out=gsm[:, :, 2:3], in_=gsm[:, :, 2:3]), [B["cmb"]], [B["cmb"]])
                        tt("dve", gsm[:, :, 3:4], gsm[:, :, 1:2], gsm[:, :, 2:3], ALU.mult, [B["cmb"]], [B["cmb"]])
                        for t4 in range(4):
                            ts("dve", cmb[:, t4, :], lg[:, t4, :], mx[:, t4, 0:1], gsm[:, t4, 2:3], ALU.is_equal, ALU.mult,
                               [B["lg"], B["mx"], B["cmb"]], [B["cmb"]])
                            ts("dve", cm2[:, t4, :], lg[:, t4, :], mx[:, t4, 1:2], gsm[:, t4, 3:4], ALU.is_equal, ALU.mult,
                               [B["lg"], B["mx"], B["cmb"]], [B["cmb"]])
                        tt("dve", cmb[:], cmb[:], cm2[:], ALU.add, [B["cmb"]], [B["cmb"]])
                    def router_part2():
                        for t4 in range(4):
                            tt("dve", dg[:], ident_f[:].unsqueeze(1).broadcast_to([128, NE, 128]),
                               cmb[:, t4, :].unsqueeze(2).broadcast_to([128, NE, 128]), ALU.mult,
                               [b_const, B["cmb"]], [B["dg"]])
                            for eh in range(2):
                                pt, bp = next_pm3()
                                mm(pt[:], ones_f[:], dg[:, eh * 4:(eh + 1) * 4, :].rearrange("p e t -> p (e t)"), True, True,
                                   [b_const, B["dg"]], [bp])
                                cp("act", cbc[:, eh * 4:(eh + 1) * 4, t4 * 128:(t4 + 1) * 128],
                                   pt[:].rearrange("p (e t) -> p e t", t=128), [bp], [B["cbc"]])
                    for e_ in range(nexp):
                        if moe:
                            wg_src, wu_src, wd_src = wge_b[e_], wue_b[e_], wde_b[e_]
                            kg, ku, kd = "ge", "ue", "de"
                        else:
                            wg_src, wu_src, wd_src = wgd_b[0], wud_b[0], wdd_b[0]
                            kg, ku, kd = "gd", "ud", "dd"
                        for f in range(nf):
                            pi = piece[0] % n_gu
                            piece[0] += 1
                            dma("sp", wgp[pi][:].rearrange("p k n -> p (k n)"), wg_src[f], [WB[(kg, e_, f)]], [B[f"wgp{pi}"]])
                            dma("sp", wup[pi][:].rearrange("p k n -> p (k n)"), wu_src[f], [WB[(ku, e_, f)]], [B[f"wup{pi}"]])
                            pg, bpg = next_pm3()
                            for k in range(8):
                                mm(pg[:], wgp[pi][:, k, :], uT[:, k, :], k == 0, k == 7, [B[f"wgp{pi}"], buT[k]], [bpg])
                            pu, bpu = next_pm3()
                            for k in range(8):
                                mm(pu[:], wup[pi][:, k, :], uT[:, k, :], k == 0, k == 7, [B[f"wup{pi}"], buT[k]], [bpu])
                            si = f % 2
                            act(sg[si][:], pg[:], AF.Silu, [bpg], [B[f"sg{si}"]])
                            tt("dve", hT[:, f, :], sg[si][:], pu[:], ALU.mult, [B[f"sg{si}"], bpu], [B["hT"]])
                            if c + 1 < NCH and ((moe and e_ == 1 and f == 2) or (not moe and f == 10)):
                                attn_norm()
                            if moe and e_ == 0 and f == 2:
                                router_part1()
                            if moe and e_ == 0 and f == 7:
                                router_part2()
                        for o in range(8):
                            di = o % 2
                            dpi = dpiece[0] % n_dp
                            dpiece[0] += 1
                            dma("sp", wdp[dpi][:].rearrange("p f n -> p (f n)"), wd_src[o], [WB[(kd, e_, o)]], [B[f"wdp{dpi}"]])
                            pt, bp = next_pm3()
                            for f in range(nf):
                                mm(pt[:], wdp[dpi][:, f, :], hT[:, f, :], f == 0, f == nf - 1, [B[f"wdp{dpi}"], B["hT"]], [bp])
                            if moe:
                                tt("dve", tmp[di][:], pt[:], cbc[:, e_, :], ALU.mult, [bp, B["cbc"]], [B[f"tmp{di}"]])
                                tt("dve", xc[:, o, :], xc[:, o, :], tmp[di][:], ALU.add, [bxc[o], B[f"tmp{di}"]], [bxc[o]])
                            else:
                                tt("dve", xc[:, o, :], xc[:, o, :], pt[:], ALU.add, [bxc[o], bp], [bxc[o]])
                    rmsnorm_full(16, False)
                    for o in range(8):
                        gi = o % 2
                        pt, bp = next_pm3()
                        for k in range(8):
                            mm(pt[:], wpg[:, k, o * 128:(o + 1) * 128], uT[:, k, :], k == 0, k == 7, [B["wpg"], buT[k]], [bp])
                        act(sg[gi][:], pt[:], AF.Sigmoid, [bp], [B[f"sg{gi}"]])
                        pt2, bp2 = next_pm3()
                        for k in range(2):
                            mm(pt2[:], wpp[:, k, o * 128:(o + 1) * 128], pc_b[:, k, :], k == 0, k == 1, [B["wpp"], B["pc_b"]], [bp2])
                        tt("dve", tmp[gi][:], sg[gi][:], pt2[:], ALU.mult, [B[f"sg{gi}"], bp2], [B[f"tmp{gi}"]])
                        tt("dve", tmp[gi][:], xc[:, o, :], tmp[gi][:], ALU.add, [bxc[o], B[f"tmp{gi}"]], [B[f"tmp{gi}"]])
                        dma("pool", xdv[:, o, c0:c0 + T], tmp[gi][:], [B[f"tmp{gi}"]], ())
                P.drain_dmas("sp")
                P.emit()
            if STOP_AFTER == (layer, "p3"):
                break
    return nc


def _prep_shared(inp):
    f32 = np.float32
    pvec = np.zeros((128, 2 * NPL), f32)
    wincat = np.zeros((2, D, NCAT), f32)
    for l in range(2):
        b = l * NPL
        pvec[:, b + 0:b + 8] = inp["norm1_g"][l].reshape(8, 128).T
        pvec[:, b + 8:b + 16] = inp["norm2_g"][l].reshape(8, 128).T
        pvec[:, b + 16:b + 24] = inp["ple_norm_g"][l].reshape(8, 128).T
        for tap in range(4):
            pvec[:, b + 24 + tap * 8:b + 32 + tap * 8] = inp["conv_w"][l, tap].reshape(8, 128).T
        pvec[:, b + 56:b + 64] = inp["conv_b"][l].reshape(8, 128).T
        for r0 in (0, 8, 32, 40):
            pvec[r0:r0 + 8, b + 64] = inp["dt_bias"][l]
            pvec[r0:r0 + 8, b + 65] = inp["a_log"][l]
        pvec[:, b + 66:b + 70] = np.repeat(inp["d_skip"][l], 64).reshape(4, 128).T
        pvec[:, b + 70:b + 74] = inp["ssd_norm_g"][l].reshape(4, 128).T
        pvec[0:64, b + 74:b + 82] = inp["attn_norm_g"][l].reshape(8, 64).T
        pvec[0:64, b + 82] = inp["q_norm_g"][l]
        pvec[64:128, b + 82] = inp["q_norm_g"][l]
        pvec[0:64, b + 83] = inp["k_norm_g"][l]
        pvec[64:128, b + 83] = inp["k_norm_g"][l]
        pvec[0:8, b + 84] = inp["fg_bias"][l]
        w = inp["w_in"][l]
        wincat[l, :, 0:1536] = w[:, 0:1536]
        for r0 in (0, 8, 32, 40):
            wincat[l, :, 1536 + r0:1544 + r0] = w[:, 1536:1544]
        wincat[l, :, 1584:2096] = w[:, 1544:2056]
        wincat[l, :, 2096:2608] = w[:, 2056:2568]
        wincat[l, :, 2608:3120] = w[:, 2568:3080]
        wincat[l, :, 3120:3128] = w[:, 3080:3088]
    c = np.ascontiguousarray

    def gu_layout(w):
        E, _, F = w.shape
        return c(w.reshape(E, 8, 128, F // 128, 128).transpose(0, 3, 2, 1, 4).reshape(E, F // 128, 128, 1024), dtype=f32)

    def d_layout(w):
        E, F, _ = w.shape
        return c(w.reshape(E, F // 128, 128, 8, 128).transpose(0, 3, 2, 1, 4).reshape(E, 8, 128, F), dtype=f32)

    return {
        "pvec": pvec, "wincat": wincat, "wout": c(inp["w_out"], dtype=f32),
        "wgd": gu_layout(inp["w_gate_dense"]), "wud": gu_layout(inp["w_up_dense"]),
        "wdd": d_layout(inp["w_down_dense"]), "wr": c(inp["w_router"], dtype=f32),
        "wge": gu_layout(inp["w_gate_exp"][0]), "wue": gu_layout(inp["w_up_exp"][0]),
        "wde": d_layout(inp["w_down_exp"][0]),
        "wpg": c(inp["w_ple_gate"], dtype=f32), "wpp": c(inp["w_ple_proj"], dtype=f32),
    }


def kernel(**inputs):
    inp = {k: np.asarray(v) for k, v in inputs.items()}
    shared = _prep_shared(inp)
    x = inp["x"].astype(np.float32, copy=False)
    p = inp["p"].astype(np.float32, copy=False)
    in_maps = []
    for b in range(8):
        m = dict(shared)
        m["xT"] = np.ascontiguousarray(x[b].T)
        m["pT"] = np.ascontiguousarray(p[:, b].transpose(0, 2, 1))
        in_maps.append(m)
    nc = build_program()
    res = run_bass_kernel_spmd(nc, in_maps, core_ids=list(range(8)))
    out = np.stack([np.ascontiguousarray(r["yT"].T) for r in res.results], axis=0)
    return out.astype(np.float32, copy=False)
```

```python
import contextlib
import numpy as np
import concourse.bass as bass
import concourse.mybir as mybir
from concourse.bass_utils import run_bass_kernel_spmd
from concourse.alu_op_type import AluOpType as ALU

AF = mybir.ActivationFunctionType
F32 = mybir.dt.float32
BF16 = mybir.dt.bfloat16

S = 4096
D = 1024
T = 512
NCH = S // T
NCAT = 3128
NPL = 88
EPS = 1e-6
DFF_D = 2816
DFF_E = 1408
NE = 8

SEM_WIN = 8192
NDS = 12

DEBUG = False
NO_PRECAST = False
STRICT_POOL = False
PRECAST_LIMIT = None
PE_WARM_DUMMY = True
PRECAST_STORE_Q = "pool"
STOP_AFTER = None


class Buf:
    __slots__ = ("name", "w", "r")

    def __init__(self, name=""):
        self.name = name
        self.w = None
        self.r = {}


class Prog:
    ENG = ("pe", "act", "dve", "pool", "sp")

    def __init__(self, nc, stack):
        self.nc = nc
        self.stack = stack
        self.q = {e: [] for e in self.ENG}
        self.cnt = {e: 0 for e in self.ENG}
        self.known = {e: {} for e in self.ENG}
        self.csem = {e: [] for e in self.ENG}
        self.dsem = {}
        self.dval = {}
        self.drr = {}
        for qn in ("sp", "pool", "act"):
            self.dsem[qn] = [stack.enter_context(nc.semaphore(f"d_{qn}_{i}")) for i in range(NDS)]
            self.dval[qn] = [0] * NDS
            self.drr[qn] = 0

    def _csem(self, eng, win):
        lst = self.csem[eng]
        while len(lst) <= win:
            lst.append(self.stack.enter_context(self.nc.semaphore(f"c_{eng}_{len(lst)}")))
        return lst[win]

    def _need(self, eng, waits, ev):
        key, val = ev
        if self.known[eng].get(key, 0) >= val:
            return
        self.known[eng][key] = val
        waits[key] = max(waits.get(key, 0), val)

    def _deps(self, eng, reads, writes, is_dma):
        waits = {}
        for b in reads:
            if b.w is not None:
                self._need(eng, waits, b.w[:2])
        for b in writes:
            if b.w is not None:
                k, v, we = b.w
                if is_dma or we != eng or k[0] == "d" or (STRICT_POOL and eng == "pool"):
                    self._need(eng, waits, (k, v))
            for k, (v, re) in b.r.items():
                if is_dma or re != eng or k[0] == "d" or (STRICT_POOL and eng == "pool"):
                    self._need(eng, waits, (k, v))
        return waits

    def _lower_waits(self, waits):
        out = []
        for key, val in waits.items():
            if key[0] == "c":
                win = (val - 1) // SEM_WIN
                out.append((self._csem(key[1], win), val - win * SEM_WIN))
            else:
                out.append((self.dsem[key[1]][key[2]], val))
        return out

    def _record(self, ev, eng, reads, writes):
        key, val = ev
        for b in reads:
            old = b.r.get(key)
            if old is None or old[0] < val:
                b.r[key] = (val, eng)
        for b in writes:
            b.w = (key, val, eng)
            b.r = {}

    def op(self, eng, fn, reads=(), writes=()):
        waits = self._deps(eng, reads, writes, False)
        self.cnt[eng] += 1
        idx = self.cnt[eng]
        win = (idx - 1) // SEM_WIN
        sem = self._csem(eng, win)
        self.q[eng].append((self._lower_waits(waits), fn, (sem, 1)))
        ev = (("c", eng), idx)
        self._record(ev, eng, reads, writes)
        return ev

    def dma(self, qn, fn, reads=(), writes=()):
        waits = self._deps(qn, reads, writes, True)
        slot = self.drr[qn]
        self.drr[qn] = (slot + 1) % NDS
        cur = self.dval[qn][slot]
        key = ("d", qn, slot)
        if cur > 0:
            self._need(qn, waits, (key, cur))
        self.dval[qn][slot] = cur + 16
        self.q[qn].append((self._lower_waits(waits), fn, (self.dsem[qn][slot], 16)))
        ev = (key, cur + 16)
        self._record(ev, qn, reads, writes)
        return ev

    def drain_dmas(self, eng="sp"):
        waits = {}
        for qn in ("sp", "pool", "act"):
            for s in range(NDS):
                if self.dval[qn][s] > 0:
                    self._need(eng, waits, (("d", qn, s), self.dval[qn][s]))
        self.q[eng].append((self._lower_waits(waits), None, None))

    def emit(self):
        nc = self.nc
        qs = self.q
        self.q = {e: [] for e in self.ENG}

        def run(engobj, lst):
            for waits, fn, inc in lst:
                for s, v in waits:
                    engobj.wait_ge(s, v)
                if fn is not None:
                    ins = fn(engobj)
                    ins.then_inc(inc[0], inc[1])

        with nc.Block() as block:
            @block.tensor
            def _(e):
                run(e, qs["pe"])

            @block.scalar
            def _(e):
                run(e, qs["act"])

            @block.vector
            def _(e):
                run(e, qs["dve"])

            @block.gpsimd
            def _(e):
                run(e, qs["pool"])

            @block.sync
            def _(e):
                run(e, qs["sp"])


def build_program():
    nc = bass.Bass("TRN2", target_bir_lowering=False)
    dr = lambda name, shape, dt, kind: nc.dram_tensor(name, shape, dt, kind=kind).ap()
    skind = "ExternalOutput" if DEBUG else "Internal"
    xT_in = dr("xT", [D, S], F32, "ExternalInput")
    pT_in = dr("pT", [2, 256, S], F32, "ExternalInput")
    pvec_in = dr("pvec", [128, 2 * NPL], F32, "ExternalInput")
    wincat_in = dr("wincat", [2, D, NCAT], F32, "ExternalInput")
    wout_in = dr("wout", [2, D, D], F32, "ExternalInput")
    NFD, NFE = DFF_D // 128, DFF_E // 128
    wgd_in = dr("wgd", [1, NFD, 128, 1024], F32, "ExternalInput")
    wud_in = dr("wud", [1, NFD, 128, 1024], F32, "ExternalInput")
    wdd_in = dr("wdd", [1, 8, 128, NFD * 128], F32, "ExternalInput")
    wr_in = dr("wr", [1, D, NE], F32, "ExternalInput")
    wge_in = dr("wge", [NE, NFE, 128, 1024], F32, "ExternalInput")
    wue_in = dr("wue", [NE, NFE, 128, 1024], F32, "ExternalInput")
    wde_in = dr("wde", [NE, 8, 128, NFE * 128], F32, "ExternalInput")
    wpg_in = dr("wpg", [2, D, D], F32, "ExternalInput")
    wpp_in = dr("wpp", [2, 256, D], F32, "ExternalInput")
    yT_out = dr("yT", [D, S], F32, "ExternalOutput")

    wgd_b = dr("wgd_b", [1, NFD, 128, 1024], BF16, "Internal")
    wud_b = dr("wud_b", [1, NFD, 128, 1024], BF16, "Internal")
    wdd_b = dr("wdd_b", [1, 8, 128, NFD * 128], BF16, "Internal")
    wge_b = dr("wge_b", [NE, NFE, 128, 1024], BF16, "Internal")
    wue_b = dr("wue_b", [NE, NFE, 128, 1024], BF16, "Internal")
    wde_b = dr("wde_b", [NE, 8, 128, NFE * 128], BF16, "Internal")
    qaug_d = dr("qaug_d", [8, 66, S], BF16, skind)
    kaug_d = dr("kaug_d", [8, 65, S], BF16, skind)
    v_d = dr("v_d", [8, 128, 32, 64], BF16, skind)
    yssd_d = dr("yssd_d", [512, S], BF16, skind)
    o_d = dr("o_d", [8, 64, S], F32, skind)
    xmid_d = dr("xmid_d", [D, S], F32, skind)

    with contextlib.ExitStack() as gst:
        P = Prog(nc, gst)

        uid = [0]

        def sbuf(st, name, shape, dt):
            uid[0] += 1
            return st.enter_context(nc.sbuf_tensor(f"s{uid[0]}_{name}", shape, dt))

        def psum(st, name, shape, dt):
            uid[0] += 1
            return st.enter_context(nc.psum_tensor(f"p{uid[0]}_{name}", shape, dt))

        def mm(out, lhsT, rhs, start, stop, reads, writes):
            P.op("pe", lambda e: e.matmul(out, lhsT=lhsT, rhs=rhs, start=start, stop=stop), reads, writes)

        def tr(out, in_, ident, reads, writes):
            P.op("pe", lambda e: e.transpose(out, in_, ident), reads, writes)

        def act(out, in_, func, reads, writes, bias=None, scale=None, eng="act"):
            kw = {}
            if bias is not None:
                kw["bias"] = bias
            if scale is not None:
                kw["scale"] = scale
            P.op(eng, lambda e: e.activation(out=out, in_=in_, func=func, **kw), reads, writes)

        def tt(eng, out, in0, in1, op, reads, writes):
            P.op(eng, lambda e: e.tensor_tensor(out=out, in0=in0, in1=in1, op=op), reads, writes)

        def ts(eng, out, in0, s1, s2, op0, op1, reads, writes):
            if op1 is None:
                P.op(eng, lambda e: e.tensor_scalar(out=out, in0=in0, scalar1=s1, scalar2=None, op0=op0), reads, writes)
            else:
                P.op(eng, lambda e: e.tensor_scalar(out=out, in0=in0, scalar1=s1, scalar2=s2, op0=op0, op1=op1), reads, writes)

        def stt(out, in0, scalar, in1, op0, op1, reads, writes):
            P.op("dve", lambda e: e.scalar_tensor_tensor(out=out, in0=in0, scalar=scalar, in1=in1, op0=op0, op1=op1), reads, writes)

        def cp(eng, out, in_, reads, writes):
            if eng == "act":
                P.op("act", lambda e: e.activation(out=out, in_=in_, func=AF.Copy), reads, writes)
            else:
                P.op(eng, lambda e: e.tensor_copy(out=out, in_=in_), reads, writes)

        def memset(eng, ap, val, writes):
            P.op(eng, lambda e: e.memset(ap, val), (), writes)

        def dma(qn, out, in_, reads, writes):
            P.dma(qn, lambda e: e.dma_start(out=out, in_=in_), reads, writes)

        def load_cast(dst3, src2, ncols, reads, writes):
            kc = dst3.shape[1]
            srcv = src2.rearrange("(k p) n -> p k n", p=128)
            for k in range(kc):
                c0 = 0
                while c0 < ncols:
                    c1 = min(ncols, c0 + 2048)
                    dma("pool", dst3[:, k, c0:c1], srcv[:, k, c0:c1], reads, writes)
                    c0 = c1

        ident_bf = sbuf(gst, "ident_bf", [128, 128], BF16)
        ident_f = sbuf(gst, "ident_f", [128, 128], F32)
        ones_bf = sbuf(gst, "ones_bf", [128, 128], BF16)
        ones_f = sbuf(gst, "ones_f", [128, 128], F32)
        bdones = sbuf(gst, "bdones", [128, 128], BF16)
        maskb = sbuf(gst, "maskb", [128, 4, T], BF16)
        ssdmask = sbuf(gst, "ssdmask", [128, 4, 128], BF16)
        delta = sbuf(gst, "delta", [48, 2, 4, 128], F32)
        delta_b = sbuf(gst, "delta_b", [48, 2, 4, 128], BF16)
        mhl = sbuf(gst, "mhl", [48, 2], F32)
        resetm = sbuf(gst, "resetm", [48, T], F32)
        pvec = sbuf(gst, "pvec", [128, 2 * NPL], F32)
        dvec = sbuf(gst, "dvec", [128, 8], F32)
        posF = sbuf(gst, "posF", [128, 32, 8], F32)
        st0 = contextlib.ExitStack()
        tmpf = sbuf(st0, "tmpf", [128, 4, T], F32)
        b_const = Buf("const")
        b_pvec = Buf("pvec")
        b_dvec = Buf("dvec")
        b_posF = Buf("posF")
        b_tmpf = Buf("tmpf")

        dma("sp", pvec[:], pvec_in, (), [b_pvec])
        memset("pool", ident_f[:], 1.0, [b_const])
        P.op("pool", lambda e: e.affine_select(out=ident_f[:], in_=ident_f[:], pattern=[[-1, 128]],
                                                compare_op=ALU.is_equal, fill=0.0, base=0, channel_multiplier=1),
             [b_const], [b_const])
        cp("pool", ident_bf[:], ident_f[:], [b_const], [b_const])
        memset("pool", ones_f[:], 1.0, [b_const])
        memset("pool", ones_bf[:], 1.0, [b_const])
        memset("pool", bdones[:], 0.0, [b_const])
        memset("pool", bdones[0:64, 0:64], 1.0, [b_const])
        memset("pool", bdones[64:128, 64:128], 1.0, [b_const])
        memset("pool", tmpf[:], 0.0, [b_tmpf])
        for k in range(4):
            P.op("pool", lambda e, k=k: e.affine_select(out=tmpf[:, k, :], in_=tmpf[:, k, :], pattern=[[1, T]],
                                                        compare_op=ALU.is_ge, fill=-30000.0, base=-128 * k,
                                                        channel_multiplier=-1),
                 [b_tmpf], [b_tmpf])
        cp("pool", maskb[:], tmpf[:], [b_tmpf], [b_const])
        P.op("pool", lambda e: e.affine_select(out=tmpf[:, 0, :].rearrange("p (j l) -> p j l", l=128),
                                               in_=tmpf[:, 0, :].rearrange("p (j l) -> p j l", l=128),
                                               pattern=[[0, 4], [1, 128]], compare_op=ALU.is_ge, fill=-30000.0,
                                               base=0, channel_multiplier=-1),
             [b_tmpf, b_const], [b_tmpf])
        memset("pool", tmpf[:, 1, :], 0.0, [b_tmpf])
        P.op("pool", lambda e: e.affine_select(out=tmpf[:, 1, :].rearrange("p (j l) -> p j l", l=128),
                                               in_=tmpf[:, 1, :].rearrange("p (j l) -> p j l", l=128),
                                               pattern=[[0, 4], [1, 128]], compare_op=ALU.is_ge, fill=-30000.0,
                                               base=0, channel_multiplier=-1),
             [b_tmpf], [b_tmpf])
        cp("pool", ssdmask[:].rearrange("p j l -> p (j l)"), tmpf[:, 1, :], [b_tmpf], [b_const])
        memset("pool", delta[:], 0.0, [b_const])
        for g in range(2):
            for base_p in (0, 32):
                for off in (0, 8):
                    P.op("pool", lambda e, g=g, bp=base_p, off=off: e.affine_select(
                        out=delta[bp:bp + 16, g, :, :], in_=delta[bp:bp + 16, g, :, :], pattern=[[-1, 4], [0, 128]],
                        compare_op=ALU.not_equal, fill=1.0, base=-4 * g - off, channel_multiplier=1),
                        [b_const], [b_const])
        cp("pool", delta_b[:], delta[:], [b_const], [b_const])
        memset("pool", mhl[:], 0.0, [b_const])
        for base_p in (0, 32):
            P.op("pool", lambda e, bp=base_p: e.affine_select(
                out=mhl[bp:bp + 16, 0:1], in_=mhl[bp:bp + 16, 0:1], pattern=[[0, 1]],
                compare_op=ALU.is_ge, fill=1.0, base=-8, channel_multiplier=1), [b_const], [b_const])
        ts("pool", mhl[:, 1:2], mhl[:, 0:1], -1.0, 1.0, ALU.mult, ALU.add, [b_const], [b_const])
        memset("pool", resetm[:], 1.0, [b_const])
        memset("pool", resetm[:].rearrange("p (c l) -> p c l", l=128)[:, :, 0:1], 0.0, [b_const])
        memset("pool", posF[:], 0.0, [b_posF])
        P.drain_dmas("sp")
        P.emit()
        st0.close()

        PV = lambda l, c: pvec[:, l * NPL + c: l * NPL + c + 1]

        WB = {}
        precast = []

        def add_precast(name, dst, src, ncols):
            WB[name] = Buf(name)
            c0_ = 0
            while c0_ < ncols:
                c1_ = min(ncols, c0_ + 1024)
                precast.append((WB[name], dst[:, c0_:c1_], src[:, c0_:c1_], c1_ - c0_))
                c0_ = c1_

        for f_ in range(NFD):
            add_precast(("gd", 0, f_), wgd_b[0, f_], wgd_in[0, f_], 1024)
            add_precast(("ud", 0, f_), wud_b[0, f_], wud_in[0, f_], 1024)
        for o_ in range(8):
            add_precast(("dd", 0, o_), wdd_b[0, o_], wdd_in[0, o_], NFD * 128)
        n_pre_dense = len(precast)
        for e_ in range(NE):
            for f_ in range(NFE):
                add_precast(("ge", e_, f_), wge_b[e_, f_], wge_in[e_, f_], 1024)
                add_precast(("ue", e_, f_), wue_b[e_, f_], wue_in[e_, f_], 1024)
            for o_ in range(8):
                add_precast(("de", e_, o_), wde_b[e_, o_], wde_in[e_, o_], NFE * 128)
        pre_i = [0]

        stg = sbuf(gst, "stg", [128, 3, 1024], BF16)
        b_stg = [Buf(f"stg{i}") for i in range(3)]

        def issue_precast(n, limit):
            if PRECAST_LIMIT is not None:
                limit = min(limit, PRECAST_LIMIT)
            n = min(n, limit - pre_i[0])
            while n > 0:
                g_ = min(3, n)
                items = precast[pre_i[0]:pre_i[0] + g_]
                pre_i[0] += g_
                n -= g_
                for i_, (b_, d_, s_, w_) in enumerate(items):
                    dma("pool", stg[:, i_, 0:w_], s_, (), [b_stg[i_]])
                for i_, (b_, d_, s_, w_) in enumerate(items):
                    dma(PRECAST_STORE_Q, d_, stg[:, i_, 0:w_], [b_stg[i_]], [b_])

        for layer in range(2):
            x_src = xT_in if layer == 0 else xmid_d
            x_dst = xmid_d if layer == 0 else yT_out
            xsv = x_src.rearrange("(k p) t -> p k t", p=128)
            xdv = x_dst.rearrange("(k p) t -> p k t", p=128)

            act(dvec[0:48, 0:1], PV(layer, 65)[0:48, :], AF.Exp, [b_pvec], [b_dvec])
            ts("dve", dvec[0:48, 0:1], dvec[0:48, 0:1], -1.0, None, ALU.mult, None, [b_dvec], [b_dvec])
            ts("dve", dvec[:, 1:2], PV(layer, 82), 0.125, None, ALU.mult, None, [b_pvec], [b_dvec])
            ts("dve", dvec[0:8, 2:3], PV(layer, 84)[0:8, :], -1.0, None, ALU.mult, None, [b_pvec], [b_dvec])

            with contextlib.ExitStack() as st:
                win = sbuf(st, "win", [128, 8, NCAT], BF16)
                xc = sbuf(st, "xc", [128, 8, T], F32)
                sq = sbuf(st, "sq", [128, 8, T], BF16)
                uT = sbuf(st, "uT", [128, 8, T], BF16)
                lnt = sbuf(st, "lnt", [128, T], F32)
                rstd = sbuf(st, "rstd", [128, T], F32)
                zs = sbuf(st, "zs", [128, 4, T], F32)
                xpre = sbuf(st, "xpre", [128, 8, T + 4], BF16)
                xact = sbuf(st, "xact", [128, 8, T], BF16)
                diag = sbuf(st, "diag", [128, 32, 128], BF16)
                xsB = [sbuf(st, f"xsB{i}", [128, 768], BF16) for i in range(2)]
                dt40 = sbuf(st, "dt40", [48, T], F32)
                adt = sbuf(st, "adt", [48, T], F32)
                acum = sbuf(st, "acum", [48, T], F32)
                lndt = sbuf(st, "lndt", [48, T], F32)
                lhsD = sbuf(st, "lhsD", [48, T], BF16)
                splh = sbuf(st, "splh", [48, T], BF16)
                spll = sbuf(st, "spll", [48, T], BF16)
                acomb = sbuf(st, "acomb", [48, T], BF16)
                rhsD = sbuf(st, "rhsD", [48, 2, 2, 4, 128], BF16)
                expD = [sbuf(st, f"expD{i}", [128, 4, 128], F32) for i in range(2)]
                Eb = [sbuf(st, f"Eb{i}", [128, 4, 128], F32) for i in range(2)]
                Wt = [sbuf(st, f"Wt{i}", [128, 4, 128], BF16) for i in range(2)]
                Cs = [sbuf(st, f"Cs{i}", [128, 4, 128], BF16) for i in range(2)]
                xdd = [sbuf(st, f"xdd{i}", [128, 4, 64], BF16) for i in range(2)]
                prev_f = sbuf(st, "prev_f", [128, 2, 4, 64], F32)
                prev_b = sbuf(st, "prev_b", [128, 2, 4, 64], BF16)
                ych = sbuf(st, "ych", [128, 4, T], F32)
                yout = sbuf(st, "yout", [128, 4, T], BF16)
                qkst = [sbuf(st, f"qkst{i}", [128, T], BF16) for i in range(4)]
                hsq = [sbuf(st, f"hsq{i}", [128, T], BF16) for i in range(2)]
                vsb = sbuf(st, "vsb", [128, 8, 4, 64], BF16)
                fE = sbuf(st, "fE", [8, T], F32)
                fsp = sbuf(st, "fsp", [8, T], F32)
                fcum = sbuf(st, "fcum", [8, T], F32)
                fneg = sbuf(st, "fneg", [8, T], BF16)
                fneg2 = sbuf(st, "fneg2", [8, T], BF16)
                fcar = sbuf(st, "fcar", [8, 1], F32)
                ones8 = sbuf(st, "ones8", [8, T], F32)
                pm = [psum(st, f"pm{i}", [128, T], F32) for i in range(2)]
                pn = psum(st, "pn", [128, T], F32)
                pD = psum(st, "pD", [128, 4, 128], F32)
                pAb = psum(st, "pAb", [128, 4, 128], F32)
                pGs = psum(st, "pGs", [128, T], F32)
                py = psum(st, "py", [128, 4, 128], F32)
                ptr = psum(st, "ptr", [128, 1024], BF16)
                B = {n: Buf(n) for n in ["win", "xc", "sq", "uT", "lnt", "rstd", "zs", "xpre", "xact", "diag",
                                         "dtE", "dt40", "adt", "acum", "lndt", "lhsD", "rhsD", "splh", "spll", "acomb", "prev_f", "prev_b",
                                         "ych", "ysq", "yout", "qsb", "ksb", "vsb", "fE", "fsp", "fcum", "fneg",
                                         "fcar", "fneg2", "pm0", "pm1", "pn", "pD", "pAb", "pG", "pG1", "pst", "py", "ptr",
                                         "xsB0", "xsB1", "expD0", "expD1", "Eb0", "Eb1", "Wt0", "Wt1", "Cs0", "Cs1",
                                         "xdd0", "xdd1", "hsq0", "hsq1", "qkst0", "qkst1", "qkst2", "qkst3", "rhsD0", "rhsD1",
                                         "qaug_d", "kaug_d", "v_d", "yssd_d"]}
                pmi = [0]
                deferred = [None]
                buT = [Buf(f"uT{k}") for k in range(8)]
                bsq1 = [Buf(f"sq1_{k}") for k in range(8)]

                def next_pm():
                    i = pmi[0] % 2
                    pmi[0] += 1
                    return pm[i], B[f"pm{i}"]

                load_cast(win, wincat_in[layer], NCAT, (), [B["win"]])
                for tap in range(4):
                    for o in range(8):
                        ts("dve", diag[:, tap * 8 + o, :], ident_f[:], PV(layer, 24 + tap * 8 + o), None, ALU.mult, None,
                           [b_const, b_pvec], [B["diag"]])
                memset("pool", xpre[:], 0.0, [B["xpre"]])
                memset("pool", prev_f[:], 0.0, [B["prev_f"]])
                memset("pool", prev_b[:], 0.0, [B["prev_b"]])
                memset("pool", fcar[:], 0.0, [B["fcar"]])
                memset("pool", ones8[:], 1.0, [b_const])
                memset("pool", lhsD[:], 0.0, [B["lhsD"]])
                memset("pool", lhsD[0:16, :], 1.0, [B["lhsD"]])
                memset("pool", rhsD[:], 0.0, [B["rhsD"]])
                for par in range(2):
                    for g in range(2):
                        cp("pool", rhsD[32:48, par, g, :, :], delta_b[32:48, g, :, :], [b_const], [B["rhsD"]])

                for c in range(NCH):
                    c0 = c * T
                    if layer == 0:
                        issue_precast((n_pre_dense + NCH - 1) // NCH, n_pre_dense)

                    dma("sp", xc[:], xsv[:, :, c0:c0 + T], (), [B["xc"]])
                    for k in range(8):
                        act(sq[:, k, :], xc[:, k, :], AF.Square, [B["xc"]], [bsq1[k]])
                    for k in range(8):
                        mm(pn[:], ones_bf[:], sq[:, k, :], k == 0, k == 7, [b_const, bsq1[k]], [B["pn"]])
                    act(lnt[:], pn[:], AF.Ln, [B["pn"]], [B["lnt"]], bias=EPS, scale=1.0 / D)
                    act(rstd[:], lnt[:], AF.Exp, [B["lnt"]], [B["rstd"]], scale=-0.5)
                    for k in range(8):
                        stt(uT[:, k, :], xc[:, k, :], PV(layer, k), rstd[:], ALU.mult, ALU.mult,
                            [B["xc"], b_pvec, B["rstd"]], [buT[k]])
                    for o in range(12):
                        pt, bp = next_pm()
                        for k in range(8):
                            mm(pt[:], win[:, k, o * 128:(o + 1) * 128], uT[:, k, :], k == 0, k == 7,
                               [B["win"], buT[k]], [bp])
                        if o < 4:
                            act(zs[:, o, :], pt[:], AF.Silu, [bp], [B["zs"]])
                        else:
                            cp("dve", xpre[:, o - 4, 3:3 + T], pt[:], [bp], [B["xpre"]])
                        if o == 5 and deferred[0] is not None:
                            deferred[0]()
                            deferred[0] = None
                    for o in range(8):
                        pt, bp = next_pm()
                        for tap in range(4):
                            mm(pt[:], diag[:, tap * 8 + o, :], xpre[:, o, tap:tap + T], tap == 0, tap == 3,
                               [B["diag"], B["xpre"]], [bp])
                        act(xact[:, o, :], pt[:], AF.Silu, [bp, b_pvec], [B["xact"]], bias=PV(layer, 56 + o))
                    cp("pool", xpre[:, :, 0:3], xpre[:, :, T:T + 3], [B["xpre"]], [B["xpre"]])
                    pt, bp = next_pm()
                    for k in range(8):
                        mm(pt[0:48, :], win[:, k, 1536:1584], uT[:, k, :], k == 0, k == 7, [B["win"], buT[k]], [bp])
                    act(adt[:], pt[0:48, :], AF.Exp, [bp, b_pvec], [B["adt"]], bias=PV(layer, 64)[0:48, :])
                    act(dt40[:], adt[:], AF.Ln, [B["adt"]], [B["dt40"]], bias=1.0)
                    act(lndt[:], dt40[:], AF.Ln, [B["dt40"]], [B["lndt"]])
                    ts("dve", adt[:], dt40[:], dvec[0:48, 0:1], None, ALU.mult, None, [B["dt40"], b_dvec], [B["adt"]])
                    P.op("dve", lambda e: e.tensor_tensor_scan(out=acum[:], data0=resetm[:], data1=adt[:], initial=0.0,
                                                               op0=ALU.mult, op1=ALU.add),
                         [b_const, B["adt"]], [B["acum"]])
                    tt("dve", lndt[32:48, :], lndt[32:48, :], acum[32:48, :], ALU.subtract,
                       [B["lndt"], B["acum"]], [B["lndt"]])
                    for (r0, src, bsrc, dstt, bdst) in ((0, acum, B["acum"], acomb, B["acomb"]),
                                                        (32, lndt, B["lndt"], lhsD, B["lhsD"])):
                        rs_ = slice(r0, r0 + 16)
                        cp("pool", splh[rs_, :], src[rs_, :], [bsrc], [B["splh"]])
                        tt("dve", dt40[rs_, :], src[rs_, :], splh[rs_, :], ALU.subtract, [bsrc, B["splh"], B["dt40"]], [B["dt40"]])
                        cp("pool", spll[rs_, :], dt40[rs_, :], [B["dt40"]], [B["spll"]])
                        ts("dve", dstt[rs_, :], splh[rs_, :], mhl[rs_, 0:1], None, ALU.mult, None, [B["splh"], b_const], [bdst])
                        stt(dstt[rs_, :], spll[rs_, :], mhl[rs_, 1:2], dstt[rs_, :], ALU.mult, ALU.add,
                            [B["spll"], b_const, bdst], [bdst])
                    qk_items = [(which, hp) for which in range(2) for hp in range(4)]
                    qk_pt = {}

                    qk_banks = [(pm[0][:], B["pm0"]), (pm[1][:], B["pm1"]),
                                (pD[:].rearrange("p j l -> p (j l)"), B["pD"]), (pAb[:].rearrange("p j l -> p (j l)"), B["pAb"])]

                    def qk_proj(idx):
                        which, hp = qk_items[idx]
                        col0 = (1584 if which == 0 else 2096) + hp * 128
                        pt, bp = qk_banks[idx % 4]
                        for k in range(8):
                            mm(pt[:], win[:, k, col0:col0 + 128], uT[:, k, :], k == 0, k == 7, [B["win"], buT[k]], [bp])
                        qk_pt[idx] = (pt, bp)

                    def qk_norm(idx):
                        which, hp = qk_items[idx]
                        pt, bp = qk_pt[idx]
                        i = idx % 2
                        qi = idx % 4
                        dst = qkst[qi]
                        bdst = B[f"qkst{qi}"]
                        gcol = dvec[:, 1:2] if which == 0 else PV(layer, 83)
                        act(hsq[i][:], pt[:], AF.Square, [bp], [B[f"hsq{i}"]])
                        mm(pn[:], bdones[:], hsq[i][:], True, True, [b_const, B[f"hsq{i}"]], [B["pn"]])
                        act(lnt[:], pn[:], AF.Ln, [B["pn"]], [B["lnt"]], bias=EPS, scale=1.0 / 64)
                        act(rstd[:], lnt[:], AF.Exp, [B["lnt"]], [B["rstd"]], scale=-0.5)
                        stt(dst[:], pt[:], gcol, rstd[:], ALU.mult, ALU.mult, [bp, b_dvec, b_pvec, B["rstd"]], [bdst])
                        ddst = qaug_d if which == 0 else kaug_d
                        for half in range(2):
                            dma("pool", ddst[2 * hp + half, 0:64, c0:c0 + T], dst[half * 64:(half + 1) * 64, :], [bdst], ())

                    qk_proj(0)
                    qk_proj(1)
                    for idx in range(8):
                        if idx + 2 < 8:
                            qk_proj(idx + 2)
                        qk_norm(idx)
                    pt, bp = next_pm()
                    for k in range(8):
                        mm(pt[0:8, :], win[:, k, 3120:3128], uT[:, k, :], k == 0, k == 7, [B["win"], buT[k]], [bp])
                    act(fE[:], pt[0:8, :], AF.Exp, [bp, b_dvec], [B["fE"]], bias=dvec[0:8, 2:3], scale=-1.0)
                    act(fsp[:], fE[:], AF.Ln, [B["fE"]], [B["fsp"]], bias=1.0)
                    P.op("dve", lambda e: e.tensor_tensor_scan(out=fcum[:], data0=ones8[:], data1=fsp[:], initial=fcar[:],
                                                               op0=ALU.mult, op1=ALU.add),
                         [b_const, B["fsp"], B["fcar"]], [B["fcum"]])
                    cp("dve", fcar[:], fcum[:, T - 1:T], [B["fcum"]], [B["fcar"]])
                    ts("dve", fneg[:], fcum[:], -1.0, None, ALU.mult, None, [B["fcum"]], [B["fneg"]])
                    dma("pool", qaug_d[:, 64, c0:c0 + T], fneg[:], [B["fneg"]], ())
                    stt(fE[:], fcum[:], -1.0, fneg[:], ALU.mult, ALU.subtract, [B["fcum"], B["fneg"]], [B["fE"]])
                    cp("dve", fneg2[:], fE[:], [B["fE"]], [B["fneg2"]])
                    dma("pool", qaug_d[:, 65, c0:c0 + T], fneg2[:], [B["fneg2"]], ())
                    pt, bp = next_pm()
                    for s4 in range(4):
                        tr(pt[:, s4 * 8:(s4 + 1) * 8], fcum[:, s4 * 128:(s4 + 1) * 128], ident_f[0:8, 0:8],
                           [B["fcum"], b_const], [bp])
                    cp("dve", posF[:, c * 4:(c + 1) * 4, :], pt[:, 0:32].rearrange("p (s h) -> p s h", h=8), [bp], [b_posF])
                    for t4 in range(4):
                        pt, bp = next_pm()
                        for k in range(8):
                            mm(pt[:], uT[:, k, t4 * 128:(t4 + 1) * 128], win[:, k, 2608:3120], k == 0, k == 7,
                               [B["win"], buT[k]], [bp])
                        cp("act", vsb[:, :, t4, :], pt[:].rearrange("p (h d) -> p h d", d=64), [bp], [B["vsb"]])
                    for h in range(8):
                        dma("pool", v_d[h, :, c * 4:(c + 1) * 4, :], vsb[:, h, :, :], [B["vsb"]], ())
                    pDg = [pD[:], pm[0][:].rearrange("p (j l) -> p j l", l=128)]
                    pAg = [pAb[:], pm[1][:].rearrange("p (j l) -> p j l", l=128)]
                    bpD = [B["pD"], B["pm0"]]
                    bpA = [B["pAb"], B["pm1"]]
                    pGg = [pGs[:, 0:128], pGs[:, 384:512]]
                    bpG = [B["pG"], B["pG"]]
                    B["pst"] = B["pG"]

                    def ssd_prep(sc):
                        cs = slice(sc * 128, (sc + 1) * 128)
                        xb = xsB[sc % 2]
                        bxb = B[f"xsB{sc % 2}"]
                        for o in range(6):
                            tr(ptr[:, o * 128:(o + 1) * 128], xact[:, o, cs], ident_bf[:], [B["xact"], b_const], [B["ptr"]])
                        cp("act", xb[:], ptr[:, 0:768], [B["ptr"]], [bxb])
                        par = sc % 2
                        brh = B[f"rhsD{par}"]
                        for g in range(2):
                            tt("dve", rhsD[0:16, par, g, :, :],
                               acomb[0:16, cs].unsqueeze(1).broadcast_to([16, 4, 128]),
                               delta[0:16, g, :, :], ALU.mult, [B["acomb"], b_const, B["rhsD"]], [brh])

                    def stageA(sc, g):
                        cs = slice(sc * 128, (sc + 1) * 128)
                        par = sc % 2
                        brh = B[f"rhsD{par}"]
                        mm(pGg[g], xact[:, 4 + g, cs], xact[:, 6 + g, cs], True, True, [B["xact"]], [bpG[g]])
                        mm(pDg[g].rearrange("p j l -> p (j l)"), lhsD[0:48, cs],
                           rhsD[0:48, par, g, :, :].rearrange("p j l -> p (j l)"), True, False,
                           [B["lhsD"], B["rhsD"], brh], [bpD[g]])
                        mm(pDg[g].rearrange("p j l -> p (j l)"), ident_bf[:], ssdmask[:].rearrange("p j l -> p (j l)"),
                           False, True, [b_const], [bpD[g]])
                        mm(pAg[g].rearrange("p j l -> p (j l)"), ones_bf[0:16, :],
                           rhsD[0:16, par, g, :, :].rearrange("p j l -> p (j l)"), True, True,
                           [b_const, brh], [bpA[g]])

                    def stageAct(sc, g):
                        cs = slice(sc * 128, (sc + 1) * 128)
                        i = g
                        act(expD[i][:], pDg[g], AF.Exp, [bpD[g]], [B[f"expD{i}"]])
                        act(Eb[i][:], pAg[g], AF.Exp, [bpA[g]], [B[f"Eb{i}"]])
                        tt("dve", Wt[i][:], expD[i][:], pGg[g].unsqueeze(1).broadcast_to([128, 4, 128]), ALU.mult,
                           [B[f"expD{i}"], bpG[g]], [B[f"Wt{i}"]])
                        tt("pool", Cs[i][:], Eb[i][:], xact[:, 6 + g, cs].unsqueeze(1).broadcast_to([128, 4, 128]), ALU.mult,
                           [B[f"Eb{i}"], B["xact"]], [B[f"Cs{i}"]])

                    def stageB(sc, g):
                        i = g
                        xb = xsB[sc % 2]
                        bxb = B[f"xsB{sc % 2}"]
                        for j in range(4):
                            h = 4 * g + j
                            hp, half = h // 2, h % 2
                            mm(py[half * 64:(half + 1) * 64, hp, :], xb[:, h * 64:(h + 1) * 64], Wt[i][:, j, :], True, False,
                               [bxb, B[f"Wt{i}"]], [B["py"]])
                            mm(py[half * 64:(half + 1) * 64, hp, :], prev_b[:, g, j, :], Cs[i][:, j, :], False, True,
                               [B["prev_b"], B[f"Cs{i}"]], [B["py"]])

                    def stageC(sc, g):
                        i = g
                        xb = xsB[sc % 2]
                        bxb = B[f"xsB{sc % 2}"]
                        tt("dve", xdd[i][:], xb[:, g * 256:(g + 1) * 256].rearrange("p (j d) -> p j d", d=64),
                           expD[i][:, :, 127:128].broadcast_to([128, 4, 64]), ALU.mult,
                           [bxb, B[f"expD{i}"]], [B[f"xdd{i}"]])
                        mm(pGs[:, 128:384], xb[:, 512 + g * 128:512 + (g + 1) * 128], xdd[i][:].rearrange("p j d -> p (j d)"),
                           True, True, [bxb, B[f"xdd{i}"]], [B["pst"]])
                        tt("dve", prev_f[:, g, :, :], prev_f[:, g, :, :], Eb[i][:, :, 127:128].broadcast_to([128, 4, 64]), ALU.mult,
                           [B["prev_f"], B[f"Eb{i}"]], [B["prev_f"]])
                        tt("dve", prev_f[:, g, :, :], prev_f[:, g, :, :], pGs[:, 128:384].rearrange("p (j d) -> p j d", d=64), ALU.add,
                           [B["prev_f"], B["pst"]], [B["prev_f"]])
                        cp("pool", prev_b[:, g, :, :], prev_f[:, g, :, :], [B["prev_f"]], [B["prev_b"]])

                    ssd_prep(0)
                    for sc in range(4):
                        cs = slice(sc * 128, (sc + 1) * 128)
                        for g in range(2):
                            stageA(sc, g)
                        for g in range(2):
                            stageAct(sc, g)
                        if sc + 1 < 4:
                            ssd_prep(sc + 1)
                        for g in range(2):
                            stageB(sc, g)
                        for g in range(2):
                            stageC(sc, g)
                        for hp in range(4):
                            stt(ych[:, hp, cs], xact[:, hp, cs], PV(layer, 66 + hp), py[:, hp, :], ALU.mult, ALU.add,
                                [B["xact"], b_pvec, B["py"]], [B["ych"]])
                    tt("pool", ych[:], ych[:], zs[:], ALU.mult, [B["ych"], B["zs"]], [B["ych"]])

                    def finish_ssd(c0=c0):
                        for k in range(4):
                            act(sq[:, k, :], ych[:, k, :], AF.Square, [B["ych"]], [bsq1[k]])
                        for k in range(4):
                            mm(pn[:], ones_bf[:], sq[:, k, :], k == 0, k == 3, [b_const, bsq1[k]], [B["pn"]])
                        act(lnt[:], pn[:], AF.Ln, [B["pn"]], [B["lnt"]], bias=EPS, scale=1.0 / 512)
                        act(rstd[:], lnt[:], AF.Exp, [B["lnt"]], [B["rstd"]], scale=-0.5)
                        for k in range(4):
                            stt(yout[:, k, :], ych[:, k, :], PV(layer, 70 + k), rstd[:], ALU.mult, ALU.mult,
                                [B["ych"], b_pvec, B["rstd"]], [B["yout"]])
                        dma("pool", yssd_d.rearrange("(k p) t -> p k t", p=128)[:, :, c0:c0 + T], yout[:], [B["yout"]], ())

                    deferred[0] = finish_ssd
                deferred[0]()
                P.drain_dmas("sp")
                P.emit()
            if STOP_AFTER == (layer, "p1"):
                break

            with contextlib.ExitStack() as st:
                kaug = [sbuf(st, f"kaug{i}", [66, S], BF16) for i in range(2)]
                vaug = [sbuf(st, f"vaug{i}", [128, 32, 128], BF16) for i in range(2)]
                qaug = [sbuf(st, f"qaug{i}", [66, T], BF16) for i in range(2)]
                pT = [sbuf(st, f"pT{i}", [128, T], BF16) for i in range(4)]
                rec = [sbuf(st, f"rec{i}", [64, T], F32) for i in range(2)]
                osb = [sbuf(st, f"osb{i}", [64, T], F32) for i in range(2)]
                ps_s = [psum(st, f"ps_s{i}", [128, T], F32) for i in range(4)]
                ps_o = [psum(st, f"ps_o{i}", [128, T], F32) for i in range(2)]
                ps_w = psum(st, "ps_w", [128, T], F32)
                b_psw = Buf("ps_w")
                B = {n: Buf(n) for n in ["kaug0", "kaug1", "vaug0", "vaug1", "qaug0", "qaug1", "pT0", "pT1", "pT2", "pT3", "ps_s3",
                                         "rec0", "rec1", "osb0", "osb1", "ps_s0", "ps_s1", "ps_s2", "ps_o0", "ps_o1", "o_d"]}
                for i in range(2):
                    memset("pool", vaug[i][:, :, 64:128], 1.0, [B[f"vaug{i}"]])
                    memset("pool", kaug[i][64:66, :], 1.0, [B[f"kaug{i}"]])
                blocks = []
                for h in range(8):
                    for c in range(NCH):
                        for j in range(4 * c + 4):
                            blocks.append((h, c, j))
                NB = len(blocks)
                dma("sp", kaug[0][0:64, :], kaug_d[0, 0:64, :], (), [B["kaug0"]])
                dma("sp", vaug[0][:, :, 0:64], v_d[0], (), [B["vaug0"]])

                def s_step(bi):
                    h, c, j = blocks[bi]
                    hb = h % 2
                    qb = (h * NCH + c) % 2
                    si = bi % 4
                    if j == 0:
                        dma("sp", qaug[qb][:], qaug_d[h, :, c * T:(c + 1) * T], (), [B[f"qaug{qb}"]])
                        issue_precast(3 if layer == 0 else 2, len(precast))
                    lo = max(0, j - 4 * c) * 128
                    mm(ps_s[si][:, lo:T], kaug[hb][0:66, j * 128:(j + 1) * 128], qaug[qb][0:66, lo:T], True, j < 4 * c,
                       [B[f"kaug{hb}"], B[f"qaug{qb}"]], [B[f"ps_s{si}"]])
                    if j >= 4 * c:
                        mm(ps_s[si][:, lo:T], ident_bf[:], maskb[:, j - 4 * c, lo:T], False, True, [b_const], [B[f"ps_s{si}"]])

                s_step(0)
                s_step(1)
                s_step(2)
                for bi in range(NB):
                    h, c, j = blocks[bi]
                    hb = h % 2
                    qb = (h * NCH + c) % 2
                    si = bi % 4
                    nj = 4 * c + 4
                    po, bpo = ps_o[qb], B[f"ps_o{qb}"]
                    if bi + 3 < NB:
                        s_step(bi + 3)
                    lo = max(0, j - 4 * c) * 128
                    act(pT[si][:, lo:T], ps_s[si][:, lo:T], AF.Exp, [B[f"ps_s{si}"], b_posF], [B[f"pT{si}"]], bias=posF[:, j, h:h + 1])
                    mm(po[:, lo:T], vaug[hb][:, j, :], pT[si][:, lo:T], j == 0, j == nj - 1, [B[f"vaug{hb}"], B[f"pT{si}"]], [bpo])
                    if PE_WARM_DUMMY:
                        mm(ps_w[:], ident_bf[:], maskb[:, 0, :], True, True, [b_const], [b_psw])
                    if j == nj - 1:
                        P.op("dve", lambda e, qb=qb, po=po: e.reciprocal(out=rec[qb][:], in_=po[64:128, :]), [bpo], [B[f"rec{qb}"]])
                        tt("dve", osb[qb][:], po[0:64, :], rec[qb][:], ALU.mult, [bpo, B[f"rec{qb}"]], [B[f"osb{qb}"]])
                        dma("pool", o_d[h, :, c * T:(c + 1) * T], osb[qb][:], [B[f"osb{qb}"]], ())
                        if c == 1 and h + 1 < 8:
                            nb_ = (h + 1) % 2
                            dma("sp", kaug[nb_][0:64, :], kaug_d[h + 1, 0:64, :], (), [B[f"kaug{nb_}"]])
                            dma("sp", vaug[nb_][:, :, 0:64], v_d[h + 1], (), [B[f"vaug{nb_}"]])
                P.drain_dmas("sp")
                P.emit()
            if STOP_AFTER == (layer, "p2"):
                break

            with contextlib.ExitStack() as st:
                moe = (layer == 1)
                nexp = NE if moe else 1
                nf = (DFF_E if moe else DFF_D) // 128
                wo_s = sbuf(st, "wo_s", [128, 4, D], BF16)
                wo_a = sbuf(st, "wo_a", [64, 8, D], BF16)
                wpg = sbuf(st, "wpg", [128, 8, D], BF16)
                wpp = sbuf(st, "wpp", [128, 2, D], BF16)
                xc = sbuf(st, "xc3", [128, 8, T], F32)
                ys = sbuf(st, "ys3", [128, 4, T], BF16)
                oc = sbuf(st, "oc3", [64, 8, T], F32)
                ya = sbuf(st, "ya3", [64, 8, T], BF16)
                sq = sbuf(st, "sq3", [128, 8, T], BF16)
                uT = sbuf(st, "uT3", [128, 8, T], BF16)
                lnt = sbuf(st, "lnt3", [128, T], F32)
                rstd = sbuf(st, "rstd3", [128, T], F32)
                hT = sbuf(st, "hT3", [128, nf, T], BF16)
                sg = [sbuf(st, f"sg3{i}", [128, T], F32) for i in range(2)]
                n_gu = 3 if moe else 5
                n_dp = 2 if moe else 3
                wgp = [sbuf(st, f"wgp{i}", [128, 8, 128], BF16) for i in range(n_gu)]
                wup = [sbuf(st, f"wup{i}", [128, 8, 128], BF16) for i in range(n_gu)]
                wdp = [sbuf(st, f"wdp{i}", [128, nf, 128], BF16) for i in range(n_dp)]
                pc_b = sbuf(st, "pc_b", [128, 2, T], BF16)
                tmp = [sbuf(st, f"tmp3{i}", [128, T], F32) for i in range(2)]
                pm = [psum(st, f"pm3{i}", [128, T], F32) for i in range(6)]
                pn = psum(st, "pn3", [128, T], F32)
                names = ["wo_s", "wo_a", "wpg", "wpp", "xc", "ys", "oc", "osq", "ya", "sq", "uT", "lnt", "rstd", "hT",
                         "sg0", "sg1", "wgp0", "wgp1", "wgp2", "wup0", "wup1", "wup2", "wdp0", "wdp1", "pc_f", "pc_b",
                         "gate0", "gate1", "tmp0", "tmp1", "pm0", "pm1", "pm2", "pm3", "pm4", "pm5", "pn", "x_dst",
                         "wr", "u2f", "lg", "cmb", "cbc", "dg", "mx"]
                B = {n: Buf(n) for n in names}
                for i_ in range(5):
                    B.setdefault(f"wgp{i_}", Buf(f"wgp{i_}"))
                    B.setdefault(f"wup{i_}", Buf(f"wup{i_}"))
                    B.setdefault(f"wdp{i_}", Buf(f"wdp{i_}"))
                if moe:
                    wr = sbuf(st, "wr3", [128, 8, NE], F32)
                    u2f = sbuf(st, "u2f3", [128, 8, T], F32)
                    lg = sbuf(st, "lg3", [128, 4, NE], F32)
                    mx = sbuf(st, "mx3", [128, 4, 8], F32)
                    cmb = sbuf(st, "cmb3", [128, 4, NE], F32)
                    cm2 = sbuf(st, "cm23", [128, 4, NE], F32)
                    gsm = sbuf(st, "gsm3", [128, 4, 4], F32)
                    dg = sbuf(st, "dg3", [128, NE, 128], F32)
                    cbc = sbuf(st, "cbc3", [128, NE, T], F32)
                pmi = [0]

                def next_pm3():
                    i = pmi[0] % 6
                    pmi[0] += 1
                    return pm[i], B[f"pm{i}"]

                load_cast(wo_s, wout_in[layer, 0:512, :], D, (), [B["wo_s"]])
                wo_av = wout_in[layer, 512:1024, :].rearrange("(h p) n -> p h n", p=64)
                for h in range(8):
                    dma("pool", wo_a[:, h, :], wo_av[:, h, :], (), [B["wo_a"]])
                load_cast(wpg, wpg_in[layer], D, (), [B["wpg"]])
                load_cast(wpp, wpp_in[layer], D, (), [B["wpp"]])
                if moe:
                    dma("sp", wr[:], wr_in[0].rearrange("(k p) n -> p k n", p=128), (), [B["wr"]])

                bsq = [Buf(f"sq{k}") for k in range(8)]
                bxc = [Buf(f"xc{k}") for k in range(8)]
                buT = [Buf(f"uT3_{k}") for k in range(8)]
                bu2f = [Buf(f"u2f{k}") for k in range(8)]

                def rmsnorm_full(gcol0, want_f32):
                    for k in range(8):
                        act(sq[:, k, :], xc[:, k, :], AF.Square, [bxc[k]], [bsq[k], B["sq"]])
                    for k in range(8):
                        mm(pn[:], ones_bf[:], sq[:, k, :], k == 0, k == 7, [b_const, bsq[k]], [B["pn"]])
                    act(lnt[:], pn[:], AF.Ln, [B["pn"]], [B["lnt"]], bias=EPS, scale=1.0 / D)
                    act(rstd[:], lnt[:], AF.Exp, [B["lnt"]], [B["rstd"]], scale=-0.5)
                    for k in range(8):
                        if want_f32:
                            stt(u2f[:, k, :], xc[:, k, :], PV(layer, gcol0 + k), rstd[:], ALU.mult, ALU.mult,
                                [bxc[k], b_pvec, B["rstd"]], [bu2f[k]])
                            cp("act", uT[:, k, :], u2f[:, k, :], [bu2f[k]], [buT[k]])
                        else:
                            stt(uT[:, k, :], xc[:, k, :], PV(layer, gcol0 + k), rstd[:], ALU.mult, ALU.mult,
                                [bxc[k], b_pvec, B["rstd"]], [buT[k]])

                piece = [0]
                dpiece = [0]

                def load_side(cc):
                    cc0 = cc * T
                    dma("sp", ys[:], yssd_d.rearrange("(k p) t -> p k t", p=128)[:, :, cc0:cc0 + T], (), [B["ys"]])
                    dma("sp", oc[:], o_d.rearrange("h p t -> p h t")[:, :, cc0:cc0 + T], (), [B["oc"]])

                def attn_norm():
                    for h in range(8):
                        act(sq[0:64, h, :], oc[:, h, :], AF.Square, [B["oc"]], [bsq[h], B["sq"]])
                    for h in range(8):
                        mm(pn[:], ones_bf[0:64, :], sq[0:64, h, :], h == 0, h == 7, [b_const, bsq[h]], [B["pn"]])
                    act(lnt[:], pn[:], AF.Ln, [B["pn"]], [B["lnt"]], bias=EPS, scale=1.0 / 512)
                    act(rstd[:], lnt[:], AF.Exp, [B["lnt"]], [B["rstd"]], scale=-0.5)
                    for h in range(8):
                        stt(ya[:, h, :], oc[:, h, :], PV(layer, 74 + h)[0:64, :], rstd[0:64, :], ALU.mult, ALU.mult,
                            [B["oc"], b_pvec, B["rstd"]], [B["ya"]])

                load_side(0)
                for c in range(NCH):
                    c0 = c * T
                    for k in range(8):
                        dma("sp", xc[:, k, :], xsv[:, k, c0:c0 + T], (), [bxc[k]])
                    dma("pool", pc_b[:], pT_in[layer].rearrange("(k p) t -> p k t", p=128)[:, :, c0:c0 + T], (), [B["pc_b"]])
                    if c == 0:
                        attn_norm()
                    for o in range(8):
                        pt, bp = next_pm3()
                        for j in range(4):
                            mm(pt[:], wo_s[:, j, o * 128:(o + 1) * 128], ys[:, j, :], j == 0, False, [B["wo_s"], B["ys"]], [bp])
                        for h in range(8):
                            mm(pt[:], wo_a[:, h, o * 128:(o + 1) * 128], ya[:, h, :], False, h == 7, [B["wo_a"], B["ya"]], [bp])
                        tt("dve", xc[:, o, :], xc[:, o, :], pt[:], ALU.add, [bxc[o], bp], [bxc[o]])
                    if c + 1 < NCH:
                        load_side(c + 1)
                    rmsnorm_full(8, moe)
                    def router_part1():
                        pt, bp = next_pm3()
                        for t4 in range(4):
                            for k in range(8):
                                mm(pt[:, t4 * 8:(t4 + 1) * 8], u2f[:, k, t4 * 128:(t4 + 1) * 128], wr[:, k, :], k == 0, k == 7,
                                   [bu2f[k], B["wr"]], [bp])
                        cp("dve", lg[:].rearrange("p a b -> p (a b)"), pt[:, 0:32], [bp], [B["lg"]])
                        for t4 in range(4):
                            P.op("dve", lambda e, t4=t4: e.max(out=mx[:, t4, :], in_=lg[:, t4, :]), [B["lg"]], [B["mx"]])
                        tt("dve", gsm[:, :, 0:1], mx[:, :, 1:2], mx[:, :, 0:1], ALU.subtract, [B["mx"]], [B["cmb"]])
                        act(gsm[:, :, 1:2], gsm[:, :, 0:1], AF.Exp, [B["cmb"]], [B["cmb"]])
                        ts("dve", gsm[:, :, 2:3], gsm[:, :, 1:2], 1.0, None, ALU.add, None, [B["cmb"]], [B["cmb"]])
                        P.op("dve", lambda e: e.reciprocal(out=gsm[:, :, 2:3], in_=gsm[:, :, 2:3]), [B["cmb"]], [B["cmb"]])
                        tt("dve", gsm[:, :, 3:4], gsm[:, :, 1:2], gsm[:, :, 2:3], ALU.mult, [B["cmb"]], [B["cmb"]])
                        for t4 in range(4):
                            ts("dve", cmb[:, t4, :], lg[:, t4, :], mx[:, t4, 0:1], gsm[:, t4, 2:3], ALU.is_equal, ALU.mult,
                               [B["lg"], B["mx"], B["cmb"]], [B["cmb"]])
                            ts("dve", cm2[:, t4, :], lg[:, t4, :], mx[:, t4, 1:2], gsm[:, t4, 3:4], ALU.is_equal, ALU.mult,
                               [B["lg"], B["mx"], B["cmb"]], [B["cmb"]])
                        tt("dve", cmb[:], cmb[:], cm2[:], ALU.add, [B["cmb"]], [B["cmb"]])
                    def router_part2():
                        for t4 in range(4):
                            tt("dve", dg[:], ident_f[:].unsqueeze(1).broadcast_to([128, NE, 128]),
                               cmb[:, t4, :].unsqueeze(2).broadcast_to([128, NE, 128]), ALU.mult,
                               [b_const, B["cmb"]], [B["dg"]])
                            for eh in range(2):
                                pt, bp = next_pm3()
                                mm(pt[:], ones_f[:], dg[:, eh * 4:(eh + 1) * 4, :].rearrange("p e t -> p (e t)"), True, True,
                                   [b_const, B["dg"]], [bp])
                                cp("act", cbc[:, eh * 4:(eh + 1) * 4, t4 * 128:(t4 + 1) * 128],
                                   pt[:].rearrange("p (e t) -> p e t", t=128), [bp], [B["cbc"]])
                    for e_ in range(nexp):
                        if moe:
                            wg_src, wu_src, wd_src = wge_b[e_], wue_b[e_], wde_b[e_]
                            kg, ku, kd = "ge", "ue", "de"
                        else:
                            wg_src, wu_src, wd_src = wgd_b[0], wud_b[0], wdd_b[0]
                            kg, ku, kd = "gd", "ud", "dd"
                        for f in range(nf):
                            pi = piece[0] % n_gu
                            piece[0] += 1
                            dma("sp", wgp[pi][:].rearrange("p k n -> p (k n)"), wg_src[f], [WB[(kg, e_, f)]], [B[f"wgp{pi}"]])
                            dma("sp", wup[pi][:].rearrange("p k n -> p (k n)"), wu_src[f], [WB[(ku, e_, f)]], [B[f"wup{pi}"]])
                            pg, bpg = next_pm3()
                            for k in range(8):
                                mm(pg[:], wgp[pi][:, k, :], uT[:, k, :], k == 0, k == 7, [B[f"wgp{pi}"], buT[k]], [bpg])
                            pu, bpu = next_pm3()
                            for k in range(8):
                                mm(pu[:], wup[pi][:, k, :], uT[:, k, :], k == 0, k == 7, [B[f"wup{pi}"], buT[k]], [bpu])
                            si = f % 2
                            act(sg[si][:], pg[:], AF.Silu, [bpg], [B[f"sg{si}"]])
                            tt("dve", hT[:, f, :], sg[si][:], pu[:], ALU.mult, [B[f"sg{si}"], bpu], [B["hT"]])
                            if c + 1 < NCH and ((moe and e_ == 1 and f == 2) or (not moe and f == 10)):
                                attn_norm()
                            if moe and e_ == 0 and f == 2:
                                router_part1()
                            if moe and e_ == 0 and f == 7:
                                router_part2()
                        for o in range(8):
                            di = o % 2
                            dpi = dpiece[0] % n_dp
                            dpiece[0] += 1
                            dma("sp", wdp[dpi][:].rearrange("p f n -> p (f n)"), wd_src[o], [WB[(kd, e_, o)]], [B[f"wdp{dpi}"]])
                            pt, bp = next_pm3()
                            for f in range(nf):
                                mm(pt[:], wdp[dpi][:, f, :], hT[:, f, :], f == 0, f == nf - 1, [B[f"wdp{dpi}"], B["hT"]], [bp])
                            if moe:
                                tt("dve", tmp[di][:], pt[:], cbc[:, e_, :], ALU.mult, [bp, B["cbc"]], [B[f"tmp{di}"]])
                                tt("dve", xc[:, o, :], xc[:, o, :], tmp[di][:], ALU.add, [bxc[o], B[f"tmp{di}"]], [bxc[o]])
                            else:
                                tt("dve", xc[:, o, :], xc[:, o, :], pt[:], ALU.add, [bxc[o], bp], [bxc[o]])
                    rmsnorm_full(16, False)
                    for o in range(8):
                        gi = o % 2
                        pt, bp = next_pm3()
                        for k in range(8):
                            mm(pt[:], wpg[:, k, o * 128:(o + 1) * 128], uT[:, k, :], k == 0, k == 7, [B["wpg"], buT[k]], [bp])
                        act(sg[gi][:], pt[:], AF.Sigmoid, [bp], [B[f"sg{gi}"]])
                        pt2, bp2 = next_pm3()
                        for k in range(2):
                            mm(pt2[:], wpp[:, k, o * 128:(o + 1) * 128], pc_b[:, k, :], k == 0, k == 1, [B["wpp"], B["pc_b"]], [bp2])
                        tt("dve", tmp[gi][:], sg[gi][:], pt2[:], ALU.mult, [B[f"sg{gi}"], bp2], [B[f"tmp{gi}"]])
                        tt("dve", tmp[gi][:], xc[:, o, :], tmp[gi][:], ALU.add, [bxc[o], B[f"tmp{gi}"]], [B[f"tmp{gi}"]])
                        dma("pool", xdv[:, o, c0:c0 + T], tmp[gi][:], [B[f"tmp{gi}"]], ())
                P.drain_dmas("sp")
                P.emit()
            if STOP_AFTER == (layer, "p3"):
                break
    return nc


def _prep_shared(inp):
    f32 = np.float32
    pvec = np.zeros((128, 2 * NPL), f32)
    wincat = np.zeros((2, D, NCAT), f32)
    for l in range(2):
        b = l * NPL
        pvec[:, b + 0:b + 8] = inp["norm1_g"][l].reshape(8, 128).T
        pvec[:, b + 8:b + 16] = inp["norm2_g"][l].reshape(8, 128).T
        pvec[:, b + 16:b + 24] = inp["ple_norm_g"][l].reshape(8, 128).T
        for tap in range(4):
            pvec[:, b + 24 + tap * 8:b + 32 + tap * 8] = inp["conv_w"][l, tap].reshape(8, 128).T
        pvec[:, b + 56:b + 64] = inp["conv_b"][l].reshape(8, 128).T
        for r0 in (0, 8, 32, 40):
            pvec[r0:r0 + 8, b + 64] = inp["dt_bias"][l]
            pvec[r0:r0 + 8, b + 65] = inp["a_log"][l]
        pvec[:, b + 66:b + 70] = np.repeat(inp["d_skip"][l], 64).reshape(4, 128).T
        pvec[:, b + 70:b + 74] = inp["ssd_norm_g"][l].reshape(4, 128).T
        pvec[0:64, b + 74:b + 82] = inp["attn_norm_g"][l].reshape(8, 64).T
        pvec[0:64, b + 82] = inp["q_norm_g"][l]
        pvec[64:128, b + 82] = inp["q_norm_g"][l]
        pvec[0:64, b + 83] = inp["k_norm_g"][l]
        pvec[64:128, b + 83] = inp["k_norm_g"][l]
        pvec[0:8, b + 84] = inp["fg_bias"][l]
        w = inp["w_in"][l]
        wincat[l, :, 0:1536] = w[:, 0:1536]
        for r0 in (0, 8, 32, 40):
            wincat[l, :, 1536 + r0:1544 + r0] = w[:, 1536:1544]
        wincat[l, :, 1584:2096] = w[:, 1544:2056]
        wincat[l, :, 2096:2608] = w[:, 2056:2568]
        wincat[l, :, 2608:3120] = w[:, 2568:3080]
        wincat[l, :, 3120:3128] = w[:, 3080:3088]
    c = np.ascontiguousarray

    def gu_layout(w):
        E, _, F = w.shape
        return c(w.reshape(E, 8, 128, F // 128, 128).transpose(0, 3, 2, 1, 4).reshape(E, F // 128, 128, 1024), dtype=f32)

    def d_layout(w):
        E, F, _ = w.shape
        return c(w.reshape(E, F // 128, 128, 8, 128).transpose(0, 3, 2, 1, 4).reshape(E, 8, 128, F), dtype=f32)

    return {
        "pvec": pvec, "wincat": wincat, "wout": c(inp["w_out"], dtype=f32),
        "wgd": gu_layout(inp["w_gate_dense"]), "wud": gu_layout(inp["w_up_dense"]),
        "wdd": d_layout(inp["w_down_dense"]), "wr": c(inp["w_router"], dtype=f32),
        "wge": gu_layout(inp["w_gate_exp"][0]), "wue": gu_layout(inp["w_up_exp"][0]),
        "wde": d_layout(inp["w_down_exp"][0]),
        "wpg": c(inp["w_ple_gate"], dtype=f32), "wpp": c(inp["w_ple_proj"], dtype=f32),
    }


def kernel(**inputs):
    inp = {k: np.asarray(v) for k, v in inputs.items()}
    shared = _prep_shared(inp)
    x = inp["x"].astype(np.float32, copy=False)
    p = inp["p"].astype(np.float32, copy=False)
    in_maps = []
    for b in range(8):
        m = dict(shared)
        m["xT"] = np.ascontiguousarray(x[b].T)
        m["pT"] = np.ascontiguousarray(p[:, b].transpose(0, 2, 1))
        in_maps.append(m)
    nc = build_program()
    res = run_bass_kernel_spmd(nc, in_maps, core_ids=list(range(8)))
    out = np.stack([np.ascontiguousarray(r["yT"].T) for r in res.results], axis=0)
    return out.astype(np.float32, copy=False)
```

```python
import contextlib
import numpy as np
import concourse.bass as bass
import concourse.mybir as mybir
from concourse.bass_utils import run_bass_kernel_spmd
from concourse.alu_op_type import AluOpType as ALU

AF = mybir.ActivationFunctionType
F32 = mybir.dt.float32
BF16 = mybir.dt.bfloat16

S = 4096
D = 1024
T = 512
NCH = S // T
NCAT = 3128
NPL = 88
EPS = 1e-6
DFF_D = 2816
DFF_E = 1408
NE = 8

SEM_WIN = 8192
NDS = 12

DEBUG = False
NO_PRECAST = False
STRICT_POOL = False
PRECAST_LIMIT = None
PE_WARM_DUMMY = True
PRECAST_STORE_Q = "pool"
STOP_AFTER = None


class Buf:
    __slots__ = ("name", "w", "r")

    def __init__(self, name=""):
        self.name = name
        self.w = None
        self.r = {}


class Prog:
    ENG = ("pe", "act", "dve", "pool", "sp")

    def __init__(self, nc, stack):
        self.nc = nc
        self.stack = stack
        self.q = {e: [] for e in self.ENG}
        self.cnt = {e: 0 for e in self.ENG}
        self.known = {e: {} for e in self.ENG}
        self.csem = {e: [] for e in self.ENG}
        self.dsem = {}
        self.dval = {}
        self.drr = {}
        for qn in ("sp", "pool", "act"):
            self.dsem[qn] = [stack.enter_context(nc.semaphore(f"d_{qn}_{i}")) for i in range(NDS)]
            self.dval[qn] = [0] * NDS
            self.drr[qn] = 0

    def _csem(self, eng, win):
        lst = self.csem[eng]
        while len(lst) <= win:
            lst.append(self.stack.enter_context(self.nc.semaphore(f"c_{eng}_{len(lst)}")))
        return lst[win]

    def _need(self, eng, waits, ev):
        key, val = ev
        if self.known[eng].get(key, 0) >= val:
            return
        self.known[eng][key] = val
        waits[key] = max(waits.get(key, 0), val)

    def _deps(self, eng, reads, writes, is_dma):
        waits = {}
        for b in reads:
            if b.w is not None:
                self._need(eng, waits, b.w[:2])
        for b in writes:
            if b.w is not None:
                k, v, we = b.w
                if is_dma or we != eng or k[0] == "d" or (STRICT_POOL and eng == "pool"):
                    self._need(eng, waits, (k, v))
            for k, (v, re) in b.r.items():
                if is_dma or re != eng or k[0] == "d" or (STRICT_POOL and eng == "pool"):
                    self._need(eng, waits, (k, v))
        return waits

    def _lower_waits(self, waits):
        out = []
        for key, val in waits.items():
            if key[0] == "c":
                win = (val - 1) // SEM_WIN
                out.append((self._csem(key[1], win), val - win * SEM_WIN))
            else:
                out.append((self.dsem[key[1]][key[2]], val))
        return out

    def _record(self, ev, eng, reads, writes):
        key, val = ev
        for b in reads:
            old = b.r.get(key)
            if old is None or old[0] < val:
                b.r[key] = (val, eng)
        for b in writes:
            b.w = (key, val, eng)
            b.r = {}

    def op(self, eng, fn, reads=(), writes=()):
        waits = self._deps(eng, reads, writes, False)
        self.cnt[eng] += 1
        idx = self.cnt[eng]
        win = (idx - 1) // SEM_WIN
        sem = self._csem(eng, win)
        self.q[eng].append((self._lower_waits(waits), fn, (sem, 1)))
        ev = (("c", eng), idx)
        self._record(ev, eng, reads, writes)
        return ev

    def dma(self, qn, fn, reads=(), writes=()):
        waits = self._deps(qn, reads, writes, True)
        slot = self.drr[qn]
        self.drr[qn] = (slot + 1) % NDS
        cur = self.dval[qn][slot]
        key = ("d", qn, slot)
        if cur > 0:
            self._need(qn, waits, (key, cur))
        self.dval[qn][slot] = cur + 16
        self.q[qn].append((self._lower_waits(waits), fn, (self.dsem[qn][slot], 16)))
        ev = (key, cur + 16)
        self._record(ev, qn, reads, writes)
        return ev

    def drain_dmas(self, eng="sp"):
        waits = {}
        for qn in ("sp", "pool", "act"):
            for s in range(NDS):
                if self.dval[qn][s] > 0:
                    self._need(eng, waits, (("d", qn, s), self.dval[qn][s]))
        self.q[eng].append((self._lower_waits(waits), None, None))

    def emit(self):
        nc = self.nc
        qs = self.q
        self.q = {e: [] for e in self.ENG}

        def run(engobj, lst):
            for waits, fn, inc in lst:
                for s, v in waits:
                    engobj.wait_ge(s, v)
                if fn is not None:
                    ins = fn(engobj)
                    ins.then_inc(inc[0], inc[1])

        with nc.Block() as block:
            @block.tensor
            def _(e):
                run(e, qs["pe"])

            @block.scalar
            def _(e):
                run(e, qs["act"])

            @block.vector
            def _(e):
                run(e, qs["dve"])

            @block.gpsimd
            def _(e):
                run(e, qs["pool"])

            @block.sync
            def _(e):
                run(e, qs["sp"])


def build_program():
    nc = bass.Bass("TRN2", target_bir_lowering=False)
    dr = lambda name, shape, dt, kind: nc.dram_tensor(name, shape, dt, kind=kind).ap()
    skind = "ExternalOutput" if DEBUG else "Internal"
    xT_in = dr("xT", [D, S], F32, "ExternalInput")
    pT_in = dr("pT", [2, 256, S], F32, "ExternalInput")
    pvec_in = dr("pvec", [128, 2 * NPL], F32, "ExternalInput")
    wincat_in = dr("wincat", [2, D, NCAT], F32, "ExternalInput")
    wout_in = dr("wout", [2, D, D], F32, "ExternalInput")
    NFD, NFE = DFF_D // 128, DFF_E // 128
    wgd_in = dr("wgd", [1, NFD, 128, 1024], F32, "ExternalInput")
    wud_in = dr("wud", [1, NFD, 128, 1024], F32, "ExternalInput")
    wdd_in = dr("wdd", [1, 8, 128, NFD * 128], F32, "ExternalInput")
    wr_in = dr("wr", [1, D, NE], F32, "ExternalInput")
    wge_in = dr("wge", [NE, NFE, 128, 1024], F32, "ExternalInput")
    wue_in = dr("wue", [NE, NFE, 128, 1024], F32, "ExternalInput")
    wde_in = dr("wde", [NE, 8, 128, NFE * 128], F32, "ExternalInput")
    wpg_in = dr("wpg", [2, D, D], F32, "ExternalInput")
    wpp_in = dr("wpp", [2, 256, D], F32, "ExternalInput")
    yT_out = dr("yT", [D, S], F32, "ExternalOutput")

    wgd_b = dr("wgd_b", [1, NFD, 128, 1024], BF16, "Internal")
    wud_b = dr("wud_b", [1, NFD, 128, 1024], BF16, "Internal")
    wdd_b = dr("wdd_b", [1, 8, 128, NFD * 128], BF16, "Internal")
    wge_b = dr("wge_b", [NE, NFE, 128, 1024], BF16, "Internal")
    wue_b = dr("wue_b", [NE, NFE, 128, 1024], BF16, "Internal")
    wde_b = dr("wde_b", [NE, 8, 128, NFE * 128], BF16, "Internal")
    qaug_d = dr("qaug_d", [8, 66, S], BF16, skind)
    kaug_d = dr("kaug_d", [8, 65, S], BF16, skind)
    v_d = dr("v_d", [8, 128, 32, 64], BF16, skind)
    yssd_d = dr("yssd_d", [512, S], BF16, skind)
    o_d = dr("o_d", [8, 64, S], F32, skind)
    xmid_d = dr("xmid_d", [D, S], F32, skind)

    with contextlib.ExitStack() as gst:
        P = Prog(nc, gst)

        uid = [0]

        def sbuf(st, name, shape, dt):
            uid[0] += 1
            return st.enter_context(nc.sbuf_tensor(f"s{uid[0]}_{name}", shape, dt))

        def psum(st, name, shape, dt):
            uid[0] += 1
            return st.enter_context(nc.psum_tensor(f"p{uid[0]}_{name}", shape, dt))

        def mm(out, lhsT, rhs, start, stop, reads, writes):
            P.op("pe", lambda e: e.matmul(out, lhsT=lhsT, rhs=rhs, start=start, stop=stop), reads, writes)

        def tr(out, in_, ident, reads, writes):
            P.op("pe", lambda e: e.transpose(out, in_, ident), reads, writes)

        def act(out, in_, func, reads, writes, bias=None, scale=None, eng="act"):
            kw = {}
            if bias is not None:
                kw["bias"] = bias
            if scale is not None:
                kw["scale"] = scale
            P.op(eng, lambda e: e.activation(out=out, in_=in_, func=func, **kw), reads, writes)

        def tt(eng, out, in0, in1, op, reads, writes):
            P.op(eng, lambda e: e.tensor_tensor(out=out, in0=in0, in1=in1, op=op), reads, writes)

        def ts(eng, out, in0, s1, s2, op0, op1, reads, writes):
            if op1 is None:
                P.op(eng, lambda e: e.tensor_scalar(out=out, in0=in0, scalar1=s1, scalar2=None, op0=op0), reads, writes)
            else:
                P.op(eng, lambda e: e.tensor_scalar(out=out, in0=in0, scalar1=s1, scalar2=s2, op0=op0, op1=op1), reads, writes)

        def stt(out, in0, scalar, in1, op0, op1, reads, writes):
            P.op("dve", lambda e: e.scalar_tensor_tensor(out=out, in0=in0, scalar=scalar, in1=in1, op0=op0, op1=op1), reads, writes)

        def cp(eng, out, in_, reads, writes):
            if eng == "act":
                P.op("act", lambda e: e.activation(out=out, in_=in_, func=AF.Copy), reads, writes)
            else:
                P.op(eng, lambda e: e.tensor_copy(out=out, in_=in_), reads, writes)

        def memset(eng, ap, val, writes):
            P.op(eng, lambda e: e.memset(ap, val), (), writes)

        def dma(qn, out, in_, reads, writes):
            P.dma(qn, lambda e: e.dma_start(out=out, in_=in_), reads, writes)

        def load_cast(dst3, src2, ncols, reads, writes):
            kc = dst3.shape[1]
            srcv = src2.rearrange("(k p) n -> p k n", p=128)
            for k in range(kc):
                c0 = 0
                while c0 < ncols:
                    c1 = min(ncols, c0 + 2048)
                    dma("pool", dst3[:, k, c0:c1], srcv[:, k, c0:c1], reads, writes)
                    c0 = c1

        ident_bf = sbuf(gst, "ident_bf", [128, 128], BF16)
        ident_f = sbuf(gst, "ident_f", [128, 128], F32)
        ones_bf = sbuf(gst, "ones_bf", [128, 128], BF16)
        ones_f = sbuf(gst, "ones_f", [128, 128], F32)
        bdones = sbuf(gst, "bdones", [128, 128], BF16)
        maskb = sbuf(gst, "maskb", [128, 4, T], BF16)
        ssdmask = sbuf(gst, "ssdmask", [128, 4, 128], BF16)
        delta = sbuf(gst, "delta", [48, 2, 4, 128], F32)
        delta_b = sbuf(gst, "delta_b", [48, 2, 4, 128], BF16)
        mhl = sbuf(gst, "mhl", [48, 2], F32)
        resetm = sbuf(gst, "resetm", [48, T], F32)
        pvec = sbuf(gst, "pvec", [128, 2 * NPL], F32)
        dvec = sbuf(gst, "dvec", [128, 8], F32)
        posF = sbuf(gst, "posF", [128, 32, 8], F32)
        st0 = contextlib.ExitStack()
        tmpf = sbuf(st0, "tmpf", [128, 4, T], F32)
        b_const = Buf("const")
        b_pvec = Buf("pvec")
        b_dvec = Buf("dvec")
        b_posF = Buf("posF")
        b_tmpf = Buf("tmpf")

        dma("sp", pvec[:], pvec_in, (), [b_pvec])
        memset("pool", ident_f[:], 1.0, [b_const])
        P.op("pool", lambda e: e.affine_select(out=ident_f[:], in_=ident_f[:], pattern=[[-1, 128]],
                                                compare_op=ALU.is_equal, fill=0.0, base=0, channel_multiplier=1),
             [b_const], [b_const])
        cp("pool", ident_bf[:], ident_f[:], [b_const], [b_const])
        memset("pool", ones_f[:], 1.0, [b_const])
        memset("pool", ones_bf[:], 1.0, [b_const])
        memset("pool", bdones[:], 0.0, [b_const])
        memset("pool", bdones[0:64, 0:64], 1.0, [b_const])
        memset("pool", bdones[64:128, 64:128], 1.0, [b_const])
        memset("pool", tmpf[:], 0.0, [b_tmpf])
        for k in range(4):
            P.op("pool", lambda e, k=k: e.affine_select(out=tmpf[:, k, :], in_=tmpf[:, k, :], pattern=[[1, T]],
                                                        compare_op=ALU.is_ge, fill=-30000.0, base=-128 * k,
                                                        channel_multiplier=-1),
                 [b_tmpf], [b_tmpf])
        cp("pool", maskb[:], tmpf[:], [b_tmpf], [b_const])
        P.op("pool", lambda e: e.affine_select(out=tmpf[:, 0, :].rearrange("p (j l) -> p j l", l=128),
                                               in_=tmpf[:, 0, :].rearrange("p (j l) -> p j l", l=128),
                                               pattern=[[0, 4], [1, 128]], compare_op=ALU.is_ge, fill=-30000.0,
                                               base=0, channel_multiplier=-1),
             [b_tmpf, b_const], [b_tmpf])
        memset("pool", tmpf[:, 1, :], 0.0, [b_tmpf])
        P.op("pool", lambda e: e.affine_select(out=tmpf[:, 1, :].rearrange("p (j l) -> p j l", l=128),
                                               in_=tmpf[:, 1, :].rearrange("p (j l) -> p j l", l=128),
                                               pattern=[[0, 4], [1, 128]], compare_op=ALU.is_ge, fill=-30000.0,
                                               base=0, channel_multiplier=-1),
             [b_tmpf], [b_tmpf])
        cp("pool", ssdmask[:].rearrange("p j l -> p (j l)"), tmpf[:, 1, :], [b_tmpf], [b_const])
        memset("pool", delta[:], 0.0, [b_const])
        for g in range(2):
            for base_p in (0, 32):
                for off in (0, 8):
                    P.op("pool", lambda e, g=g, bp=base_p, off=off: e.affine_select(
                        out=delta[bp:bp + 16, g, :, :], in_=delta[bp:bp + 16, g, :, :], pattern=[[-1, 4], [0, 128]],
                        compare_op=ALU.not_equal, fill=1.0, base=-4 * g - off, channel_multiplier=1),
                        [b_const], [b_const])
        cp("pool", delta_b[:], delta[:], [b_const], [b_const])
        memset("pool", mhl[:], 0.0, [b_const])
        for base_p in (0, 32):
            P.op("pool", lambda e, bp=base_p: e.affine_select(
                out=mhl[bp:bp + 16, 0:1], in_=mhl[bp:bp + 16, 0:1], pattern=[[0, 1]],
                compare_op=ALU.is_ge, fill=1.0, base=-8, channel_multiplier=1), [b_const], [b_const])
        ts("pool", mhl[:, 1:2], mhl[:, 0:1], -1.0, 1.0, ALU.mult, ALU.add, [b_const], [b_const])
        memset("pool", resetm[:], 1.0, [b_const])
        memset("pool", resetm[:].rearrange("p (c l) -> p c l", l=128)[:, :, 0:1], 0.0, [b_const])
        memset("pool", posF[:], 0.0, [b_posF])
        P.drain_dmas("sp")
        P.emit()
        st0.close()

        PV = lambda l, c: pvec[:, l * NPL + c: l * NPL + c + 1]

        WB = {}
        precast = []

        def add_precast(name, dst, src, ncols):
            WB[name] = Buf(name)
            c0_ = 0
            while c0_ < ncols:
                c1_ = min(ncols, c0_ + 1024)
                precast.append((WB[name], dst[:, c0_:c1_], src[:, c0_:c1_], c1_ - c0_))
                c0_ = c1_

        for f_ in range(NFD):
            add_precast(("gd", 0, f_), wgd_b[0, f_], wgd_in[0, f_], 1024)
            add_precast(("ud", 0, f_), wud_b[0, f_], wud_in[0, f_], 1024)
        for o_ in range(8):
            add_precast(("dd", 0, o_), wdd_b[0, o_], wdd_in[0, o_], NFD * 128)
        n_pre_dense = len(precast)
        for e_ in range(NE):
            for f_ in range(NFE):
                add_precast(("ge", e_, f_), wge_b[e_, f_], wge_in[e_, f_], 1024)
                add_precast(("ue", e_, f_), wue_b[e_, f_], wue_in[e_, f_], 1024)
            for o_ in range(8):
                add_precast(("de", e_, o_), wde_b[e_, o_], wde_in[e_, o_], NFE * 128)
        pre_i = [0]

        stg = sbuf(gst, "stg", [128, 3, 1024], BF16)
        b_stg = [Buf(f"stg{i}") for i in range(3)]

        def issue_precast(n, limit):
            if PRECAST_LIMIT is not None:
                limit = min(limit, PRECAST_LIMIT)
            n = min(n, limit - pre_i[0])
            while n > 0:
                g_ = min(3, n)
                items = precast[pre_i[0]:pre_i[0] + g_]
                pre_i[0] += g_
                n -= g_
                for i_, (b_, d_, s_, w_) in enumerate(items):
                    dma("pool", stg[:, i_, 0:w_], s_, (), [b_stg[i_]])
                for i_, (b_, d_, s_, w_) in enumerate(items):
                    dma(PRECAST_STORE_Q, d_, stg[:, i_, 0:w_], [b_stg[i_]], [b_])

        for layer in range(2):
            x_src = xT_in if layer == 0 else xmid_d
            x_dst = xmid_d if layer == 0 else yT_out
            xsv = x_src.rearrange("(k p) t -> p k t", p=128)
            xdv = x_dst.rearrange("(k p) t -> p k t", p=128)

            act(dvec[0:48, 0:1], PV(layer, 65)[0:48, :], AF.Exp, [b_pvec], [b_dvec])
            ts("dve", dvec[0:48, 0:1], dvec[0:48, 0:1], -1.0, None, ALU.mult, None, [b_dvec], [b_dvec])
            ts("dve", dvec[:, 1:2], PV(layer, 82), 0.125, None, ALU.mult, None, [b_pvec], [b_dvec])
            ts("dve", dvec[0:8, 2:3], PV(layer, 84)[0:8, :], -1.0, None, ALU.mult, None, [b_pvec], [b_dvec])

            with contextlib.ExitStack() as st:
                win = sbuf(st, "win", [128, 8, NCAT], BF16)
                xc = sbuf(st, "xc", [128, 8, T], F32)
                sq = sbuf(st, "sq", [128, 8, T], BF16)
                uT = sbuf(st, "uT", [128, 8, T], BF16)
                lnt = sbuf(st, "lnt", [128, T], F32)
                rstd = sbuf(st, "rstd", [128, T], F32)
                zs = sbuf(st, "zs", [128, 4, T], F32)
                xpre = sbuf(st, "xpre", [128, 8, T + 4], BF16)
                xact = sbuf(st, "xact", [128, 8, T], BF16)
                diag = sbuf(st, "diag", [128, 32, 128], BF16)
                xsB = [sbuf(st, f"xsB{i}", [128, 768], BF16) for i in range(2)]
                dt40 = sbuf(st, "dt40", [48, T], F32)
                adt = sbuf(st, "adt", [48, T], F32)
                acum = sbuf(st, "acum", [48, T], F32)
                lndt = sbuf(st, "lndt", [48, T], F32)
                lhsD = sbuf(st, "lhsD", [48, T], BF16)
                splh = sbuf(st, "splh", [48, T], BF16)
                spll = sbuf(st, "spll", [48, T], BF16)
                acomb = sbuf(st, "acomb", [48, T], BF16)
                rhsD = sbuf(st, "rhsD", [48, 2, 2, 4, 128], BF16)
                expD = [sbuf(st, f"expD{i}", [128, 4, 128], F32) for i in range(2)]
                Eb = [sbuf(st, f"Eb{i}", [128, 4, 128], F32) for i in range(2)]
                Wt = [sbuf(st, f"Wt{i}", [128, 4, 128], BF16) for i in range(2)]
                Cs = [sbuf(st, f"Cs{i}", [128, 4, 128], BF16) for i in range(2)]
                xdd = [sbuf(st, f"xdd{i}", [128, 4, 64], BF16) for i in range(2)]
                prev_f = sbuf(st, "prev_f", [128, 2, 4, 64], F32)
                prev_b = sbuf(st, "prev_b", [128, 2, 4, 64], BF16)
                ych = sbuf(st, "ych", [128, 4, T], F32)
                yout = sbuf(st, "yout", [128, 4, T], BF16)
                qkst = [sbuf(st, f"qkst{i}", [128, T], BF16) for i in range(4)]
                hsq = [sbuf(st, f"hsq{i}", [128, T], BF16) for i in range(2)]
                vsb = sbuf(st, "vsb", [128, 8, 4, 64], BF16)
                fE = sbuf(st, "fE", [8, T], F32)
                fsp = sbuf(st, "fsp", [8, T], F32)
                fcum = sbuf(st, "fcum", [8, T], F32)
                fneg = sbuf(st, "fneg", [8, T], BF16)
                fneg2 = sbuf(st, "fneg2", [8, T], BF16)
                fcar = sbuf(st, "fcar", [8, 1], F32)
                ones8 = sbuf(st, "ones8", [8, T], F32)
                pm = [psum(st, f"pm{i}", [128, T], F32) for i in range(2)]
                pn = psum(st, "pn", [128, T], F32)
                pD = psum(st, "pD", [128, 4, 128], F32)
                pAb = psum(st, "pAb", [128, 4, 128], F32)
                pGs = psum(st, "pGs", [128, T], F32)
                py = psum(st, "py", [128, 4, 128], F32)
                ptr = psum(st, "ptr", [128, 1024], BF16)
                B = {n: Buf(n) for n in ["win", "xc", "sq", "uT", "lnt", "rstd", "zs", "xpre", "xact", "diag",
                                         "dtE", "dt40", "adt", "acum", "lndt", "lhsD", "rhsD", "splh", "spll", "acomb", "prev_f", "prev_b",
                                         "ych", "ysq", "yout", "qsb", "ksb", "vsb", "fE", "fsp", "fcum", "fneg",
                                         "fcar", "fneg2", "pm0", "pm1", "pn", "pD", "pAb", "pG", "pG1", "pst", "py", "ptr",
                                         "xsB0", "xsB1", "expD0", "expD1", "Eb0", "Eb1", "Wt0", "Wt1", "Cs0", "Cs1",
                                         "xdd0", "xdd1", "hsq0", "hsq1", "qkst0", "qkst1", "qkst2", "qkst3", "rhsD0", "rhsD1",
                                         "qaug_d", "kaug_d", "v_d", "yssd_d"]}
                pmi = [0]
                deferred = [None]
                buT = [Buf(f"uT{k}") for k in range(8)]
                bsq1 = [Buf(f"sq1_{k}") for k in range(8)]

                def next_pm():
                    i = pmi[0] % 2
                    pmi[0] += 1
                    return pm[i], B[f"pm{i}"]

                load_cast(win, wincat_in[layer], NCAT, (), [B["win"]])
                for tap in range(4):
                    for o in range(8):
                        ts("dve", diag[:, tap * 8 + o, :], ident_f[:], PV(layer, 24 + tap * 8 + o), None, ALU.mult, None,
                           [b_const, b_pvec], [B["diag"]])
                memset("pool", xpre[:], 0.0, [B["xpre"]])
                memset("pool", prev_f[:], 0.0, [B["prev_f"]])
                memset("pool", prev_b[:], 0.0, [B["prev_b"]])
                memset("pool", fcar[:], 0.0, [B["fcar"]])
                memset("pool", ones8[:], 1.0, [b_const])
                memset("pool", lhsD[:], 0.0, [B["lhsD"]])
                memset("pool", lhsD[0:16, :], 1.0, [B["lhsD"]])
                memset("pool", rhsD[:], 0.0, [B["rhsD"]])
                for par in range(2):
                    for g in range(2):
                        cp("pool", rhsD[32:48, par, g, :, :], delta_b[32:48, g, :, :], [b_const], [B["rhsD"]])

                for c in range(NCH):
                    c0 = c * T
                    if layer == 0:
                        issue_precast((n_pre_dense + NCH - 1) // NCH, n_pre_dense)

                    dma("sp", xc[:], xsv[:, :, c0:c0 + T], (), [B["xc"]])
                    for k in range(8):
                        act(sq[:, k, :], xc[:, k, :], AF.Square, [B["xc"]], [bsq1[k]])
                    for k in range(8):
                        mm(pn[:], ones_bf[:], sq[:, k, :], k == 0, k == 7, [b_const, bsq1[k]], [B["pn"]])
                    act(lnt[:], pn[:], AF.Ln, [B["pn"]], [B["lnt"]], bias=EPS, scale=1.0 / D)
                    act(rstd[:], lnt[:], AF.Exp, [B["lnt"]], [B["rstd"]], scale=-0.5)
                    for k in range(8):
                        stt(uT[:, k, :], xc[:, k, :], PV(layer, k), rstd[:], ALU.mult, ALU.mult,
                            [B["xc"], b_pvec, B["rstd"]], [buT[k]])
                    for o in range(12):
                        pt, bp = next_pm()
                        for k in range(8):
                            mm(pt[:], win[:, k, o * 128:(o + 1) * 128], uT[:, k, :], k == 0, k == 7,
                               [B["win"], buT[k]], [bp])
                        if o < 4:
                            act(zs[:, o, :], pt[:], AF.Silu, [bp], [B["zs"]])
                        else:
                            cp("dve", xpre[:, o - 4, 3:3 + T], pt[:], [bp], [B["xpre"]])
                        if o == 5 and deferred[0] is not None:
                            deferred[0]()
                            deferred[0] = None
                    for o in range(8):
                        pt, bp = next_pm()
                        for tap in range(4):
                            mm(pt[:], diag[:, tap * 8 + o, :], xpre[:, o, tap:tap + T], tap == 0, tap == 3,
                               [B["diag"], B["xpre"]], [bp])
                        act(xact[:, o, :], pt[:], AF.Silu, [bp, b_pvec], [B["xact"]], bias=PV(layer, 56 + o))
                    cp("pool", xpre[:, :, 0:3], xpre[:, :, T:T + 3], [B["xpre"]], [B["xpre"]])
                    pt, bp = next_pm()
                    for k in range(8):
                        mm(pt[0:48, :], win[:, k, 1536:1584], uT[:, k, :], k == 0, k == 7, [B["win"], buT[k]], [bp])
                    act(adt[:], pt[0:48, :], AF.Exp, [bp, b_pvec], [B["adt"]], bias=PV(layer, 64)[0:48, :])
                    act(dt40[:], adt[:], AF.Ln, [B["adt"]], [B["dt40"]], bias=1.0)
                    act(lndt[:], dt40[:], AF.Ln, [B["dt40"]], [B["lndt"]])
                    ts("dve", adt[:], dt40[:], dvec[0:48, 0:1], None, ALU.mult, None, [B["dt40"], b_dvec], [B["adt"]])
                    P.op("dve", lambda e: e.tensor_tensor_scan(out=acum[:], data0=resetm[:], data1=adt[:], initial=0.0,
                                                               op0=ALU.mult, op1=ALU.add),
                         [b_const, B["adt"]], [B["acum"]])
                    tt("dve", lndt[32:48, :], lndt[32:48, :], acum[32:48, :], ALU.subtract,
                       [B["lndt"], B["acum"]], [B["lndt"]])
                    for (r0, src, bsrc, dstt, bdst) in ((0, acum, B["acum"], acomb, B["acomb"]),
                                                        (32, lndt, B["lndt"], lhsD, B["lhsD"])):
                        rs_ = slice(r0, r0 + 16)
                        cp("pool", splh[rs_, :], src[rs_, :], [bsrc], [B["splh"]])
                        tt("dve", dt40[rs_, :], src[rs_, :], splh[rs_, :], ALU.subtract, [bsrc, B["splh"], B["dt40"]], [B["dt40"]])
                        cp("pool", spll[rs_, :], dt40[rs_, :], [B["dt40"]], [B["spll"]])
                        ts("dve", dstt[rs_, :], splh[rs_, :], mhl[rs_, 0:1], None, ALU.mult, None, [B["splh"], b_const], [bdst])
                        stt(dstt[rs_, :], spll[rs_, :], mhl[rs_, 1:2], dstt[rs_, :], ALU.mult, ALU.add,
                            [B["spll"], b_const, bdst], [bdst])
                    qk_items = [(which, hp) for which in range(2) for hp in range(4)]
                    qk_pt = {}

                    qk_banks = [(pm[0][:], B["pm0"]), (pm[1][:], B["pm1"]),
                                (pD[:].rearrange("p j l -> p (j l)"), B["pD"]), (pAb[:].rearrange("p j l -> p (j l)"), B["pAb"])]

                    def qk_proj(idx):
                        which, hp = qk_items[idx]
                        col0 = (1584 if which == 0 else 2096) + hp * 128
                        pt, bp = qk_banks[idx % 4]
                        for k in range(8):
                            mm(pt[:], win[:, k, col0:col0 + 128], uT[:, k, :], k == 0, k == 7, [B["win"], buT[k]], [bp])
                        qk_pt[idx] = (pt, bp)

                    def qk_norm(idx):
                        which, hp = qk_items[idx]
                        pt, bp = qk_pt[idx]
                        i = idx % 2
                        qi = idx % 4
                        dst = qkst[qi]
                        bdst = B[f"qkst{qi}"]
                        gcol = dvec[:, 1:2] if which == 0 else PV(layer, 83)
                        act(hsq[i][:], pt[:], AF.Square, [bp], [B[f"hsq{i}"]])
                        mm(pn[:], bdones[:], hsq[i][:], True, True, [b_const, B[f"hsq{i}"]], [B["pn"]])
                        act(lnt[:], pn[:], AF.Ln, [B["pn"]], [B["lnt"]], bias=EPS, scale=1.0 / 64)
                        act(rstd[:], lnt[:], AF.Exp, [B["lnt"]], [B["rstd"]], scale=-0.5)
                        stt(dst[:], pt[:], gcol, rstd[:], ALU.mult, ALU.mult, [bp, b_dvec, b_pvec, B["rstd"]], [bdst])
                        ddst = qaug_d if which == 0 else kaug_d
                        for half in range(2):
                            dma("pool", ddst[2 * hp + half, 0:64, c0:c0 + T], dst[half * 64:(half + 1) * 64, :], [bdst], ())

                    qk_proj(0)
                    qk_proj(1)
                    for idx in range(8):
                        if idx + 2 < 8:
                            qk_proj(idx + 2)
                        qk_norm(idx)
                    pt, bp = next_pm()
                    for k in range(8):
                        mm(pt[0:8, :], win[:, k, 3120:3128], uT[:, k, :], k == 0, k == 7, [B["win"], buT[k]], [bp])
                    act(fE[:], pt[0:8, :], AF.Exp, [bp, b_dvec], [B["fE"]], bias=dvec[0:8, 2:3], scale=-1.0)
                    act(fsp[:], fE[:], AF.Ln, [B["fE"]], [B["fsp"]], bias=1.0)
                    P.op("dve", lambda e: e.tensor_tensor_scan(out=fcum[:], data0=ones8[:], data1=fsp[:], initial=fcar[:],
                                                               op0=ALU.mult, op1=ALU.add),
                         [b_const, B["fsp"], B["fcar"]], [B["fcum"]])
                    cp("dve", fcar[:], fcum[:, T - 1:T], [B["fcum"]], [B["fcar"]])
                    ts("dve", fneg[:], fcum[:], -1.0, None, ALU.mult, None, [B["fcum"]], [B["fneg"]])
                    dma("pool", qaug_d[:, 64, c0:c0 + T], fneg[:], [B["fneg"]], ())
                    stt(fE[:], fcum[:], -1.0, fneg[:], ALU.mult, ALU.subtract, [B["fcum"], B["fneg"]], [B["fE"]])
                    cp("dve", fneg2[:], fE[:], [B["fE"]], [B["fneg2"]])
                    dma("pool", qaug_d[:, 65, c0:c0 + T], fneg2[:], [B["fneg2"]], ())
                    pt, bp = next_pm()
                    for s4 in range(4):
                        tr(pt[:, s4 * 8:(s4 + 1) * 8], fcum[:, s4 * 128:(s4 + 1) * 128], ident_f[0:8, 0:8],
                           [B["fcum"], b_const], [bp])
                    cp("dve", posF[:, c * 4:(c + 1) * 4, :], pt[:, 0:32].rearrange("p (s h) -> p s h", h=8), [bp], [b_posF])
                    for t4 in range(4):
                        pt, bp = next_pm()
                        for k in range(8):
                            mm(pt[:], uT[:, k, t4 * 128:(t4 + 1) * 128], win[:, k, 2608:3120], k == 0, k == 7,
                               [B["win"], buT[k]], [bp])
                        cp("act", vsb[:, :, t4, :], pt[:].rearrange("p (h d) -> p h d", d=64), [bp], [B["vsb"]])
                    for h in range(8):
                        dma("pool", v_d[h, :, c * 4:(c + 1) * 4, :], vsb[:, h, :, :], [B["vsb"]], ())
                    pDg = [pD[:], pm[0][:].rearrange("p (j l) -> p j l", l=128)]
                    pAg = [pAb[:], pm[1][:].rearrange("p (j l) -> p j l", l=128)]
                    bpD = [B["pD"], B["pm0"]]
                    bpA = [B["pAb"], B["pm1"]]
                    pGg = [pGs[:, 0:128], pGs[:, 384:512]]
                    bpG = [B["pG"], B["pG"]]
                    B["pst"] = B["pG"]

                    def ssd_prep(sc):
                        cs = slice(sc * 128, (sc + 1) * 128)
                        xb = xsB[sc % 2]
                        bxb = B[f"xsB{sc % 2}"]
                        for o in range(6):
                            tr(ptr[:, o * 128:(o + 1) * 128], xact[:, o, cs], ident_bf[:], [B["xact"], b_const], [B["ptr"]])
                        cp("act", xb[:], ptr[:, 0:768], [B["ptr"]], [bxb])
                        par = sc % 2
                        brh = B[f"rhsD{par}"]
                        for g in range(2):
                            tt("dve", rhsD[0:16, par, g, :, :],
                               acomb[0:16, cs].unsqueeze(1).broadcast_to([16, 4, 128]),
                               delta[0:16, g, :, :], ALU.mult, [B["acomb"], b_const, B["rhsD"]], [brh])

                    def stageA(sc, g):
                        cs = slice(sc * 128, (sc + 1) * 128)
                        par = sc % 2
                        brh = B[f"rhsD{par}"]
                        mm(pGg[g], xact[:, 4 + g, cs], xact[:, 6 + g, cs], True, True, [B["xact"]], [bpG[g]])
                        mm(pDg[g].rearrange("p j l -> p (j l)"), lhsD[0:48, cs],
                           rhsD[0:48, par, g, :, :].rearrange("p j l -> p (j l)"), True, False,
                           [B["lhsD"], B["rhsD"], brh], [bpD[g]])
                        mm(pDg[g].rearrange("p j l -> p (j l)"), ident_bf[:], ssdmask[:].rearrange("p j l -> p (j l)"),
                           False, True, [b_const], [bpD[g]])
                        mm(pAg[g].rearrange("p j l -> p (j l)"), ones_bf[0:16, :],
                           rhsD[0:16, par, g, :, :].rearrange("p j l -> p (j l)"), True, True,
                           [b_const, brh], [bpA[g]])

                    def stageAct(sc, g):
                        cs = slice(sc * 128, (sc + 1) * 128)
                        i = g
                        act(expD[i][:], pDg[g], AF.Exp, [bpD[g]], [B[f"expD{i}"]])
                        act(Eb[i][:], pAg[g], AF.Exp, [bpA[g]], [B[f"Eb{i}"]])
                        tt("dve", Wt[i][:], expD[i][:], pGg[g].unsqueeze(1).broadcast_to([128, 4, 128]), ALU.mult,
                           [B[f"expD{i}"], bpG[g]], [B[f"Wt{i}"]])
                        tt("pool", Cs[i][:], Eb[i][:], xact[:, 6 + g, cs].unsqueeze(1).broadcast_to([128, 4, 128]), ALU.mult,
                           [B[f"Eb{i}"], B["xact"]], [B[f"Cs{i}"]])

                    def stageB(sc, g):
                        i = g
                        xb = xsB[sc % 2]
                        bxb = B[f"xsB{sc % 2}"]
                        for j in range(4):
                            h = 4 * g + j
                            hp, half = h // 2, h % 2
                            mm(py[half * 64:(half + 1) * 64, hp, :], xb[:, h * 64:(h + 1) * 64], Wt[i][:, j, :], True, False,
                               [bxb, B[f"Wt{i}"]], [B["py"]])
                            mm(py[half * 64:(half + 1) * 64, hp, :], prev_b[:, g, j, :], Cs[i][:, j, :], False, True,
                               [B["prev_b"], B[f"Cs{i}"]], [B["py"]])

                    def stageC(sc, g):
                        i = g
                        xb = xsB[sc % 2]
                        bxb = B[f"xsB{sc % 2}"]
                        tt("dve", xdd[i][:], xb[:, g * 256:(g + 1) * 256].rearrange("p (j d) -> p j d", d=64),
                           expD[i][:, :, 127:128].broadcast_to([128, 4, 64]), ALU.mult,
                           [bxb, B[f"expD{i}"]], [B[f"xdd{i}"]])
                        mm(pGs[:, 128:384], xb[:, 512 + g * 128:512 + (g + 1) * 128], xdd[i][:].rearrange("p j d -> p (j d)"),
                           True, True, [bxb, B[f"xdd{i}"]], [B["pst"]])
                        tt("dve", prev_f[:, g, :, :], prev_f[:, g, :, :], Eb[i][:, :, 127:128].broadcast_to([128, 4, 64]), ALU.mult,
                           [B["prev_f"], B[f"Eb{i}"]], [B["prev_f"]])
                        tt("dve", prev_f[:, g, :, :], prev_f[:, g, :, :], pGs[:, 128:384].rearrange("p (j d) -> p j d", d=64), ALU.add,
                           [B["prev_f"], B["pst"]], [B["prev_f"]])
                        cp("pool", prev_b[:, g, :, :], prev_f[:, g, :, :], [B["prev_f"]], [B["prev_b"]])

                    ssd_prep(0)
                    for sc in range(4):
                        cs = slice(sc * 128, (sc + 1) * 128)
                        for g in range(2):
                            stageA(sc, g)
                        for g in range(2):
                            stageAct(sc, g)
                        if sc + 1 < 4:
                            ssd_prep(sc + 1)
                        for g in range(2):
                            stageB(sc, g)
                        for g in range(2):
                            stageC(sc, g)
                        for hp in range(4):
                            stt(ych[:, hp, cs], xact[:, hp, cs], PV(layer, 66 + hp), py[:, hp, :], ALU.mult, ALU.add,
                                [B["xact"], b_pvec, B["py"]], [B["ych"]])
                    tt("pool", ych[:], ych[:], zs[:], ALU.mult, [B["ych"], B["zs"]], [B["ych"]])

                    def finish_ssd(c0=c0):
                        for k in range(4):
                            act(sq[:, k, :], ych[:, k, :], AF.Square, [B["ych"]], [bsq1[k]])
                        for k in range(4):
                            mm(pn[:], ones_bf[:], sq[:, k, :], k == 0, k == 3, [b_const, bsq1[k]], [B["pn"]])
                        act(lnt[:], pn[:], AF.Ln, [B["pn"]], [B["lnt"]], bias=EPS, scale=1.0 / 512)
                        act(rstd[:], lnt[:], AF.Exp, [B["lnt"]], [B["rstd"]], scale=-0.5)
                        for k in range(4):
                            stt(yout[:, k, :], ych[:, k, :], PV(layer, 70 + k), rstd[:], ALU.mult, ALU.mult,
                                [B["ych"], b_pvec, B["rstd"]], [B["yout"]])
                        dma("pool", yssd_d.rearrange("(k p) t -> p k t", p=128)[:, :, c0:c0 + T], yout[:], [B["yout"]], ())

                    deferred[0] = finish_ssd
                deferred[0]()
                P.drain_dmas("sp")
                P.emit()
            if STOP_AFTER == (layer, "p1"):
                break

            with contextlib.ExitStack() as st:
                kaug = [sbuf(st, f"kaug{i}", [66, S], BF16) for i in range(2)]
                vaug = [sbuf(st, f"vaug{i}", [128, 32, 128], BF16) for i in range(2)]
                qaug = [sbuf(st, f"qaug{i}", [66, T], BF16) for i in range(2)]
                pT = [sbuf(st, f"pT{i}", [128, T], BF16) for i in range(3)]
                rec = [sbuf(st, f"rec{i}", [64, T], F32) for i in range(2)]
                osb = [sbuf(st, f"osb{i}", [64, T], F32) for i in range(2)]
                ps_s = [psum(st, f"ps_s{i}", [128, T], F32) for i in range(3)]
                ps_o = [psum(st, f"ps_o{i}", [128, T], F32) for i in range(2)]
                ps_w = psum(st, "ps_w", [128, T], F32)
                b_psw = Buf("ps_w")
                B = {n: Buf(n) for n in ["kaug0", "kaug1", "vaug0", "vaug1", "qaug0", "qaug1", "pT0", "pT1", "pT2",
                                         "rec0", "rec1", "osb0", "osb1", "ps_s0", "ps_s1", "ps_s2", "ps_o0", "ps_o1", "o_d"]}
                for i in range(2):
                    memset("pool", vaug[i][:, :, 64:128], 1.0, [B[f"vaug{i}"]])
                    memset("pool", kaug[i][64:66, :], 1.0, [B[f"kaug{i}"]])
                blocks = []
                for h in range(8):
                    for c in range(NCH):
                        for j in range(4 * c + 4):
                            blocks.append((h, c, j))
                NB = len(blocks)
                dma("sp", kaug[0][0:64, :], kaug_d[0, 0:64, :], (), [B["kaug0"]])
                dma("sp", vaug[0][:, :, 0:64], v_d[0], (), [B["vaug0"]])

                def s_step(bi):
                    h, c, j = blocks[bi]
                    hb = h % 2
                    qb = (h * NCH + c) % 2
                    si = bi % 3
                    if j == 0:
                        dma("sp", qaug[qb][:], qaug_d[h, :, c * T:(c + 1) * T], (), [B[f"qaug{qb}"]])
                        issue_precast(3 if layer == 0 else 2, len(precast))
                    lo = max(0, j - 4 * c) * 128
                    mm(ps_s[si][:, lo:T], kaug[hb][0:66, j * 128:(j + 1) * 128], qaug[qb][0:66, lo:T], True, j < 4 * c,
                       [B[f"kaug{hb}"], B[f"qaug{qb}"]], [B[f"ps_s{si}"]])
                    if j >= 4 * c:
                        mm(ps_s[si][:, lo:T], ident_bf[:], maskb[:, j - 4 * c, lo:T], False, True, [b_const], [B[f"ps_s{si}"]])

                s_step(0)
                s_step(1)
                for bi in range(NB):
                    h, c, j = blocks[bi]
                    hb = h % 2
                    qb = (h * NCH + c) % 2
                    si = bi % 3
                    nj = 4 * c + 4
                    po, bpo = ps_o[qb], B[f"ps_o{qb}"]
                    if bi + 2 < NB:
                        s_step(bi + 2)
                    lo = max(0, j - 4 * c) * 128
                    act(pT[si][:, lo:T], ps_s[si][:, lo:T], AF.Exp, [B[f"ps_s{si}"], b_posF], [B[f"pT{si}"]], bias=posF[:, j, h:h + 1])
                    mm(po[:, lo:T], vaug[hb][:, j, :], pT[si][:, lo:T], j == 0, j == nj - 1, [B[f"vaug{hb}"], B[f"pT{si}"]], [bpo])
                    if PE_WARM_DUMMY:
                        mm(ps_w[:], ident_bf[:], maskb[:, 0, :], True, True, [b_const], [b_psw])
                    if j == nj - 1:
                        P.op("dve", lambda e, qb=qb, po=po: e.reciprocal(out=rec[qb][:], in_=po[64:128, :]), [bpo], [B[f"rec{qb}"]])
                        tt("dve", osb[qb][:], po[0:64, :], rec[qb][:], ALU.mult, [bpo, B[f"rec{qb}"]], [B[f"osb{qb}"]])
                        dma("pool", o_d[h, :, c * T:(c + 1) * T], osb[qb][:], [B[f"osb{qb}"]], ())
                        if c == 1 and h + 1 < 8:
                            nb_ = (h + 1) % 2
                            dma("sp", kaug[nb_][0:64, :], kaug_d[h + 1, 0:64, :], (), [B[f"kaug{nb_}"]])
                            dma("sp", vaug[nb_][:, :, 0:64], v_d[h + 1], (), [B[f"vaug{nb_}"]])
                P.drain_dmas("sp")
                P.emit()
            if STOP_AFTER == (layer, "p2"):
                break

            with contextlib.ExitStack() as st:
                moe = (layer == 1)
                nexp = NE if moe else 1
                nf = (DFF_E if moe else DFF_D) // 128
                wo_s = sbuf(st, "wo_s", [128, 4, D], BF16)
                wo_a = sbuf(st, "wo_a", [64, 8, D], BF16)
                wpg = sbuf(st, "wpg", [128, 8, D], BF16)
                wpp = sbuf(st, "wpp", [128, 2, D], BF16)
                xc = sbuf(st, "xc3", [128, 8, T], F32)
                ys = sbuf(st, "ys3", [128, 4, T], BF16)
                oc = sbuf(st, "oc3", [64, 8, T], F32)
                ya = sbuf(st, "ya3", [64, 8, T], BF16)
                sq = sbuf(st, "sq3", [128, 8, T], BF16)
                uT = sbuf(st, "uT3", [128, 8, T], BF16)
                lnt = sbuf(st, "lnt3", [128, T], F32)
                rstd = sbuf(st, "rstd3", [128, T], F32)
                hT = sbuf(st, "hT3", [128, nf, T], BF16)
                sg = [sbuf(st, f"sg3{i}", [128, T], F32) for i in range(2)]
                n_gu = 3 if moe else 6
                n_dp = 2 if moe else 3
                wgp = [sbuf(st, f"wgp{i}", [128, 8, 128], BF16) for i in range(n_gu)]
                wup = [sbuf(st, f"wup{i}", [128, 8, 128], BF16) for i in range(n_gu)]
                wdp = [sbuf(st, f"wdp{i}", [128, nf, 128], BF16) for i in range(n_dp)]
                pc_b = sbuf(st, "pc_b", [128, 2, T], BF16)
                tmp = [sbuf(st, f"tmp3{i}", [128, T], F32) for i in range(2)]
                pm = [psum(st, f"pm3{i}", [128, T], F32) for i in range(6)]
                pn = psum(st, "pn3", [128, T], F32)
                names = ["wo_s", "wo_a", "wpg", "wpp", "xc", "ys", "oc", "osq", "ya", "sq", "uT", "lnt", "rstd", "hT",
                         "sg0", "sg1", "wgp0", "wgp1", "wgp2", "wup0", "wup1", "wup2", "wdp0", "wdp1", "pc_f", "pc_b",
                         "gate0", "gate1", "tmp0", "tmp1", "pm0", "pm1", "pm2", "pm3", "pm4", "pm5", "pn", "x_dst",
                         "wr", "u2f", "lg", "cmb", "cbc", "dg", "mx"]
                B = {n: Buf(n) for n in names}
                for i_ in range(6):
                    B.setdefault(f"wgp{i_}", Buf(f"wgp{i_}"))
                    B.setdefault(f"wup{i_}", Buf(f"wup{i_}"))
                    B.setdefault(f"wdp{i_}", Buf(f"wdp{i_}"))
                if moe:
                    wr = sbuf(st, "wr3", [128, 8, NE], F32)
                    u2f = sbuf(st, "u2f3", [128, 8, T], F32)
                    lg = sbuf(st, "lg3", [128, 4, NE], F32)
                    mx = sbuf(st, "mx3", [128, 4, 8], F32)
                    cmb = sbuf(st, "cmb3", [128, 4, NE], F32)
                    cm2 = sbuf(st, "cm23", [128, 4, NE], F32)
                    gsm = sbuf(st, "gsm3", [128, 4, 4], F32)
                    dg = sbuf(st, "dg3", [128, NE, 128], F32)
                    cbc = sbuf(st, "cbc3", [128, NE, T], F32)
                pmi = [0]

                def next_pm3():
                    i = pmi[0] % 6
                    pmi[0] += 1
                    return pm[i], B[f"pm{i}"]

                load_cast(wo_s, wout_in[layer, 0:512, :], D, (), [B["wo_s"]])
                wo_av = wout_in[layer, 512:1024, :].rearrange("(h p) n -> p h n", p=64)
                for h in range(8):
                    dma("pool", wo_a[:, h, :], wo_av[:, h, :], (), [B["wo_a"]])
                load_cast(wpg, wpg_in[layer], D, (), [B["wpg"]])
                load_cast(wpp, wpp_in[layer], D, (), [B["wpp"]])
                if moe:
                    dma("sp", wr[:], wr_in[0].rearrange("(k p) n -> p k n", p=128), (), [B["wr"]])

                bsq = [Buf(f"sq{k}") for k in range(8)]
                bxc = [Buf(f"xc{k}") for k in range(8)]
                buT = [Buf(f"uT3_{k}") for k in range(8)]
                bu2f = [Buf(f"u2f{k}") for k in range(8)]

                def rmsnorm_full(gcol0, want_f32):
                    for k in range(8):
                        act(sq[:, k, :], xc[:, k, :], AF.Square, [bxc[k]], [bsq[k], B["sq"]])
                    for k in range(8):
                        mm(pn[:], ones_bf[:], sq[:, k, :], k == 0, k == 7, [b_const, bsq[k]], [B["pn"]])
                    act(lnt[:], pn[:], AF.Ln, [B["pn"]], [B["lnt"]], bias=EPS, scale=1.0 / D)
                    act(rstd[:], lnt[:], AF.Exp, [B["lnt"]], [B["rstd"]], scale=-0.5)
                    for k in range(8):
                        if want_f32:
                            stt(u2f[:, k, :], xc[:, k, :], PV(layer, gcol0 + k), rstd[:], ALU.mult, ALU.mult,
                                [bxc[k], b_pvec, B["rstd"]], [bu2f[k]])
                            cp("act", uT[:, k, :], u2f[:, k, :], [bu2f[k]], [buT[k]])
                        else:
                            stt(uT[:, k, :], xc[:, k, :], PV(layer, gcol0 + k), rstd[:], ALU.mult, ALU.mult,
                                [bxc[k], b_pvec, B["rstd"]], [buT[k]])

                piece = [0]
                dpiece = [0]

                def load_side(cc):
                    cc0 = cc * T
                    dma("sp", ys[:], yssd_d.rearrange("(k p) t -> p k t", p=128)[:, :, cc0:cc0 + T], (), [B["ys"]])
                    dma("sp", oc[:], o_d.rearrange("h p t -> p h t")[:, :, cc0:cc0 + T], (), [B["oc"]])

                def attn_norm():
                    for h in range(8):
                        act(sq[0:64, h, :], oc[:, h, :], AF.Square, [B["oc"]], [bsq[h], B["sq"]])
                    for h in range(8):
                        mm(pn[:], ones_bf[0:64, :], sq[0:64, h, :], h == 0, h == 7, [b_const, bsq[h]], [B["pn"]])
                    act(lnt[:], pn[:], AF.Ln, [B["pn"]], [B["lnt"]], bias=EPS, scale=1.0 / 512)
                    act(rstd[:], lnt[:], AF.Exp, [B["lnt"]], [B["rstd"]], scale=-0.5)
                    for h in range(8):
                        stt(ya[:, h, :], oc[:, h, :], PV(layer, 74 + h)[0:64, :], rstd[0:64, :], ALU.mult, ALU.mult,
                            [B["oc"], b_pvec, B["rstd"]], [B["ya"]])

                load_side(0)
                for c in range(NCH):
                    c0 = c * T
                    for k in range(8):
                        dma("sp", xc[:, k, :], xsv[:, k, c0:c0 + T], (), [bxc[k]])
                    dma("pool", pc_b[:], pT_in[layer].rearrange("(k p) t -> p k t", p=128)[:, :, c0:c0 + T], (), [B["pc_b"]])
                    if c == 0:
                        attn_norm()
                    for o in range(8):
                        pt, bp = next_pm3()
                        for j in range(4):
                            mm(pt[:], wo_s[:, j, o * 128:(o + 1) * 128], ys[:, j, :], j == 0, False, [B["wo_s"], B["ys"]], [bp])
                        for h in range(8):
                            mm(pt[:], wo_a[:, h, o * 128:(o + 1) * 128], ya[:, h, :], False, h == 7, [B["wo_a"], B["ya"]], [bp])
                        tt("dve", xc[:, o, :], xc[:, o, :], pt[:], ALU.add, [bxc[o], bp], [bxc[o]])
                    if c + 1 < NCH:
                        load_side(c + 1)
                    rmsnorm_full(8, moe)
                    def router_part1():
                        pt, bp = next_pm3()
                        for t4 in range(4):
                            for k in range(8):
                                mm(pt[:, t4 * 8:(t4 + 1) * 8], u2f[:, k, t4 * 128:(t4 + 1) * 128], wr[:, k, :], k == 0, k == 7,
                                   [bu2f[k], B["wr"]], [bp])
                        cp("dve", lg[:].rearrange("p a b -> p (a b)"), pt[:, 0:32], [bp], [B["lg"]])
                        for t4 in range(4):
                            P.op("dve", lambda e, t4=t4: e.max(out=mx[:, t4, :], in_=lg[:, t4, :]), [B["lg"]], [B["mx"]])
                        tt("dve", gsm[:, :, 0:1], mx[:, :, 1:2], mx[:, :, 0:1], ALU.subtract, [B["mx"]], [B["cmb"]])
                        act(gsm[:, :, 1:2], gsm[:, :, 0:1], AF.Exp, [B["cmb"]], [B["cmb"]])
                        ts("dve", gsm[:, :, 2:3], gsm[:, :, 1:2], 1.0, None, ALU.add, None, [B["cmb"]], [B["cmb"]])
                        P.op("dve", lambda e: e.reciprocal(out=gsm[:, :, 2:3], in_=gsm[:, :, 2:3]), [B["cmb"]], [B["cmb"]])
                        tt("dve", gsm[:, :, 3:4], gsm[:, :, 1:2], gsm[:, :, 2:3], ALU.mult, [B["cmb"]], [B["cmb"]])
                        for t4 in range(4):
                            ts("dve", cmb[:, t4, :], lg[:, t4, :], mx[:, t4, 0:1], gsm[:, t4, 2:3], ALU.is_equal, ALU.mult,
                               [B["lg"], B["mx"], B["cmb"]], [B["cmb"]])
                            ts("dve", cm2[:, t4, :], lg[:, t4, :], mx[:, t4, 1:2], gsm[:, t4, 3:4], ALU.is_equal, ALU.mult,
                               [B["lg"], B["mx"], B["cmb"]], [B["cmb"]])
                        tt("dve", cmb[:], cmb[:], cm2[:], ALU.add, [B["cmb"]], [B["cmb"]])
                    def router_part2():
                        for t4 in range(4):
                            tt("dve", dg[:], ident_f[:].unsqueeze(1).broadcast_to([128, NE, 128]),
                               cmb[:, t4, :].unsqueeze(2).broadcast_to([128, NE, 128]), ALU.mult,
                               [b_const, B["cmb"]], [B["dg"]])
                            for eh in range(2):
                                pt, bp = next_pm3()
                                mm(pt[:], ones_f[:], dg[:, eh * 4:(eh + 1) * 4, :].rearrange("p e t -> p (e t)"), True, True,
                                   [b_const, B["dg"]], [bp])
                                cp("act", cbc[:, eh * 4:(eh + 1) * 4, t4 * 128:(t4 + 1) * 128],
                                   pt[:].rearrange("p (e t) -> p e t", t=128), [bp], [B["cbc"]])
                    for e_ in range(nexp):
                        if moe:
                            wg_src, wu_src, wd_src = wge_b[e_], wue_b[e_], wde_b[e_]
                            kg, ku, kd = "ge", "ue", "de"
                        else:
                            wg_src, wu_src, wd_src = wgd_b[0], wud_b[0], wdd_b[0]
                            kg, ku, kd = "gd", "ud", "dd"
                        for f in range(nf):
                            pi = piece[0] % n_gu
                            piece[0] += 1
                            dma("sp", wgp[pi][:].rearrange("p k n -> p (k n)"), wg_src[f], [WB[(kg, e_, f)]], [B[f"wgp{pi}"]])
                            dma("sp", wup[pi][:].rearrange("p k n -> p (k n)"), wu_src[f], [WB[(ku, e_, f)]], [B[f"wup{pi}"]])
                            pg, bpg = next_pm3()
                            for k in range(8):
                                mm(pg[:], wgp[pi][:, k, :], uT[:, k, :], k == 0, k == 7, [B[f"wgp{pi}"], buT[k]], [bpg])
                            pu, bpu = next_pm3()
                            for k in range(8):
                                mm(pu[:], wup[pi][:, k, :], uT[:, k, :], k == 0, k == 7, [B[f"wup{pi}"], buT[k]], [bpu])
                            si = f % 2
                            act(sg[si][:], pg[:], AF.Silu, [bpg], [B[f"sg{si}"]])
                            tt("dve", hT[:, f, :], sg[si][:], pu[:], ALU.mult, [B[f"sg{si}"], bpu], [B["hT"]])
                            if c + 1 < NCH and ((moe and e_ == 1 and f == 2) or (not moe and f == 10)):
                                attn_norm()
                            if moe and e_ == 0 and f == 2:
                                router_part1()
                            if moe and e_ == 0 and f == 7:
                                router_part2()
                        for o in range(8):
                            di = o % 2
                            dpi = dpiece[0] % n_dp
                            dpiece[0] += 1
                            dma("sp", wdp[dpi][:].rearrange("p f n -> p (f n)"), wd_src[o], [WB[(kd, e_, o)]], [B[f"wdp{dpi}"]])
                            pt, bp = next_pm3()
                            for f in range(nf):
                                mm(pt[:], wdp[dpi][:, f, :], hT[:, f, :], f == 0, f == nf - 1, [B[f"wdp{dpi}"], B["hT"]], [bp])
                            if moe:
                                tt("dve", tmp[di][:], pt[:], cbc[:, e_, :], ALU.mult, [bp, B["cbc"]], [B[f"tmp{di}"]])
                                tt("dve", xc[:, o, :], xc[:, o, :], tmp[di][:], ALU.add, [bxc[o], B[f"tmp{di}"]], [bxc[o]])
                            else:
                                tt("dve", xc[:, o, :], xc[:, o, :], pt[:], ALU.add, [bxc[o], bp], [bxc[o]])
                    rmsnorm_full(16, False)
                    for o in range(8):
                        gi = o % 2
                        pt, bp = next_pm3()
                        for k in range(8):
                            mm(pt[:], wpg[:, k, o * 128:(o + 1) * 128], uT[:, k, :], k == 0, k == 7, [B["wpg"], buT[k]], [bp])
                        act(sg[gi][:], pt[:], AF.Sigmoid, [bp], [B[f"sg{gi}"]])
                        pt2, bp2 = next_pm3()
                        for k in range(2):
                            mm(pt2[:], wpp[:, k, o * 128:(o + 1) * 128], pc_b[:, k, :], k == 0, k == 1, [B["wpp"], B["pc_b"]], [bp2])
                        tt("dve", tmp[gi][:], sg[gi][:], pt2[:], ALU.mult, [B[f"sg{gi}"], bp2], [B[f"tmp{gi}"]])
                        tt("dve", tmp[gi][:], xc[:, o, :], tmp[gi][:], ALU.add, [bxc[o], B[f"tmp{gi}"]], [B[f"tmp{gi}"]])
                        dma("pool", xdv[:, o, c0:c0 + T], tmp[gi][:], [B[f"tmp{gi}"]], ())
                P.drain_dmas("sp")
                P.emit()
            if STOP_AFTER == (layer, "p3"):
                break
    return nc


def _prep_shared(inp):
    f32 = np.float32
    pvec = np.zeros((128, 2 * NPL), f32)
    wincat = np.zeros((2, D, NCAT), f32)
    for l in range(2):
        b = l * NPL
        pvec[:, b + 0:b + 8] = inp["norm1_g"][l].reshape(8, 128).T
        pvec[:, b + 8:b + 16] = inp["norm2_g"][l].reshape(8, 128).T
        pvec[:, b + 16:b + 24] = inp["ple_norm_g"][l].reshape(8, 128).T
        for tap in range(4):
            pvec[:, b + 24 + tap * 8:b + 32 + tap * 8] = inp["conv_w"][l, tap].reshape(8, 128).T
        pvec[:, b + 56:b + 64] = inp["conv_b"][l].reshape(8, 128).T
        for r0 in (0, 8, 32, 40):
            pvec[r0:r0 + 8, b + 64] = inp["dt_bias"][l]
            pvec[r0:r0 + 8, b + 65] = inp["a_log"][l]
        pvec[:, b + 66:b + 70] = np.repeat(inp["d_skip"][l], 64).reshape(4, 128).T
        pvec[:, b + 70:b + 74] = inp["ssd_norm_g"][l].reshape(4, 128).T
        pvec[0:64, b + 74:b + 82] = inp["attn_norm_g"][l].reshape(8, 64).T
        pvec[0:64, b + 82] = inp["q_norm_g"][l]
        pvec[64:128, b + 82] = inp["q_norm_g"][l]
        pvec[0:64, b + 83] = inp["k_norm_g"][l]
        pvec[64:128, b + 83] = inp["k_norm_g"][l]
        pvec[0:8, b + 84] = inp["fg_bias"][l]
        w = inp["w_in"][l]
        wincat[l, :, 0:1536] = w[:, 0:1536]
        for r0 in (0, 8, 32, 40):
            wincat[l, :, 1536 + r0:1544 + r0] = w[:, 1536:1544]
        wincat[l, :, 1584:2096] = w[:, 1544:2056]
        wincat[l, :, 2096:2608] = w[:, 2056:2568]
        wincat[l, :, 2608:3120] = w[:, 2568:3080]
        wincat[l, :, 3120:3128] = w[:, 3080:3088]
    c = np.ascontiguousarray

    def gu_layout(w):
        E, _, F = w.shape
        return c(w.reshape(E, 8, 128, F // 128, 128).transpose(0, 3, 2, 1, 4).reshape(E, F // 128, 128, 1024), dtype=f32)

    def d_layout(w):
        E, F, _ = w.shape
        return c(w.reshape(E, F // 128, 128, 8, 128).transpose(0, 3, 2, 1, 4).reshape(E, 8, 128, F), dtype=f32)

    return {
        "pvec": pvec, "wincat": wincat, "wout": c(inp["w_out"], dtype=f32),
        "wgd": gu_layout(inp["w_gate_dense"]), "wud": gu_layout(inp["w_up_dense"]),
        "wdd": d_layout(inp["w_down_dense"]), "wr": c(inp["w_router"], dtype=f32),
        "wge": gu_layout(inp["w_gate_exp"][0]), "wue": gu_layout(inp["w_up_exp"][0]),
        "wde": d_layout(inp["w_down_exp"][0]),
        "wpg": c(inp["w_ple_gate"], dtype=f32), "wpp": c(inp["w_ple_proj"], dtype=f32),
    }


def kernel(**inputs):
    inp = {k: np.asarray(v) for k, v in inputs.items()}
    shared = _prep_shared(inp)
    x = inp["x"].astype(np.float32, copy=False)
    p = inp["p"].astype(np.float32, copy=False)
    in_maps = []
    for b in range(8):
        m = dict(shared)
        m["xT"] = np.ascontiguousarray(x[b].T)
        m["pT"] = np.ascontiguousarray(p[:, b].transpose(0, 2, 1))
        in_maps.append(m)
    nc = build_program()
    res = run_bass_kernel_spmd(nc, in_maps, core_ids=list(range(8)))
    out = np.stack([np.ascontiguousarray(r["yT"].T) for r in res.results], axis=0)
    return out.astype(np.float32, copy=False)
```

```python
import contextlib
import numpy as np
import concourse.bass as bass
import concourse.mybir as mybir
from concourse.bass_utils import run_bass_kernel_spmd
from concourse.alu_op_type import AluOpType as ALU

AF = mybir.ActivationFunctionType
F32 = mybir.dt.float32
BF16 = mybir.dt.bfloat16

S = 4096
D = 1024
T = 512
NCH = S // T
NCAT = 3128
NPL = 88
EPS = 1e-6
DFF_D = 2816
DFF_E = 1408
NE = 8

SEM_WIN = 8192
NDS = 12

DEBUG = False
NO_PRECAST = False
STRICT_POOL = False
PRECAST_LIMIT = None
PE_WARM_DUMMY = False
PRECAST_STORE_Q = "pool"
STOP_AFTER = None


class Buf:
    __slots__ = ("name", "w", "r")

    def __init__(self, name=""):
        self.name = name
        self.w = None
        self.r = {}


class Prog:
    ENG = ("pe", "act", "dve", "pool", "sp")

    def __init__(self, nc, stack):
        self.nc = nc
        self.stack = stack
        self.q = {e: [] for e in self.ENG}
        self.cnt = {e: 0 for e in self.ENG}
        self.known = {e: {} for e in self.ENG}
        self.csem = {e: [] for e in self.ENG}
        self.dsem = {}
        self.dval = {}
        self.drr = {}
        for qn in ("sp", "pool", "act"):
            self.dsem[qn] = [stack.enter_context(nc.semaphore(f"d_{qn}_{i}")) for i in range(NDS)]
            self.dval[qn] = [0] * NDS
            self.drr[qn] = 0

    def _csem(self, eng, win):
        lst = self.csem[eng]
        while len(lst) <= win:
            lst.append(self.stack.enter_context(self.nc.semaphore(f"c_{eng}_{len(lst)}")))
        return lst[win]

    def _need(self, eng, waits, ev):
        key, val = ev
        if self.known[eng].get(key, 0) >= val:
            return
        self.known[eng][key] = val
        waits[key] = max(waits.get(key, 0), val)

    def _deps(self, eng, reads, writes, is_dma):
        waits = {}
        for b in reads:
            if b.w is not None:
                self._need(eng, waits, b.w[:2])
        for b in writes:
            if b.w is not None:
                k, v, we = b.w
                if is_dma or we != eng or k[0] == "d" or (STRICT_POOL and eng == "pool"):
                    self._need(eng, waits, (k, v))
            for k, (v, re) in b.r.items():
                if is_dma or re != eng or k[0] == "d" or (STRICT_POOL and eng == "pool"):
                    self._need(eng, waits, (k, v))
        return waits

    def _lower_waits(self, waits):
        out = []
        for key, val in waits.items():
            if key[0] == "c":
                win = (val - 1) // SEM_WIN
                out.append((self._csem(key[1], win), val - win * SEM_WIN))
            else:
                out.append((self.dsem[key[1]][key[2]], val))
        return out

    def _record(self, ev, eng, reads, writes):
        key, val = ev
        for b in reads:
            old = b.r.get(key)
            if old is None or old[0] < val:
                b.r[key] = (val, eng)
        for b in writes:
            b.w = (key, val, eng)
            b.r = {}

    def op(self, eng, fn, reads=(), writes=()):
        waits = self._deps(eng, reads, writes, False)
        self.cnt[eng] += 1
        idx = self.cnt[eng]
        win = (idx - 1) // SEM_WIN
        sem = self._csem(eng, win)
        self.q[eng].append((self._lower_waits(waits), fn, (sem, 1)))
        ev = (("c", eng), idx)
        self._record(ev, eng, reads, writes)
        return ev

    def dma(self, qn, fn, reads=(), writes=()):
        waits = self._deps(qn, reads, writes, True)
        slot = self.drr[qn]
        self.drr[qn] = (slot + 1) % NDS
        cur = self.dval[qn][slot]
        key = ("d", qn, slot)
        if cur > 0:
            self._need(qn, waits, (key, cur))
        self.dval[qn][slot] = cur + 16
        self.q[qn].append((self._lower_waits(waits), fn, (self.dsem[qn][slot], 16)))
        ev = (key, cur + 16)
        self._record(ev, qn, reads, writes)
        return ev

    def drain_dmas(self, eng="sp"):
        waits = {}
        for qn in ("sp", "pool", "act"):
            for s in range(NDS):
                if self.dval[qn][s] > 0:
                    self._need(eng, waits, (("d", qn, s), self.dval[qn][s]))
        self.q[eng].append((self._lower_waits(waits), None, None))

    def emit(self):
        nc = self.nc
        qs = self.q
        self.q = {e: [] for e in self.ENG}

        def run(engobj, lst):
            for waits, fn, inc in lst:
                for s, v in waits:
                    engobj.wait_ge(s, v)
                if fn is not None:
                    ins = fn(engobj)
                    ins.then_inc(inc[0], inc[1])

        with nc.Block() as block:
            @block.tensor
            def _(e):
                run(e, qs["pe"])

            @block.scalar
            def _(e):
                run(e, qs["act"])

            @block.vector
            def _(e):
                run(e, qs["dve"])

            @block.gpsimd
            def _(e):
                run(e, qs["pool"])

            @block.sync
            def _(e):
                run(e, qs["sp"])


def build_program():
    nc = bass.Bass("TRN2", target_bir_lowering=False)
    dr = lambda name, shape, dt, kind: nc.dram_tensor(name, shape, dt, kind=kind).ap()
    skind = "ExternalOutput" if DEBUG else "Internal"
    xT_in = dr("xT", [D, S], F32, "ExternalInput")
    pT_in = dr("pT", [2, 256, S], F32, "ExternalInput")
    pvec_in = dr("pvec", [128, 2 * NPL], F32, "ExternalInput")
    wincat_in = dr("wincat", [2, D, NCAT], F32, "ExternalInput")
    wout_in = dr("wout", [2, D, D], F32, "ExternalInput")
    NFD, NFE = DFF_D // 128, DFF_E // 128
    wgd_in = dr("wgd", [1, NFD, 128, 1024], F32, "ExternalInput")
    wud_in = dr("wud", [1, NFD, 128, 1024], F32, "ExternalInput")
    wdd_in = dr("wdd", [1, 8, 128, NFD * 128], F32, "ExternalInput")
    wr_in = dr("wr", [1, D, NE], F32, "ExternalInput")
    wge_in = dr("wge", [NE, NFE, 128, 1024], F32, "ExternalInput")
    wue_in = dr("wue", [NE, NFE, 128, 1024], F32, "ExternalInput")
    wde_in = dr("wde", [NE, 8, 128, NFE * 128], F32, "ExternalInput")
    wpg_in = dr("wpg", [2, D, D], F32, "ExternalInput")
    wpp_in = dr("wpp", [2, 256, D], F32, "ExternalInput")
    yT_out = dr("yT", [D, S], F32, "ExternalOutput")

    wgd_b = dr("wgd_b", [1, NFD, 128, 1024], BF16, "Internal")
    wud_b = dr("wud_b", [1, NFD, 128, 1024], BF16, "Internal")
    wdd_b = dr("wdd_b", [1, 8, 128, NFD * 128], BF16, "Internal")
    wge_b = dr("wge_b", [NE, NFE, 128, 1024], BF16, "Internal")
    wue_b = dr("wue_b", [NE, NFE, 128, 1024], BF16, "Internal")
    wde_b = dr("wde_b", [NE, 8, 128, NFE * 128], BF16, "Internal")
    qaug_d = dr("qaug_d", [8, 66, S], BF16, skind)
    kaug_d = dr("kaug_d", [8, 65, S], BF16, skind)
    v_d = dr("v_d", [8, 128, 32, 64], BF16, skind)
    yssd_d = dr("yssd_d", [512, S], BF16, skind)
    o_d = dr("o_d", [8, 64, S], F32, skind)
    xmid_d = dr("xmid_d", [D, S], F32, skind)

    with contextlib.ExitStack() as gst:
        P = Prog(nc, gst)

        uid = [0]

        def sbuf(st, name, shape, dt):
            uid[0] += 1
            return st.enter_context(nc.sbuf_tensor(f"s{uid[0]}_{name}", shape, dt))

        def psum(st, name, shape, dt):
            uid[0] += 1
            return st.enter_context(nc.psum_tensor(f"p{uid[0]}_{name}", shape, dt))

        def mm(out, lhsT, rhs, start, stop, reads, writes):
            P.op("pe", lambda e: e.matmul(out, lhsT=lhsT, rhs=rhs, start=start, stop=stop), reads, writes)

        def tr(out, in_, ident, reads, writes):
            P.op("pe", lambda e: e.transpose(out, in_, ident), reads, writes)

        def act(out, in_, func, reads, writes, bias=None, scale=None, eng="act"):
            kw = {}
            if bias is not None:
                kw["bias"] = bias
            if scale is not None:
                kw["scale"] = scale
            P.op(eng, lambda e: e.activation(out=out, in_=in_, func=func, **kw), reads, writes)

        def tt(eng, out, in0, in1, op, reads, writes):
            P.op(eng, lambda e: e.tensor_tensor(out=out, in0=in0, in1=in1, op=op), reads, writes)

        def ts(eng, out, in0, s1, s2, op0, op1, reads, writes):
            if op1 is None:
                P.op(eng, lambda e: e.tensor_scalar(out=out, in0=in0, scalar1=s1, scalar2=None, op0=op0), reads, writes)
            else:
                P.op(eng, lambda e: e.tensor_scalar(out=out, in0=in0, scalar1=s1, scalar2=s2, op0=op0, op1=op1), reads, writes)

        def stt(out, in0, scalar, in1, op0, op1, reads, writes):
            P.op("dve", lambda e: e.scalar_tensor_tensor(out=out, in0=in0, scalar=scalar, in1=in1, op0=op0, op1=op1), reads, writes)

        def cp(eng, out, in_, reads, writes):
            if eng == "act":
                P.op("act", lambda e: e.activation(out=out, in_=in_, func=AF.Copy), reads, writes)
            else:
                P.op(eng, lambda e: e.tensor_copy(out=out, in_=in_), reads, writes)

        def memset(eng, ap, val, writes):
            P.op(eng, lambda e: e.memset(ap, val), (), writes)

        def dma(qn, out, in_, reads, writes):
            P.dma(qn, lambda e: e.dma_start(out=out, in_=in_), reads, writes)

        def load_cast(dst3, src2, ncols, reads, writes):
            kc = dst3.shape[1]
            srcv = src2.rearrange("(k p) n -> p k n", p=128)
            for k in range(kc):
                c0 = 0
                while c0 < ncols:
                    c1 = min(ncols, c0 + 2048)
                    dma("pool", dst3[:, k, c0:c1], srcv[:, k, c0:c1], reads, writes)
                    c0 = c1

        ident_bf = sbuf(gst, "ident_bf", [128, 128], BF16)
        ident_f = sbuf(gst, "ident_f", [128, 128], F32)
        ones_bf = sbuf(gst, "ones_bf", [128, 128], BF16)
        ones_f = sbuf(gst, "ones_f", [128, 128], F32)
        bdones = sbuf(gst, "bdones", [128, 128], BF16)
        maskb = sbuf(gst, "maskb", [128, 4, T], BF16)
        ssdmask = sbuf(gst, "ssdmask", [128, 4, 128], BF16)
        delta = sbuf(gst, "delta", [48, 2, 4, 128], F32)
        delta_b = sbuf(gst, "delta_b", [48, 2, 4, 128], BF16)
        mhl = sbuf(gst, "mhl", [48, 2], F32)
        resetm = sbuf(gst, "resetm", [48, T], F32)
        pvec = sbuf(gst, "pvec", [128, 2 * NPL], F32)
        dvec = sbuf(gst, "dvec", [128, 8], F32)
        posF = sbuf(gst, "posF", [128, 32, 8], F32)
        st0 = contextlib.ExitStack()
        tmpf = sbuf(st0, "tmpf", [128, 4, T], F32)
        b_const = Buf("const")
        b_pvec = Buf("pvec")
        b_dvec = Buf("dvec")
        b_posF = Buf("posF")
        b_tmpf = Buf("tmpf")

        dma("sp", pvec[:], pvec_in, (), [b_pvec])
        memset("pool", ident_f[:], 1.0, [b_const])
        P.op("pool", lambda e: e.affine_select(out=ident_f[:], in_=ident_f[:], pattern=[[-1, 128]],
                                                compare_op=ALU.is_equal, fill=0.0, base=0, channel_multiplier=1),
             [b_const], [b_const])
        cp("pool", ident_bf[:], ident_f[:], [b_const], [b_const])
        memset("pool", ones_f[:], 1.0, [b_const])
        memset("pool", ones_bf[:], 1.0, [b_const])
        memset("pool", bdones[:], 0.0, [b_const])
        memset("pool", bdones[0:64, 0:64], 1.0, [b_const])
        memset("pool", bdones[64:128, 64:128], 1.0, [b_const])
        memset("pool", tmpf[:], 0.0, [b_tmpf])
        for k in range(4):
            P.op("pool", lambda e, k=k: e.affine_select(out=tmpf[:, k, :], in_=tmpf[:, k, :], pattern=[[1, T]],
                                                        compare_op=ALU.is_ge, fill=-30000.0, base=-128 * k,
                                                        channel_multiplier=-1),
                 [b_tmpf], [b_tmpf])
        cp("pool", maskb[:], tmpf[:], [b_tmpf], [b_const])
        P.op("pool", lambda e: e.affine_select(out=tmpf[:, 0, :].rearrange("p (j l) -> p j l", l=128),
                                               in_=tmpf[:, 0, :].rearrange("p (j l) -> p j l", l=128),
                                               pattern=[[0, 4], [1, 128]], compare_op=ALU.is_ge, fill=-30000.0,
                                               base=0, channel_multiplier=-1),
             [b_tmpf, b_const], [b_tmpf])
        memset("pool", tmpf[:, 1, :], 0.0, [b_tmpf])
        P.op("pool", lambda e: e.affine_select(out=tmpf[:, 1, :].rearrange("p (j l) -> p j l", l=128),
                                               in_=tmpf[:, 1, :].rearrange("p (j l) -> p j l", l=128),
                                               pattern=[[0, 4], [1, 128]], compare_op=ALU.is_ge, fill=-30000.0,
                                               base=0, channel_multiplier=-1),
             [b_tmpf], [b_tmpf])
        cp("pool", ssdmask[:].rearrange("p j l -> p (j l)"), tmpf[:, 1, :], [b_tmpf], [b_const])
        memset("pool", delta[:], 0.0, [b_const])
        for g in range(2):
            for base_p in (0, 32):
                for off in (0, 8):
                    P.op("pool", lambda e, g=g, bp=base_p, off=off: e.affine_select(
                        out=delta[bp:bp + 16, g, :, :], in_=delta[bp:bp + 16, g, :, :], pattern=[[-1, 4], [0, 128]],
                        compare_op=ALU.not_equal, fill=1.0, base=-4 * g - off, channel_multiplier=1),
                        [b_const], [b_const])
        cp("pool", delta_b[:], delta[:], [b_const], [b_const])
        memset("pool", mhl[:], 0.0, [b_const])
        for base_p in (0, 32):
            P.op("pool", lambda e, bp=base_p: e.affine_select(
                out=mhl[bp:bp + 16, 0:1], in_=mhl[bp:bp + 16, 0:1], pattern=[[0, 1]],
                compare_op=ALU.is_ge, fill=1.0, base=-8, channel_multiplier=1), [b_const], [b_const])
        ts("pool", mhl[:, 1:2], mhl[:, 0:1], -1.0, 1.0, ALU.mult, ALU.add, [b_const], [b_const])
        memset("pool", resetm[:], 1.0, [b_const])
        memset("pool", resetm[:].rearrange("p (c l) -> p c l", l=128)[:, :, 0:1], 0.0, [b_const])
        memset("pool", posF[:], 0.0, [b_posF])
        P.drain_dmas("sp")
        P.emit()
        st0.close()

        PV = lambda l, c: pvec[:, l * NPL + c: l * NPL + c + 1]

        WB = {}
        precast = []

        def add_precast(name, dst, src, ncols):
            WB[name] = Buf(name)
            c0_ = 0
            while c0_ < ncols:
                c1_ = min(ncols, c0_ + 1024)
                precast.append((WB[name], dst[:, c0_:c1_], src[:, c0_:c1_], c1_ - c0_))
                c0_ = c1_

        for f_ in range(NFD):
            add_precast(("gd", 0, f_), wgd_b[0, f_], wgd_in[0, f_], 1024)
            add_precast(("ud", 0, f_), wud_b[0, f_], wud_in[0, f_], 1024)
        for o_ in range(8):
            add_precast(("dd", 0, o_), wdd_b[0, o_], wdd_in[0, o_], NFD * 128)
        n_pre_dense = len(precast)
        for e_ in range(NE):
            for f_ in range(NFE):
                add_precast(("ge", e_, f_), wge_b[e_, f_], wge_in[e_, f_], 1024)
                add_precast(("ue", e_, f_), wue_b[e_, f_], wue_in[e_, f_], 1024)
            for o_ in range(8):
                add_precast(("de", e_, o_), wde_b[e_, o_], wde_in[e_, o_], NFE * 128)
        pre_i = [0]

        stg = sbuf(gst, "stg", [128, 3, 1024], BF16)
        b_stg = [Buf(f"stg{i}") for i in range(3)]

        def issue_precast(n, limit):
            if PRECAST_LIMIT is not None:
                limit = min(limit, PRECAST_LIMIT)
            n = min(n, limit - pre_i[0])
            while n > 0:
                g_ = min(3, n)
                items = precast[pre_i[0]:pre_i[0] + g_]
                pre_i[0] += g_
                n -= g_
                for i_, (b_, d_, s_, w_) in enumerate(items):
                    dma("pool", stg[:, i_, 0:w_], s_, (), [b_stg[i_]])
                for i_, (b_, d_, s_, w_) in enumerate(items):
                    dma(PRECAST_STORE_Q, d_, stg[:, i_, 0:w_], [b_stg[i_]], [b_])

        for layer in range(2):
            x_src = xT_in if layer == 0 else xmid_d
            x_dst = xmid_d if layer == 0 else yT_out
            xsv = x_src.rearrange("(k p) t -> p k t", p=128)
            xdv = x_dst.rearrange("(k p) t -> p k t", p=128)

            act(dvec[0:48, 0:1], PV(layer, 65)[0:48, :], AF.Exp, [b_pvec], [b_dvec])
            ts("dve", dvec[0:48, 0:1], dvec[0:48, 0:1], -1.0, None, ALU.mult, None, [b_dvec], [b_dvec])
            ts("dve", dvec[:, 1:2], PV(layer, 82), 0.125, None, ALU.mult, None, [b_pvec], [b_dvec])
            ts("dve", dvec[0:8, 2:3], PV(layer, 84)[0:8, :], -1.0, None, ALU.mult, None, [b_pvec], [b_dvec])

            with contextlib.ExitStack() as st:
                win = sbuf(st, "win", [128, 8, NCAT], BF16)
                xc = sbuf(st, "xc", [128, 8, T], F32)
                sq = sbuf(st, "sq", [128, 8, T], BF16)
                uT = sbuf(st, "uT", [128, 8, T], BF16)
                lnt = sbuf(st, "lnt", [128, T], F32)
                rstd = sbuf(st, "rstd", [128, T], F32)
                zs = sbuf(st, "zs", [128, 4, T], F32)
                xpre = sbuf(st, "xpre", [128, 8, T + 4], BF16)
                xact = sbuf(st, "xact", [128, 8, T], BF16)
                diag = sbuf(st, "diag", [128, 32, 128], BF16)
                xsB = [sbuf(st, f"xsB{i}", [128, 768], BF16) for i in range(2)]
                dt40 = sbuf(st, "dt40", [48, T], F32)
                adt = sbuf(st, "adt", [48, T], F32)
                acum = sbuf(st, "acum", [48, T], F32)
                lndt = sbuf(st, "lndt", [48, T], F32)
                lhsD = sbuf(st, "lhsD", [48, T], BF16)
                splh = sbuf(st, "splh", [48, T], BF16)
                spll = sbuf(st, "spll", [48, T], BF16)
                acomb = sbuf(st, "acomb", [48, T], BF16)
                rhsD = sbuf(st, "rhsD", [48, 2, 2, 4, 128], BF16)
                expD = [sbuf(st, f"expD{i}", [128, 4, 128], F32) for i in range(2)]
                Eb = [sbuf(st, f"Eb{i}", [128, 4, 128], F32) for i in range(2)]
                Wt = [sbuf(st, f"Wt{i}", [128, 4, 128], BF16) for i in range(2)]
                Cs = [sbuf(st, f"Cs{i}", [128, 4, 128], BF16) for i in range(2)]
                xdd = [sbuf(st, f"xdd{i}", [128, 4, 64], BF16) for i in range(2)]
                prev_f = sbuf(st, "prev_f", [128, 2, 4, 64], F32)
                prev_b = sbuf(st, "prev_b", [128, 2, 4, 64], BF16)
                ych = sbuf(st, "ych", [128, 4, T], F32)
                yout = sbuf(st, "yout", [128, 4, T], BF16)
                qkst = [sbuf(st, f"qkst{i}", [128, T], BF16) for i in range(4)]
                hsq = [sbuf(st, f"hsq{i}", [128, T], BF16) for i in range(2)]
                vsb = sbuf(st, "vsb", [128, 8, 4, 64], BF16)
                fE = sbuf(st, "fE", [8, T], F32)
                fsp = sbuf(st, "fsp", [8, T], F32)
                fcum = sbuf(st, "fcum", [8, T], F32)
                fneg = sbuf(st, "fneg", [8, T], BF16)
                fneg2 = sbuf(st, "fneg2", [8, T], BF16)
                fcar = sbuf(st, "fcar", [8, 1], F32)
                ones8 = sbuf(st, "ones8", [8, T], F32)
                pm = [psum(st, f"pm{i}", [128, T], F32) for i in range(2)]
                pn = psum(st, "pn", [128, T], F32)
                pD = psum(st, "pD", [128, 4, 128], F32)
                pAb = psum(st, "pAb", [128, 4, 128], F32)
                pGs = psum(st, "pGs", [128, T], F32)
                py = psum(st, "py", [128, 4, 128], F32)
                ptr = psum(st, "ptr", [128, 1024], BF16)
                B = {n: Buf(n) for n in ["win", "xc", "sq", "uT", "lnt", "rstd", "zs", "xpre", "xact", "diag",
                                         "dtE", "dt40", "adt", "acum", "lndt", "lhsD", "rhsD", "splh", "spll", "acomb", "prev_f", "prev_b",
                                         "ych", "ysq", "yout", "qsb", "ksb", "vsb", "fE", "fsp", "fcum", "fneg",
                                         "fcar", "fneg2", "pm0", "pm1", "pn", "pD", "pAb", "pG", "pG1", "pst", "py", "ptr",
                                         "xsB0", "xsB1", "expD0", "expD1", "Eb0", "Eb1", "Wt0", "Wt1", "Cs0", "Cs1",
                                         "xdd0", "xdd1", "hsq0", "hsq1", "qkst0", "qkst1", "qkst2", "qkst3", "rhsD0", "rhsD1",
                                         "qaug_d", "kaug_d", "v_d", "yssd_d"]}
                pmi = [0]
                deferred = [None]
                buT = [Buf(f"uT{k}") for k in range(8)]
                bsq1 = [Buf(f"sq1_{k}") for k in range(8)]

                def next_pm():
                    i = pmi[0] % 2
                    pmi[0] += 1
                    return pm[i], B[f"pm{i}"]

                load_cast(win, wincat_in[layer], NCAT, (), [B["win"]])
                for tap in range(4):
                    for o in range(8):
                        ts("dve", diag[:, tap * 8 + o, :], ident_f[:], PV(layer, 24 + tap * 8 + o), None, ALU.mult, None,
                           [b_const, b_pvec], [B["diag"]])
                memset("pool", xpre[:], 0.0, [B["xpre"]])
                memset("pool", prev_f[:], 0.0, [B["prev_f"]])
                memset("pool", prev_b[:], 0.0, [B["prev_b"]])
                memset("pool", fcar[:], 0.0, [B["fcar"]])
                memset("pool", ones8[:], 1.0, [b_const])
                memset("pool", lhsD[:], 0.0, [B["lhsD"]])
                memset("pool", lhsD[0:16, :], 1.0, [B["lhsD"]])
                memset("pool", rhsD[:], 0.0, [B["rhsD"]])
                for par in range(2):
                    for g in range(2):
                        cp("pool", rhsD[32:48, par, g, :, :], delta_b[32:48, g, :, :], [b_const], [B["rhsD"]])

                for c in range(NCH):
                    c0 = c * T
                    if layer == 0:
                        issue_precast((n_pre_dense + NCH - 1) // NCH, n_pre_dense)

                    dma("sp", xc[:], xsv[:, :, c0:c0 + T], (), [B["xc"]])
                    for k in range(8):
                        act(sq[:, k, :], xc[:, k, :], AF.Square, [B["xc"]], [bsq1[k]])
                    for k in range(8):
                        mm(pn[:], ones_bf[:], sq[:, k, :], k == 0, k == 7, [b_const, bsq1[k]], [B["pn"]])
                    act(lnt[:], pn[:], AF.Ln, [B["pn"]], [B["lnt"]], bias=EPS, scale=1.0 / D)
                    act(rstd[:], lnt[:], AF.Exp, [B["lnt"]], [B["rstd"]], scale=-0.5)
                    for k in range(8):
                        stt(uT[:, k, :], xc[:, k, :], PV(layer, k), rstd[:], ALU.mult, ALU.mult,
                            [B["xc"], b_pvec, B["rstd"]], [buT[k]])
                    for o in range(12):
                        pt, bp = next_pm()
                        for k in range(8):
                            mm(pt[:], win[:, k, o * 128:(o + 1) * 128], uT[:, k, :], k == 0, k == 7,
                               [B["win"], buT[k]], [bp])
                        if o < 4:
                            act(zs[:, o, :], pt[:], AF.Silu, [bp], [B["zs"]])
                        else:
                            cp("dve", xpre[:, o - 4, 3:3 + T], pt[:], [bp], [B["xpre"]])
                        if o == 5 and deferred[0] is not None:
                            deferred[0]()
                            deferred[0] = None
                    for o in range(8):
                        pt, bp = next_pm()
                        for tap in range(4):
                            mm(pt[:], diag[:, tap * 8 + o, :], xpre[:, o, tap:tap + T], tap == 0, tap == 3,
                               [B["diag"], B["xpre"]], [bp])
                        act(xact[:, o, :], pt[:], AF.Silu, [bp, b_pvec], [B["xact"]], bias=PV(layer, 56 + o))
                    cp("pool", xpre[:, :, 0:3], xpre[:, :, T:T + 3], [B["xpre"]], [B["xpre"]])
                    pt, bp = next_pm()
                    for k in range(8):
                        mm(pt[0:48, :], win[:, k, 1536:1584], uT[:, k, :], k == 0, k == 7, [B["win"], buT[k]], [bp])
                    act(adt[:], pt[0:48, :], AF.Exp, [bp, b_pvec], [B["adt"]], bias=PV(layer, 64)[0:48, :])
                    act(dt40[:], adt[:], AF.Ln, [B["adt"]], [B["dt40"]], bias=1.0)
                    act(lndt[:], dt40[:], AF.Ln, [B["dt40"]], [B["lndt"]])
                    ts("dve", adt[:], dt40[:], dvec[0:48, 0:1], None, ALU.mult, None, [B["dt40"], b_dvec], [B["adt"]])
                    P.op("dve", lambda e: e.tensor_tensor_scan(out=acum[:], data0=resetm[:], data1=adt[:], initial=0.0,
                                                               op0=ALU.mult, op1=ALU.add),
                         [b_const, B["adt"]], [B["acum"]])
                    tt("dve", lndt[32:48, :], lndt[32:48, :], acum[32:48, :], ALU.subtract,
                       [B["lndt"], B["acum"]], [B["lndt"]])
                    for (r0, src, bsrc, dstt, bdst) in ((0, acum, B["acum"], acomb, B["acomb"]),
                                                        (32, lndt, B["lndt"], lhsD, B["lhsD"])):
                        rs_ = slice(r0, r0 + 16)
                        cp("pool", splh[rs_, :], src[rs_, :], [bsrc], [B["splh"]])
                        tt("dve", dt40[rs_, :], src[rs_, :], splh[rs_, :], ALU.subtract, [bsrc, B["splh"], B["dt40"]], [B["dt40"]])
                        cp("pool", spll[rs_, :], dt40[rs_, :], [B["dt40"]], [B["spll"]])
                        ts("dve", dstt[rs_, :], splh[rs_, :], mhl[rs_, 0:1], None, ALU.mult, None, [B["splh"], b_const], [bdst])
                        stt(dstt[rs_, :], spll[rs_, :], mhl[rs_, 1:2], dstt[rs_, :], ALU.mult, ALU.add,
                            [B["spll"], b_const, bdst], [bdst])
                    qk_items = [(which, hp) for which in range(2) for hp in range(4)]
                    qk_pt = {}

                    qk_banks = [(pm[0][:], B["pm0"]), (pm[1][:], B["pm1"]),
                                (pD[:].rearrange("p j l -> p (j l)"), B["pD"]), (pAb[:].rearrange("p j l -> p (j l)"), B["pAb"])]

                    def qk_proj(idx):
                        which, hp = qk_items[idx]
                        col0 = (1584 if which == 0 else 2096) + hp * 128
                        pt, bp = qk_banks[idx % 4]
                        for k in range(8):
                            mm(pt[:], win[:, k, col0:col0 + 128], uT[:, k, :], k == 0, k == 7, [B["win"], buT[k]], [bp])
                        qk_pt[idx] = (pt, bp)

                    def qk_norm(idx):
                        which, hp = qk_items[idx]
                        pt, bp = qk_pt[idx]
                        i = idx % 2
                        qi = idx % 4
                        dst = qkst[qi]
                        bdst = B[f"qkst{qi}"]
                        gcol = dvec[:, 1:2] if which == 0 else PV(layer, 83)
                        act(hsq[i][:], pt[:], AF.Square, [bp], [B[f"hsq{i}"]])
                        mm(pn[:], bdones[:], hsq[i][:], True, True, [b_const, B[f"hsq{i}"]], [B["pn"]])
                        act(lnt[:], pn[:], AF.Ln, [B["pn"]], [B["lnt"]], bias=EPS, scale=1.0 / 64)
                        act(rstd[:], lnt[:], AF.Exp, [B["lnt"]], [B["rstd"]], scale=-0.5)
                        stt(dst[:], pt[:], gcol, rstd[:], ALU.mult, ALU.mult, [bp, b_dvec, b_pvec, B["rstd"]], [bdst])
                        ddst = qaug_d if which == 0 else kaug_d
                        for half in range(2):
                            dma("pool", ddst[2 * hp + half, 0:64, c0:c0 + T], dst[half * 64:(half + 1) * 64, :], [bdst], ())

                    qk_proj(0)
                    qk_proj(1)
                    for idx in range(8):
                        if idx + 2 < 8:
                            qk_proj(idx + 2)
                        qk_norm(idx)
                    pt, bp = next_pm()
                    for k in range(8):
                        mm(pt[0:8, :], win[:, k, 3120:3128], uT[:, k, :], k == 0, k == 7, [B["win"], buT[k]], [bp])
                    act(fE[:], pt[0:8, :], AF.Exp, [bp, b_dvec], [B["fE"]], bias=dvec[0:8, 2:3], scale=-1.0)
                    act(fsp[:], fE[:], AF.Ln, [B["fE"]], [B["fsp"]], bias=1.0)
                    P.op("dve", lambda e: e.tensor_tensor_scan(out=fcum[:], data0=ones8[:], data1=fsp[:], initial=fcar[:],
                                                               op0=ALU.mult, op1=ALU.add),
                         [b_const, B["fsp"], B["fcar"]], [B["fcum"]])
                    cp("dve", fcar[:], fcum[:, T - 1:T], [B["fcum"]], [B["fcar"]])
                    ts("dve", fneg[:], fcum[:], -1.0, None, ALU.mult, None, [B["fcum"]], [B["fneg"]])
                    dma("pool", qaug_d[:, 64, c0:c0 + T], fneg[:], [B["fneg"]], ())
                    stt(fE[:], fcum[:], -1.0, fneg[:], ALU.mult, ALU.subtract, [B["fcum"], B["fneg"]], [B["fE"]])
                    cp("dve", fneg2[:], fE[:], [B["fE"]], [B["fneg2"]])
                    dma("pool", qaug_d[:, 65, c0:c0 + T], fneg2[:], [B["fneg2"]], ())
                    pt, bp = next_pm()
                    for s4 in range(4):
                        tr(pt[:, s4 * 8:(s4 + 1) * 8], fcum[:, s4 * 128:(s4 + 1) * 128], ident_f[0:8, 0:8],
                           [B["fcum"], b_const], [bp])
                    cp("dve", posF[:, c * 4:(c + 1) * 4, :], pt[:, 0:32].rearrange("p (s h) -> p s h", h=8), [bp], [b_posF])
                    for t4 in range(4):
                        pt, bp = next_pm()
                        for k in range(8):
                            mm(pt[:], uT[:, k, t4 * 128:(t4 + 1) * 128], win[:, k, 2608:3120], k == 0, k == 7,
                               [B["win"], buT[k]], [bp])
                        cp("act", vsb[:, :, t4, :], pt[:].rearrange("p (h d) -> p h d", d=64), [bp], [B["vsb"]])
                    for h in range(8):
                        dma("pool", v_d[h, :, c * 4:(c + 1) * 4, :], vsb[:, h, :, :], [B["vsb"]], ())
                    pDg = [pD[:], pm[0][:].rearrange("p (j l) -> p j l", l=128)]
                    pAg = [pAb[:], pm[1][:].rearrange("p (j l) -> p j l", l=128)]
                    bpD = [B["pD"], B["pm0"]]
                    bpA = [B["pAb"], B["pm1"]]
                    pGg = [pGs[:, 0:128], pGs[:, 384:512]]
                    bpG = [B["pG"], B["pG"]]
                    B["pst"] = B["pG"]

                    def ssd_prep(sc):
                        cs = slice(sc * 128, (sc + 1) * 128)
                        xb = xsB[sc % 2]
                        bxb = B[f"xsB{sc % 2}"]
                        for o in range(6):
                            tr(ptr[:, o * 128:(o + 1) * 128], xact[:, o, cs], ident_bf[:], [B["xact"], b_const], [B["ptr"]])
                        cp("act", xb[:], ptr[:, 0:768], [B["ptr"]], [bxb])
                        par = sc % 2
                        brh = B[f"rhsD{par}"]
                        for g in range(2):
                            tt("dve", rhsD[0:16, par, g, :, :],
                               acomb[0:16, cs].unsqueeze(1).broadcast_to([16, 4, 128]),
                               delta[0:16, g, :, :], ALU.mult, [B["acomb"], b_const, B["rhsD"]], [brh])

                    def stageA(sc, g):
                        cs = slice(sc * 128, (sc + 1) * 128)
                        par = sc % 2
                        brh = B[f"rhsD{par}"]
                        mm(pGg[g], xact[:, 4 + g, cs], xact[:, 6 + g, cs], True, True, [B["xact"]], [bpG[g]])
                        mm(pDg[g].rearrange("p j l -> p (j l)"), lhsD[0:48, cs],
                           rhsD[0:48, par, g, :, :].rearrange("p j l -> p (j l)"), True, False,
                           [B["lhsD"], B["rhsD"], brh], [bpD[g]])
                        mm(pDg[g].rearrange("p j l -> p (j l)"), ident_bf[:], ssdmask[:].rearrange("p j l -> p (j l)"),
                           False, True, [b_const], [bpD[g]])
                        mm(pAg[g].rearrange("p j l -> p (j l)"), ones_bf[0:16, :],
                           rhsD[0:16, par, g, :, :].rearrange("p j l -> p (j l)"), True, True,
                           [b_const, brh], [bpA[g]])

                    def stageAct(sc, g):
                        cs = slice(sc * 128, (sc + 1) * 128)
                        i = g
                        act(expD[i][:], pDg[g], AF.Exp, [bpD[g]], [B[f"expD{i}"]])
                        act(Eb[i][:], pAg[g], AF.Exp, [bpA[g]], [B[f"Eb{i}"]])
                        tt("dve", Wt[i][:], expD[i][:], pGg[g].unsqueeze(1).broadcast_to([128, 4, 128]), ALU.mult,
                           [B[f"expD{i}"], bpG[g]], [B[f"Wt{i}"]])
                        tt("pool", Cs[i][:], Eb[i][:], xact[:, 6 + g, cs].unsqueeze(1).broadcast_to([128, 4, 128]), ALU.mult,
                           [B[f"Eb{i}"], B["xact"]], [B[f"Cs{i}"]])

                    def stageB(sc, g):
                        i = g
                        xb = xsB[sc % 2]
                        bxb = B[f"xsB{sc % 2}"]
                        for j in range(4):
                            h = 4 * g + j
                            hp, half = h // 2, h % 2
                            mm(py[half * 64:(half + 1) * 64, hp, :], xb[:, h * 64:(h + 1) * 64], Wt[i][:, j, :], True, False,
                               [bxb, B[f"Wt{i}"]], [B["py"]])
                            mm(py[half * 64:(half + 1) * 64, hp, :], prev_b[:, g, j, :], Cs[i][:, j, :], False, True,
                               [B["prev_b"], B[f"Cs{i}"]], [B["py"]])

                    def stageC(sc, g):
                        i = g
                        xb = xsB[sc % 2]
                        bxb = B[f"xsB{sc % 2}"]
                        tt("dve", xdd[i][:], xb[:, g * 256:(g + 1) * 256].rearrange("p (j d) -> p j d", d=64),
                           expD[i][:, :, 127:128].broadcast_to([128, 4, 64]), ALU.mult,
                           [bxb, B[f"expD{i}"]], [B[f"xdd{i}"]])
                        mm(pGs[:, 128:384], xb[:, 512 + g * 128:512 + (g + 1) * 128], xdd[i][:].rearrange("p j d -> p (j d)"),
                           True, True, [bxb, B[f"xdd{i}"]], [B["pst"]])
                        tt("dve", prev_f[:, g, :, :], prev_f[:, g, :, :], Eb[i][:, :, 127:128].broadcast_to([128, 4, 64]), ALU.mult,
                           [B["prev_f"], B[f"Eb{i}"]], [B["prev_f"]])
                        tt("dve", prev_f[:, g, :, :], prev_f[:, g, :, :], pGs[:, 128:384].rearrange("p (j d) -> p j d", d=64), ALU.add,
                           [B["prev_f"], B["pst"]], [B["prev_f"]])
                        cp("pool", prev_b[:, g, :, :], prev_f[:, g, :, :], [B["prev_f"]], [B["prev_b"]])

                    ssd_prep(0)
                    for sc in range(4):
                        cs = slice(sc * 128, (sc + 1) * 128)
                        for g in range(2):
                            stageA(sc, g)
                        for g in range(2):
                            stageAct(sc, g)
                        if sc + 1 < 4:
                            ssd_prep(sc + 1)
                        for g in range(2):
                            stageB(sc, g)
                        for g in range(2):
                            stageC(sc, g)
                        for hp in range(4):
                            stt(ych[:, hp, cs], xact[:, hp, cs], PV(layer, 66 + hp), py[:, hp, :], ALU.mult, ALU.add,
                                [B["xact"], b_pvec, B["py"]], [B["ych"]])
                    tt("pool", ych[:], ych[:], zs[:], ALU.mult, [B["ych"], B["zs"]], [B["ych"]])

                    def finish_ssd(c0=c0):
                        for k in range(4):
                            act(sq[:, k, :], ych[:, k, :], AF.Square, [B["ych"]], [bsq1[k]])
                        for k in range(4):
                            mm(pn[:], ones_bf[:], sq[:, k, :], k == 0, k == 3, [b_const, bsq1[k]], [B["pn"]])
                        act(lnt[:], pn[:], AF.Ln, [B["pn"]], [B["lnt"]], bias=EPS, scale=1.0 / 512)
                        act(rstd[:], lnt[:], AF.Exp, [B["lnt"]], [B["rstd"]], scale=-0.5)
                        for k in range(4):
                            stt(yout[:, k, :], ych[:, k, :], PV(layer, 70 + k), rstd[:], ALU.mult, ALU.mult,
                                [B["ych"], b_pvec, B["rstd"]], [B["yout"]])
                        dma("pool", yssd_d.rearrange("(k p) t -> p k t", p=128)[:, :, c0:c0 + T], yout[:], [B["yout"]], ())

                    deferred[0] = finish_ssd
                deferred[0]()
                P.drain_dmas("sp")
                P.emit()
            if STOP_AFTER == (layer, "p1"):
                break

            with contextlib.ExitStack() as st:
                kaug = [sbuf(st, f"kaug{i}", [66, S], BF16) for i in range(2)]
                vaug = [sbuf(st, f"vaug{i}", [128, 32, 128], BF16) for i in range(2)]
                qaug = [sbuf(st, f"qaug{i}", [66, T], BF16) for i in range(2)]
                pT = [sbuf(st, f"pT{i}", [128, T], BF16) for i in range(3)]
                rec = [sbuf(st, f"rec{i}", [64, T], F32) for i in range(2)]
                osb = [sbuf(st, f"osb{i}", [64, T], F32) for i in range(2)]
                ps_s = [psum(st, f"ps_s{i}", [128, T], F32) for i in range(3)]
                ps_o = [psum(st, f"ps_o{i}", [128, T], F32) for i in range(2)]
                ps_w = psum(st, "ps_w", [128, T], F32)
                b_psw = Buf("ps_w")
                B = {n: Buf(n) for n in ["kaug0", "kaug1", "vaug0", "vaug1", "qaug0", "qaug1", "pT0", "pT1", "pT2",
                                         "rec0", "rec1", "osb0", "osb1", "ps_s0", "ps_s1", "ps_s2", "ps_o0", "ps_o1", "o_d"]}
                for i in range(2):
                    memset("pool", vaug[i][:, :, 64:128], 1.0, [B[f"vaug{i}"]])
                    memset("pool", kaug[i][64:66, :], 1.0, [B[f"kaug{i}"]])
                blocks = []
                for h in range(8):
                    for c in range(NCH):
                        for j in range(4 * c + 4):
                            blocks.append((h, c, j))
                NB = len(blocks)
                dma("sp", kaug[0][0:64, :], kaug_d[0, 0:64, :], (), [B["kaug0"]])
                dma("sp", vaug[0][:, :, 0:64], v_d[0], (), [B["vaug0"]])

                def s_step(bi):
                    h, c, j = blocks[bi]
                    hb = h % 2
                    qb = (h * NCH + c) % 2
                    si = bi % 3
                    if j == 0:
                        dma("sp", qaug[qb][:], qaug_d[h, :, c * T:(c + 1) * T], (), [B[f"qaug{qb}"]])
                        issue_precast(3 if layer == 0 else 2, len(precast))
                    lo = max(0, j - 4 * c) * 128
                    mm(ps_s[si][:, lo:T], kaug[hb][0:66, j * 128:(j + 1) * 128], qaug[qb][0:66, lo:T], True, j < 4 * c,
                       [B[f"kaug{hb}"], B[f"qaug{qb}"]], [B[f"ps_s{si}"]])
                    if j >= 4 * c:
                        mm(ps_s[si][:, lo:T], ident_bf[:], maskb[:, j - 4 * c, lo:T], False, True, [b_const], [B[f"ps_s{si}"]])

                s_step(0)
                s_step(1)
                for bi in range(NB):
                    h, c, j = blocks[bi]
                    hb = h % 2
                    qb = (h * NCH + c) % 2
                    si = bi % 3
                    nj = 4 * c + 4
                    po, bpo = ps_o[qb], B[f"ps_o{qb}"]
                    if bi + 2 < NB:
                        s_step(bi + 2)
                    lo = max(0, j - 4 * c) * 128
                    act(pT[si][:, lo:T], ps_s[si][:, lo:T], AF.Exp, [B[f"ps_s{si}"], b_posF], [B[f"pT{si}"]], bias=posF[:, j, h:h + 1])
                    mm(po[:, lo:T], vaug[hb][:, j, :], pT[si][:, lo:T], j == 0, j == nj - 1, [B[f"vaug{hb}"], B[f"pT{si}"]], [bpo])
                    if PE_WARM_DUMMY:
                        mm(ps_w[:], ident_bf[:], maskb[:, 0, :], True, True, [b_const], [b_psw])
                    if j == nj - 1:
                        P.op("dve", lambda e, qb=qb, po=po: e.reciprocal(out=rec[qb][:], in_=po[64:128, :]), [bpo], [B[f"rec{qb}"]])
                        tt("dve", osb[qb][:], po[0:64, :], rec[qb][:], ALU.mult, [bpo, B[f"rec{qb}"]], [B[f"osb{qb}"]])
                        dma("pool", o_d[h, :, c * T:(c + 1) * T], osb[qb][:], [B[f"osb{qb}"]], ())
                        if c == 1 and h + 1 < 8:
                            nb_ = (h + 1) % 2
                            dma("sp", kaug[nb_][0:64, :], kaug_d[h + 1, 0:64, :], (), [B[f"kaug{nb_}"]])
                            dma("sp", vaug[nb_][:, :, 0:64], v_d[h + 1], (), [B[f"vaug{nb_}"]])
                P.drain_dmas("sp")
                P.emit()
            if STOP_AFTER == (layer, "p2"):
                break

            with contextlib.ExitStack() as st:
                moe = (layer == 1)
                nexp = NE if moe else 1
                nf = (DFF_E if moe else DFF_D) // 128
                wo_s = sbuf(st, "wo_s", [128, 4, D], BF16)
                wo_a = sbuf(st, "wo_a", [64, 8, D], BF16)
                wpg = sbuf(st, "wpg", [128, 8, D], BF16)
                wpp = sbuf(st, "wpp", [128, 2, D], BF16)
                xc = sbuf(st, "xc3", [128, 8, T], F32)
                ys = sbuf(st, "ys3", [128, 4, T], BF16)
                oc = sbuf(st, "oc3", [64, 8, T], F32)
                ya = sbuf(st, "ya3", [64, 8, T], BF16)
                sq = sbuf(st, "sq3", [128, 8, T], BF16)
                uT = sbuf(st, "uT3", [128, 8, T], BF16)
                lnt = sbuf(st, "lnt3", [128, T], F32)
                rstd = sbuf(st, "rstd3", [128, T], F32)
                hT = sbuf(st, "hT3", [128, nf, T], BF16)
                sg = [sbuf(st, f"sg3{i}", [128, T], F32) for i in range(2)]
                n_gu = 3 if moe else 5
                n_dp = 2 if moe else 3
                wgp = [sbuf(st, f"wgp{i}", [128, 8, 128], BF16) for i in range(n_gu)]
                wup = [sbuf(st, f"wup{i}", [128, 8, 128], BF16) for i in range(n_gu)]
                wdp = [sbuf(st, f"wdp{i}", [128, nf, 128], BF16) for i in range(n_dp)]
                pc_b = sbuf(st, "pc_b", [128, 2, T], BF16)
                tmp = [sbuf(st, f"tmp3{i}", [128, T], F32) for i in range(2)]
                pm = [psum(st, f"pm3{i}", [128, T], F32) for i in range(6)]
                pn = psum(st, "pn3", [128, T], F32)
                names = ["wo_s", "wo_a", "wpg", "wpp", "xc", "ys", "oc", "osq", "ya", "sq", "uT", "lnt", "rstd", "hT",
                         "sg0", "sg1", "wgp0", "wgp1", "wgp2", "wup0", "wup1", "wup2", "wdp0", "wdp1", "pc_f", "pc_b",
                         "gate0", "gate1", "tmp0", "tmp1", "pm0", "pm1", "pm2", "pm3", "pm4", "pm5", "pn", "x_dst",
                         "wr", "u2f", "lg", "cmb", "cbc", "dg", "mx"]
                B = {n: Buf(n) for n in names}
                for i_ in range(5):
                    B.setdefault(f"wgp{i_}", Buf(f"wgp{i_}"))
                    B.setdefault(f"wup{i_}", Buf(f"wup{i_}"))
                    B.setdefault(f"wdp{i_}", Buf(f"wdp{i_}"))
                if moe:
                    wr = sbuf(st, "wr3", [128, 8, NE], F32)
                    u2f = sbuf(st, "u2f3", [128, 8, T], F32)
                    lg = sbuf(st, "lg3", [128, 4, NE], F32)
                    mx = sbuf(st, "mx3", [128, 4, 8], F32)
                    cmb = sbuf(st, "cmb3", [128, 4, NE], F32)
                    cm2 = sbuf(st, "cm23", [128, 4, NE], F32)
                    gsm = sbuf(st, "gsm3", [128, 4, 4], F32)
                    dg = sbuf(st, "dg3", [128, NE, 128], F32)
                    cbc = sbuf(st, "cbc3", [128, NE, T], F32)
                pmi = [0]

                def next_pm3():
                    i = pmi[0] % 6
                    pmi[0] += 1
                    return pm[i], B[f"pm{i}"]

                load_cast(wo_s, wout_in[layer, 0:512, :], D, (), [B["wo_s"]])
                wo_av = wout_in[layer, 512:1024, :].rearrange("(h p) n -> p h n", p=64)
                for h in range(8):
                    dma("pool", wo_a[:, h, :], wo_av[:, h, :], (), [B["wo_a"]])
                load_cast(wpg, wpg_in[layer], D, (), [B["wpg"]])
                load_cast(wpp, wpp_in[layer], D, (), [B["wpp"]])
                if moe:
                    dma("sp", wr[:], wr_in[0].rearrange("(k p) n -> p k n", p=128), (), [B["wr"]])

                bsq = [Buf(f"sq{k}") for k in range(8)]
                bxc = [Buf(f"xc{k}") for k in range(8)]
                buT = [Buf(f"uT3_{k}") for k in range(8)]
                bu2f = [Buf(f"u2f{k}") for k in range(8)]

                def rmsnorm_full(gcol0, want_f32):
                    for k in range(8):
                        act(sq[:, k, :], xc[:, k, :], AF.Square, [bxc[k]], [bsq[k], B["sq"]])
                    for k in range(8):
                        mm(pn[:], ones_bf[:], sq[:, k, :], k == 0, k == 7, [b_const, bsq[k]], [B["pn"]])
                    act(lnt[:], pn[:], AF.Ln, [B["pn"]], [B["lnt"]], bias=EPS, scale=1.0 / D)
                    act(rstd[:], lnt[:], AF.Exp, [B["lnt"]], [B["rstd"]], scale=-0.5)
                    for k in range(8):
                        if want_f32:
                            stt(u2f[:, k, :], xc[:, k, :], PV(layer, gcol0 + k), rstd[:], ALU.mult, ALU.mult,
                                [bxc[k], b_pvec, B["rstd"]], [bu2f[k]])
                            cp("act", uT[:, k, :], u2f[:, k, :], [bu2f[k]], [buT[k]])
                        else:
                            stt(uT[:, k, :], xc[:, k, :], PV(layer, gcol0 + k), rstd[:], ALU.mult, ALU.mult,
                                [bxc[k], b_pvec, B["rstd"]], [buT[k]])

                piece = [0]
                dpiece = [0]

                def load_side(cc):
                    cc0 = cc * T
                    dma("sp", ys[:], yssd_d.rearrange("(k p) t -> p k t", p=128)[:, :, cc0:cc0 + T], (), [B["ys"]])
                    dma("sp", oc[:], o_d.rearrange("h p t -> p h t")[:, :, cc0:cc0 + T], (), [B["oc"]])

                def attn_norm():
                    for h in range(8):
                        act(sq[0:64, h, :], oc[:, h, :], AF.Square, [B["oc"]], [bsq[h], B["sq"]])
                    for h in range(8):
                        mm(pn[:], ones_bf[0:64, :], sq[0:64, h, :], h == 0, h == 7, [b_const, bsq[h]], [B["pn"]])
                    act(lnt[:], pn[:], AF.Ln, [B["pn"]], [B["lnt"]], bias=EPS, scale=1.0 / 512)
                    act(rstd[:], lnt[:], AF.Exp, [B["lnt"]], [B["rstd"]], scale=-0.5)
                    for h in range(8):
                        stt(ya[:, h, :], oc[:, h, :], PV(layer, 74 + h)[0:64, :], rstd[0:64, :], ALU.mult, ALU.mult,
                            [B["oc"], b_pvec, B["rstd"]], [B["ya"]])

                load_side(0)
                for c in range(NCH):
                    c0 = c * T
                    for k in range(8):
                        dma("sp", xc[:, k, :], xsv[:, k, c0:c0 + T], (), [bxc[k]])
                    dma("pool", pc_b[:], pT_in[layer].rearrange("(k p) t -> p k t", p=128)[:, :, c0:c0 + T], (), [B["pc_b"]])
                    if c == 0:
                        attn_norm()
                    for o in range(8):
                        pt, bp = next_pm3()
                        for j in range(4):
                            mm(pt[:], wo_s[:, j, o * 128:(o + 1) * 128], ys[:, j, :], j == 0, False, [B["wo_s"], B["ys"]], [bp])
                        for h in range(8):
                            mm(pt[:], wo_a[:, h, o * 128:(o + 1) * 128], ya[:, h, :], False, h == 7, [B["wo_a"], B["ya"]], [bp])
                        tt("dve", xc[:, o, :], xc[:, o, :], pt[:], ALU.add, [bxc[o], bp], [bxc[o]])
                    if c + 1 < NCH:
                        load_side(c + 1)
                    rmsnorm_full(8, moe)
                    def router_part1():
                        pt, bp = next_pm3()
                        for t4 in range(4):
                            for k in range(8):
                                mm(pt[:, t4 * 8:(t4 + 1) * 8], u2f[:, k, t4 * 128:(t4 + 1) * 128], wr[:, k, :], k == 0, k == 7,
                                   [bu2f[k], B["wr"]], [bp])
                        cp("dve", lg[:].rearrange("p a b -> p (a b)"), pt[:, 0:32], [bp], [B["lg"]])
                        for t4 in range(4):
                            P.op("dve", lambda e, t4=t4: e.max(out=mx[:, t4, :], in_=lg[:, t4, :]), [B["lg"]], [B["mx"]])
                        tt("dve", gsm[:, :, 0:1], mx[:, :, 1:2], mx[:, :, 0:1], ALU.subtract, [B["mx"]], [B["cmb"]])
                        act(gsm[:, :, 1:2], gsm[:, :, 0:1], AF.Exp, [B["cmb"]], [B["cmb"]])
                        ts("dve", gsm[:, :, 2:3], gsm[:, :, 1:2], 1.0, None, ALU.add, None, [B["cmb"]], [B["cmb"]])
                        P.op("dve", lambda e: e.reciprocal(out=gsm[:, :, 2:3], in_=gsm[:, :, 2:3]), [B["cmb"]], [B["cmb"]])
                        tt("dve", gsm[:, :, 3:4], gsm[:, :, 1:2], gsm[:, :, 2:3], ALU.mult, [B["cmb"]], [B["cmb"]])
                        for t4 in range(4):
                            ts("dve", cmb[:, t4, :], lg[:, t4, :], mx[:, t4, 0:1], gsm[:, t4, 2:3], ALU.is_equal, ALU.mult,
                               [B["lg"], B["mx"], B["cmb"]], [B["cmb"]])
                            ts("dve", cm2[:, t4, :], lg[:, t4, :], mx[:, t4, 1:2], gsm[:, t4, 3:4], ALU.is_equal, ALU.mult,
                               [B["lg"], B["mx"], B["cmb"]], [B["cmb"]])
                        tt("dve", cmb[:], cmb[:], cm2[:], ALU.add, [B["cmb"]], [B["cmb"]])
                    def router_part2():
                        for t4 in range(4):
                            tt("dve", dg[:], ident_f[:].unsqueeze(1).broadcast_to([128, NE, 128]),
                               cmb[:, t4, :].unsqueeze(2).broadcast_to([128, NE, 128]), ALU.mult,
                               [b_const, B["cmb"]], [B["dg"]])
                            for eh in range(2):
                                pt, bp = next_pm3()
                                mm(pt[:], ones_f[:], dg[:, eh * 4:(eh + 1) * 4, :].rearrange("p e t -> p (e t)"), True, True,
                                   [b_const, B["dg"]], [bp])
                                cp("act", cbc[:, eh * 4:(eh + 1) * 4, t4 * 128:(t4 + 1) * 128],
                                   pt[:].rearrange("p (e t) -> p e t", t=128), [bp], [B["cbc"]])
                    for e_ in range(nexp):
                        if moe:
                            wg_src, wu_src, wd_src = wge_b[e_], wue_b[e_], wde_b[e_]
                            kg, ku, kd = "ge", "ue", "de"
                        else:
                            wg_src, wu_src, wd_src = wgd_b[0], wud_b[0], wdd_b[0]
                            kg, ku, kd = "gd", "ud", "dd"
                        for f in range(nf):
                            pi = piece[0] % n_gu
                            piece[0] += 1
                            dma("sp", wgp[pi][:].rearrange("p k n -> p (k n)"), wg_src[f], [WB[(kg, e_, f)]], [B[f"wgp{pi}"]])
                            dma("sp", wup[pi][:].rearrange("p k n -> p (k n)"), wu_src[f], [WB[(ku, e_, f)]], [B[f"wup{pi}"]])
                            pg, bpg = next_pm3()
                            for k in range(8):
                                mm(pg[:], wgp[pi][:, k, :], uT[:, k, :], k == 0, k == 7, [B[f"wgp{pi}"], buT[k]], [bpg])
                            pu, bpu = next_pm3()
                            for k in range(8):
                                mm(pu[:], wup[pi][:, k, :], uT[:, k, :], k == 0, k == 7, [B[f"wup{pi}"], buT[k]], [bpu])
                            si = f % 2
                            act(sg[si][:], pg[:], AF.Silu, [bpg], [B[f"sg{si}"]])
                            tt("dve", hT[:, f, :], sg[si][:], pu[:], ALU.mult, [B[f"sg{si}"], bpu], [B["hT"]])
                            if c + 1 < NCH and ((moe and e_ == 1 and f == 2) or (not moe and f == 10)):
                                attn_norm()
                            if moe and e_ == 0 and f == 2:
                                router_part1()
                            if moe and e_ == 0 and f == 7:
                                router_part2()
                        for o in range(8):
                            di = o % 2
                            dpi = dpiece[0] % n_dp
                            dpiece[0] += 1
                            dma("sp", wdp[dpi][:].rearrange("p f n -> p (f n)"), wd_src[o], [WB[(kd, e_, o)]], [B[f"wdp{dpi}"]])
                            pt, bp = next_pm3()
                            for f in range(nf):
                                mm(pt[:], wdp[dpi][:, f, :], hT[:, f, :], f == 0, f == nf - 1, [B[f"wdp{dpi}"], B["hT"]], [bp])
                            if moe:
                                tt("dve", tmp[di][:], pt[:], cbc[:, e_, :], ALU.mult, [bp, B["cbc"]], [B[f"tmp{di}"]])
                                tt("dve", xc[:, o, :], xc[:, o, :], tmp[di][:], ALU.add, [bxc[o], B[f"tmp{di}"]], [bxc[o]])
                            else:
                                tt("dve", xc[:, o, :], xc[:, o, :], pt[:], ALU.add, [bxc[o], bp], [bxc[o]])
                    rmsnorm_full(16, False)
                    for o in range(8):
                        gi = o % 2
                        pt, bp = next_pm3()
                        for k in range(8):
                            mm(pt[:], wpg[:, k, o * 128:(o + 1) * 128], uT[:, k, :], k == 0, k == 7, [B["wpg"], buT[k]], [bp])
                        act(sg[gi][:], pt[:], AF.Sigmoid, [bp], [B[f"sg{gi}"]])
                        pt2, bp2 = next_pm3()
                        for k in range(2):
                            mm(pt2[:], wpp[:, k, o * 128:(o + 1) * 128], pc_b[:, k, :], k == 0, k == 1, [B["wpp"], B["pc_b"]], [bp2])
                        tt("dve", tmp[gi][:], sg[gi][:], pt2[:], ALU.mult, [B[f"sg{gi}"], bp2], [B[f"tmp{gi}"]])
                        tt("dve", tmp[gi][:], xc[:, o, :], tmp[gi][:], ALU.add, [bxc[o], B[f"tmp{gi}"]], [B[f"tmp{gi}"]])
                        dma("pool", xdv[:, o, c0:c0 + T], tmp[gi][:], [B[f"tmp{gi}"]], ())
                P.drain_dmas("sp")
                P.emit()
            if STOP_AFTER == (layer, "p3"):
                break
    return nc


def _prep_shared(inp):
    f32 = np.float32
    pvec = np.zeros((128, 2 * NPL), f32)
    wincat = np.zeros((2, D, NCAT), f32)
    for l in range(2):
        b = l * NPL
        pvec[:, b + 0:b + 8] = inp["norm1_g"][l].reshape(8, 128).T
        pvec[:, b + 8:b + 16] = inp["norm2_g"][l].reshape(8, 128).T
        pvec[:, b + 16:b + 24] = inp["ple_norm_g"][l].reshape(8, 128).T
        for tap in range(4):
            pvec[:, b + 24 + tap * 8:b + 32 + tap * 8] = inp["conv_w"][l, tap].reshape(8, 128).T
        pvec[:, b + 56:b + 64] = inp["conv_b"][l].reshape(8, 128).T
        for r0 in (0, 8, 32, 40):
            pvec[r0:r0 + 8, b + 64] = inp["dt_bias"][l]
            pvec[r0:r0 + 8, b + 65] = inp["a_log"][l]
        pvec[:, b + 66:b + 70] = np.repeat(inp["d_skip"][l], 64).reshape(4, 128).T
        pvec[:, b + 70:b + 74] = inp["ssd_norm_g"][l].reshape(4, 128).T
        pvec[0:64, b + 74:b + 82] = inp["attn_norm_g"][l].reshape(8, 64).T
        pvec[0:64, b + 82] = inp["q_norm_g"][l]
        pvec[64:128, b + 82] = inp["q_norm_g"][l]
        pvec[0:64, b + 83] = inp["k_norm_g"][l]
        pvec[64:128, b + 83] = inp["k_norm_g"][l]
        pvec[0:8, b + 84] = inp["fg_bias"][l]
        w = inp["w_in"][l]
        wincat[l, :, 0:1536] = w[:, 0:1536]
        for r0 in (0, 8, 32, 40):
            wincat[l, :, 1536 + r0:1544 + r0] = w[:, 1536:1544]
        wincat[l, :, 1584:2096] = w[:, 1544:2056]
        wincat[l, :, 2096:2608] = w[:, 2056:2568]
        wincat[l, :, 2608:3120] = w[:, 2568:3080]
        wincat[l, :, 3120:3128] = w[:, 3080:3088]
    c = np.ascontiguousarray

    def gu_layout(w):
        E, _, F = w.shape
        return c(w.reshape(E, 8, 128, F // 128, 128).transpose(0, 3, 2, 1, 4).reshape(E, F // 128, 128, 1024), dtype=f32)

    def d_layout(w):
        E, F, _ = w.shape
        return c(w.reshape(E, F // 128, 128, 8, 128).transpose(0, 3, 2, 1, 4).reshape(E, 8, 128, F), dtype=f32)

    return {
        "pvec": pvec, "wincat": wincat, "wout": c(inp["w_out"], dtype=f32),
        "wgd": gu_layout(inp["w_gate_dense"]), "wud": gu_layout(inp["w_up_dense"]),
        "wdd": d_layout(inp["w_down_dense"]), "wr": c(inp["w_router"], dtype=f32),
        "wge": gu_layout(inp["w_gate_exp"][0]), "wue": gu_layout(inp["w_up_exp"][0]),
        "wde": d_layout(inp["w_down_exp"][0]),
        "wpg": c(inp["w_ple_gate"], dtype=f32), "wpp": c(inp["w_ple_proj"], dtype=f32),
    }


def kernel(**inputs):
    inp = {k: np.asarray(v) for k, v in inputs.items()}
    shared = _prep_shared(inp)
    x = inp["x"].astype(np.float32, copy=False)
    p = inp["p"].astype(np.float32, copy=False)
    in_maps = []
    for b in range(8):
        m = dict(shared)
        m["xT"] = np.ascontiguousarray(x[b].T)
        m["pT"] = np.ascontiguousarray(p[:, b].transpose(0, 2, 1))
        in_maps.append(m)
    nc = build_program()
    res = run_bass_kernel_spmd(nc, in_maps, core_ids=list(range(8)))
    out = np.stack([np.ascontiguousarray(r["yT"].T) for r in res.results], axis=0)
    return out.astype(np.float32, copy=False)
```
